# Optimizing a Trainium2 kernel written in Bass

```python
import math
import jax, jax.numpy as jnp
from jax import lax
import numpy as np

D_MODEL = 1024
BATCH = 4
SEQ = 8192
DEPTH = 1
DEC_BATCH = 16
DEC_SEQ = 16
PAST_LEN = 4096

CHUNK = 64
QBLOCK = 128
EPS = 1e-6
MLA_HEADS = 8
MLA_NOPE = 64
MLA_ROPE = 32
MLA_V = 64
Q_LORA = 256
KV_LORA = 256
ROPE_THETA = 10000.0
MLA_SCALE = (MLA_NOPE + MLA_ROPE) ** -0.5
DIFF_HEADS = 4
DIFF_DH = 64
DIFF_V = 2 * DIFF_DH
DIFF_SCALE = DIFF_DH ** -0.5
NUM_BUCKETS = 32
MAX_DISTANCE = 128
MLA_WIDTH = MLA_HEADS * MLA_V
DIFF_WIDTH = DIFF_HEADS * DIFF_V
MIX_WIDTH = MLA_WIDTH + DIFF_WIDTH
DIFF_QK = DIFF_HEADS * 2 * DIFF_DH
IN_SIZES = (Q_LORA, KV_LORA, MLA_ROPE, DIFF_QK, DIFF_QK, DIFF_WIDTH)
IN_WIDTH = Q_LORA + KV_LORA + MLA_ROPE + 2 * DIFF_QK + DIFF_WIDTH
N_EXPERTS = 256
TOP_K = 8
N_GROUPS = 8
TOPK_GROUPS = 4
EXPERT_FF = 256
SHARED_FF = 256
ROUTE_SCALE = 2.5
ROW_BLOCK = 128

kernel_name = 'hybrid_mla_diffattn_moe_stream_step'


def _rmsnorm(x, g):
    xf = x.astype(jnp.float32)
    y = xf * lax.rsqrt(jnp.mean(xf * xf, axis=-1, keepdims=True) + EPS)
    return (y * g.astype(jnp.float32)).astype(x.dtype)


def _rope(x, pos):
    half = x.shape[-1] // 2
    inv = ROPE_THETA ** (-jnp.arange(half, dtype=jnp.float32) / half)
    ang = pos.astype(jnp.float32)[:, None] * inv
    shp = (pos.shape[0],) + (1,) * (x.ndim - 3) + (half,)
    cos, sin = jnp.cos(ang).reshape(shp), jnp.sin(ang).reshape(shp)
    xf = x.astype(jnp.float32)
    x1, x2 = xf[..., :half], xf[..., half:]
    return jnp.concatenate([x1 * cos - x2 * sin, x2 * cos + x1 * sin], axis=-1).astype(x.dtype)


def _rel_bucket(rel):
    nb = NUM_BUCKETS // 2
    max_exact = nb // 2
    n = jnp.abs(rel)
    nf = jnp.maximum(n, 1).astype(jnp.float32)
    large = max_exact + (jnp.log(nf / max_exact) / math.log(MAX_DISTANCE / max_exact)
                         * (nb - max_exact)).astype(jnp.int32)
    large = jnp.minimum(large, nb - 1)
    return jnp.where(rel > 0, nb, 0) + jnp.where(n < max_exact, n, large)


def _chunk_mask(q_pos, k_pos):
    return (k_pos[None, :] // CHUNK) <= (q_pos[:, None] // CHUNK)


def _split_cols(a, sizes):
    out, start = [], 0
    for sz in sizes:
        out.append(a[..., start:start + sz])
        start += sz
    return out


def _sweep(fn, q_pos, *qs):
    s = q_pos.shape[0]
    blk = QBLOCK if s % QBLOCK == 0 else s
    nb = s // blk

    def split(a):
        return jnp.moveaxis(a.reshape(a.shape[0], nb, blk, *a.shape[2:]), 1, 0)

    out = lax.map(lambda args: fn(*args), (q_pos.reshape(nb, blk),) + tuple(split(a) for a in qs))
    out = jnp.moveaxis(out, 0, 1)
    return out.reshape(out.shape[0], s, *out.shape[3:])


def _mixers(h, q_pos, past, p, l, lam_init):
    b, s, _ = h.shape
    c_q, c_kv, k_pe, dq, dk, dv = _split_cols(h @ p['w_in'][l], IN_SIZES)
    c_kv = _rmsnorm(c_kv, p['mla_kv_norm'][l])
    k_pe = _rope(k_pe, q_pos)
    q = (_rmsnorm(c_q, p['mla_q_norm'][l]) @ p['w_uq'][l]).reshape(b, s, MLA_HEADS, MLA_NOPE + MLA_ROPE)
    q_nope, q_pe = q[..., :MLA_NOPE], _rope(q[..., MLA_NOPE:], q_pos)
    dq = dq.reshape(b, s, DIFF_HEADS, 2, DIFF_DH)
    dk = dk.reshape(b, s, DIFF_HEADS, 2, DIFF_DH)
    dv = dv.reshape(b, s, DIFF_HEADS, DIFF_V)
    new_rows = (c_kv, k_pe, dk, dv)
    if past is None:
        k_pos = q_pos
        lat_all, kpe_all, dk_all, dv_all = new_rows
    else:
        k_pos = jnp.concatenate([jnp.arange(past[0].shape[1], dtype=jnp.int32), q_pos])
        lat_all, kpe_all, dk_all, dv_all = (jnp.concatenate([a, n.astype(a.dtype)], axis=1)
                                            for a, n in zip(past, new_rows))
    kv = (lat_all @ p['w_ukv'][l]).reshape(b, -1, MLA_HEADS, MLA_NOPE + MLA_V)
    k_nope, v_mla = kv[..., :MLA_NOPE], kv[..., MLA_NOPE:]

    def mla_blk(qp, qn, qr):
        sc = (jnp.einsum('bqhd,bkhd->bhqk', qn, k_nope)
              + jnp.einsum('bqhr,bkr->bhqk', qr, kpe_all)).astype(jnp.float32) * MLA_SCALE
        sc = jnp.where(_chunk_mask(qp, k_pos), sc, -jnp.inf)
        pr = jax.nn.softmax(sc, axis=-1).astype(v_mla.dtype)
        return jnp.einsum('bhqk,bkhd->bqhd', pr, v_mla)

    o_mla = _sweep(mla_blk, q_pos, q_nope, q_pe).reshape(b, s, MLA_WIDTH)

    lam = (jnp.exp(jnp.sum((p['lambda_q1'][l] * p['lambda_k1'][l]).astype(jnp.float32)))
           - jnp.exp(jnp.sum((p['lambda_q2'][l] * p['lambda_k2'][l]).astype(jnp.float32)))
           + lam_init)
    rel_bias = p['rel_bias']

    def diff_blk(qp, qb):
        sc = jnp.einsum('bqhmd,bkhmd->bhmqk', qb, dk_all).astype(jnp.float32) * DIFF_SCALE
        bias = jnp.take(rel_bias, _rel_bucket(k_pos[None, :] - qp[:, None]), axis=0)
        sc = sc + jnp.transpose(bias, (2, 3, 0, 1)).astype(jnp.float32)
        sc = jnp.where(_chunk_mask(qp, k_pos), sc, -jnp.inf)
        pr = jax.nn.softmax(sc, axis=-1)
        wgt = pr[:, :, 0] - lam * pr[:, :, 1]
        return jnp.einsum('bhqk,bkhd->bqhd', wgt.astype(dv_all.dtype), dv_all)

    o_diff = _sweep(diff_blk, q_pos, dq)
    o_diff = _rmsnorm(o_diff, p['diff_subln'][l]) * (1.0 - lam_init)
    o = jnp.concatenate([o_mla, o_diff.reshape(b, s, DIFF_WIDTH)], axis=-1)
    return o, new_rows


def _swiglu(x, w_gu, w_down):
    g, u = jnp.split(x @ w_gu, 2, axis=-1)
    return (jax.nn.silu(g) * u) @ w_down


def _dispatch(x, idx, gates, w_gu, w_down):
    t, d = x.shape
    a = t * TOP_K
    flat_e = idx.reshape(a)
    order = jnp.argsort(flat_e)
    e_s = flat_e[order]
    t_s = (order // TOP_K).astype(jnp.int32)
    g_s = gates.reshape(a)[order]
    counts = jnp.bincount(flat_e, length=N_EXPERTS)
    padded = (counts + ROW_BLOCK - 1) // ROW_BLOCK * ROW_BLOCK
    pad_end = jnp.cumsum(padded)
    pad_start = pad_end - padded
    grp_start = jnp.cumsum(counts) - counts
    dest = pad_start[e_s] + jnp.arange(a) - grp_start[e_s]
    n_blocks = -(-a // ROW_BLOCK) + N_EXPERTS
    n_slots = n_blocks * ROW_BLOCK
    slot_tok = jnp.full((n_slots,), t, jnp.int32).at[dest].set(t_s)
    slot_gate = jnp.zeros((n_slots,), gates.dtype).at[dest].set(g_s)
    blk_e = jnp.minimum(jnp.searchsorted(pad_end, jnp.arange(n_blocks) * ROW_BLOCK, side='right'),
                        N_EXPERTS - 1)
    x_pad = jnp.concatenate([x, jnp.zeros((1, d), x.dtype)], axis=0)

    def run(args):
        tok, e = args
        return _swiglu(x_pad[tok], w_gu[e], w_down[e])

    rows = lax.map(run, (slot_tok.reshape(n_blocks, ROW_BLOCK), blk_e))
    rows = rows.reshape(n_slots, d) * slot_gate[:, None].astype(x.dtype)
    return jax.ops.segment_sum(rows, slot_tok, num_segments=t + 1)[:t]


def _moe(h, w_router, router_bias, w_gu, w_down, w_sh_gu, w_sh_down):
    b, s, d = h.shape
    x = h.reshape(b * s, d)
    t = x.shape[0]
    scores = jax.nn.sigmoid((x @ w_router).astype(jnp.float32))
    biased = scores + router_bias.astype(jnp.float32)
    grp_score = jnp.sum(lax.top_k(biased.reshape(t, N_GROUPS, -1), 2)[0], axis=-1)
    _, top_g = lax.top_k(grp_score, TOPK_GROUPS)
    gmask = jnp.any(top_g[..., None] == jnp.arange(N_GROUPS), axis=-2)
    masked = jnp.where(jnp.repeat(gmask, N_EXPERTS // N_GROUPS, axis=-1), biased, -jnp.inf)
    _, idx = lax.top_k(masked, TOP_K)
    sel = jnp.take_along_axis(scores, idx, axis=-1)
    gates = sel / jnp.sum(sel, axis=-1, keepdims=True) * ROUTE_SCALE
    routed = _dispatch(x, idx, gates, w_gu, w_down)
    shared = _swiglu(x, w_sh_gu, w_sh_down)
    return (routed + shared).reshape(b, s, d)


def _trunk(x, c, q_pos, past, p):
    rows = []
    for l in range(DEPTH):
        lam_init = 0.8 - 0.6 * math.exp(-0.3 * l)
        mod = jax.nn.silu(c) @ p['w_ada'][l] + p['b_ada'][l]
        sh_a, sc_a, g_a, sh_f, sc_f, g_f = jnp.split(mod[:, None, :], 6, axis=-1)
        h = _rmsnorm(x, p['norm_attn'][l]) * (1 + sc_a) + sh_a
        lay_past = None if past is None else tuple(a[l] for a in past)
        o, new = _mixers(h, q_pos, lay_past, p, l, lam_init)
        x = x + g_a * (o @ p['w_out'][l])
        h = _rmsnorm(x, p['norm_ffn'][l]) * (1 + sc_f) + sh_f
        x = x + g_f * _moe(h, p['w_router'][l], p['router_bias'][l], p['w_exp_gu'][l],
                           p['w_exp_down'][l], p['w_shared_gu'][l], p['w_shared_down'][l])
        rows.append(new)
    lat = jnp.stack([r[0] for r in rows])
    kpe = jnp.stack([r[1] for r in rows])
    dk = jnp.stack([r[2] for r in rows])
    dv = jnp.stack([r[3] for r in rows])
    return _rmsnorm(x, p['final_norm']), (lat, kpe, dk, dv)


def setup_inputs(seed: int = 0) -> dict:
    key = jax.random.key(seed)
    ks = iter(jax.random.split(key, 48))

    def nrm(shape, scale=1.0):
        return jax.random.normal(next(ks), shape, jnp.float32) * scale

    def gain(shape):
        return 1.0 + nrm(shape, 0.02)

    return {
        'x_prompt': nrm((BATCH, SEQ, D_MODEL)),
        'x_sample': nrm((DEC_BATCH, DEC_SEQ, D_MODEL)),
        'cache_mla_latent': nrm((DEPTH, DEC_BATCH, PAST_LEN, KV_LORA)),
        'cache_mla_krope': nrm((DEPTH, DEC_BATCH, PAST_LEN, MLA_ROPE)),
        'cache_diff_k': nrm((DEPTH, DEC_BATCH, PAST_LEN, DIFF_HEADS, 2, DIFF_DH)),
        'cache_diff_v': nrm((DEPTH, DEC_BATCH, PAST_LEN, DIFF_HEADS, DIFF_V)),
        'c_prompt': nrm((BATCH, D_MODEL)),
        'c_sample': nrm((DEC_BATCH, D_MODEL)),
        'w_ada': nrm((DEPTH, D_MODEL, 6 * D_MODEL), 0.5 * D_MODEL ** -0.5),
        'b_ada': nrm((DEPTH, 6 * D_MODEL), 0.02),
        'norm_attn': gain((DEPTH, D_MODEL)),
        'w_in': nrm((DEPTH, D_MODEL, IN_WIDTH), D_MODEL ** -0.5),
        'mla_q_norm': gain((DEPTH, Q_LORA)),
        'mla_kv_norm': gain((DEPTH, KV_LORA)),
        'w_uq': nrm((DEPTH, Q_LORA, MLA_HEADS * (MLA_NOPE + MLA_ROPE)), Q_LORA ** -0.5),
        'w_ukv': nrm((DEPTH, KV_LORA, MLA_HEADS * (MLA_NOPE + MLA_V)), KV_LORA ** -0.5),
        'lambda_q1': nrm((DEPTH, DIFF_DH), 0.1),
        'lambda_k1': nrm((DEPTH, DIFF_DH), 0.1),
        'lambda_q2': nrm((DEPTH, DIFF_DH), 0.1),
        'lambda_k2': nrm((DEPTH, DIFF_DH), 0.1),
        'diff_subln': gain((DEPTH, DIFF_V)),
        'rel_bias': nrm((NUM_BUCKETS, DIFF_HEADS, 2), 0.2),
        'w_out': nrm((DEPTH, MIX_WIDTH, D_MODEL), MIX_WIDTH ** -0.5),
        'norm_ffn': gain((DEPTH, D_MODEL)),
        'w_router': nrm((DEPTH, D_MODEL, N_EXPERTS), D_MODEL ** -0.5),
        'router_bias': nrm((DEPTH, N_EXPERTS), 0.01),
        'w_exp_gu': nrm((DEPTH, N_EXPERTS, D_MODEL, 2 * EXPERT_FF), D_MODEL ** -0.5),
        'w_exp_down': nrm((DEPTH, N_EXPERTS, EXPERT_FF, D_MODEL), EXPERT_FF ** -0.5),
        'w_shared_gu': nrm((DEPTH, D_MODEL, 2 * SHARED_FF), D_MODEL ** -0.5),
        'w_shared_down': nrm((DEPTH, SHARED_FF, D_MODEL), SHARED_FF ** -0.5),
        'final_norm': gain((D_MODEL,)),
    }


def reference(x_prompt, x_sample, cache_mla_latent, cache_mla_krope, cache_diff_k, cache_diff_v,
              c_prompt, c_sample, w_ada, b_ada, norm_attn, w_in, mla_q_norm, mla_kv_norm, w_uq, w_ukv,
              lambda_q1, lambda_k1, lambda_q2, lambda_k2, diff_subln, rel_bias, w_out, norm_ffn,
              w_router, router_bias, w_exp_gu, w_exp_down, w_shared_gu, w_shared_down, final_norm):
    p = dict(w_ada=w_ada, b_ada=b_ada, norm_attn=norm_attn, w_in=w_in, mla_q_norm=mla_q_norm,
             mla_kv_norm=mla_kv_norm, w_uq=w_uq, w_ukv=w_ukv, lambda_q1=lambda_q1, lambda_k1=lambda_k1,
             lambda_q2=lambda_q2, lambda_k2=lambda_k2, diff_subln=diff_subln, rel_bias=rel_bias,
             w_out=w_out, norm_ffn=norm_ffn, w_router=w_router, router_bias=router_bias,
             w_exp_gu=w_exp_gu, w_exp_down=w_exp_down, w_shared_gu=w_shared_gu,
             w_shared_down=w_shared_down, final_norm=final_norm)
    pos_p = jnp.arange(x_prompt.shape[1], dtype=jnp.int32)
    y_prompt, (lat_p, kpe_p, dk_p, dv_p) = _trunk(x_prompt, c_prompt, pos_p, None, p)
    past_len = cache_mla_latent.shape[2]
    pos_s = past_len + jnp.arange(x_sample.shape[1], dtype=jnp.int32)
    y_sample, (lat_s, kpe_s, dk_s, dv_s) = _trunk(
        x_sample, c_sample, pos_s, (cache_mla_latent, cache_mla_krope, cache_diff_k, cache_diff_v), p)
    return (y_prompt, y_sample, lat_p, kpe_p, dk_p, dv_p, lat_s, kpe_s, dk_s, dv_s)
```

```python
import math
import os
from contextlib import ExitStack
import numpy as np
import concourse.bass as bass
import concourse.mybir as mybir
from concourse.bass_utils import run_bass_kernel_spmd

F32 = mybir.dt.float32; BF16 = mybir.dt.bfloat16; I32 = mybir.dt.int32
ALU = mybir.AluOpType; AF = mybir.ActivationFunctionType; AX = mybir.AxisListType
ENG = ("pe", "act", "dve", "pool", "sp")
EPS = 1e-6
MLA_SCALE = 96 ** -0.5
DIFF_SCALE = 64 ** -0.5
LAM_INIT = 0.8 - 0.6 * math.exp(0.0)
CAP = 640
NST = CAP // 128
HALF = 128 * CAP
BIG = 1.0e6
STAGE = 9
KCUT = int(os.environ.get('KCUT', '99'))


class Buf:
    __slots__ = ("w", "r", "excl")

    def __init__(self, excl=False):
        self.w = []; self.r = []; self.excl = excl


def _prune(toks):
    best = {}
    for t in toks:
        k = (t[0], t[1])
        if k not in best or best[k][2] < t[2]:
            best[k] = t
    return list(best.values())


class Prog:
    def __init__(self, nc, es, nds=48):
        self.nc = nc
        self.ops = {e: [] for e in ENG}
        self.need = {e: set() for e in ENG}
        self.psem = {e: es.enter_context(nc.semaphore("pg_" + e)) for e in ENG}
        self.dsem = [es.enter_context(nc.semaphore("dq%d" % i)) for i in range(nds)]
        self.dcnt = [0] * nds
        self.dn = 0; self.dns = 0; self.NHW = 32
        self.nops = 0; self.limit = int(os.environ.get("KOPS", "100000000"))
        self.pend = {e: [] for e in ENG}

    def _deps(self, eng, reads, writes, waits):
        toks = list(waits) + self.pend[eng]
        self.pend[eng] = []
        for b in reads:
            toks += b.w
            if b.excl:
                toks += [t for t in b.r if not (t[0] == "c" and t[1] == eng)]
        for b in writes:
            toks += b.w; toks += b.r
        res = []
        for t in _prune(toks):
            if t[0] == "c":
                if t[1] == eng and eng == "pe":
                    continue
                self.need[t[1]].add(t[2])
            res.append(t)
        return res

    def _upd(self, tok, reads, writes):
        for b in reads:
            b.r = _prune(b.r + [tok])
        for b in writes:
            b.w = [tok]; b.r = []

    def op(self, eng, fn, reads=(), writes=(), waits=()):
        self.nops += 1
        if self.nops > self.limit:
            return ("d", 0, 0)
        if os.environ.get("KDBG"):
            import inspect
            fr = inspect.stack()[1]; fr2 = inspect.stack()[2]
            print("OP", self.nops, eng, fr.lineno, fr2.lineno)
        deps = self._deps(eng, reads, writes, waits)
        tok = ("c", eng, len(self.ops[eng]))
        self.ops[eng].append((fn, deps, None))
        self._upd(tok, reads, writes)
        return tok

    def dma(self, eng, fn, reads=(), writes=(), waits=()):
        self.nops += 1
        if self.nops > self.limit:
            return ("d", 0, 0)
        if os.environ.get("KDBG"):
            import inspect
            fr = inspect.stack()[1]; fr2 = inspect.stack()[2]
            print("DMA", self.nops, eng, fr.lineno, fr2.lineno)
        if eng == "pool":
            i = self.NHW + self.dns; self.dns = (self.dns + 1) % (len(self.dsem) - self.NHW)
        else:
            i = self.dn; self.dn = (self.dn + 1) % self.NHW
        w = list(waits)
        if self.dcnt[i] > 0:
            w.append(("d", i, self.dcnt[i]))
        deps = self._deps(eng, reads, writes, w)
        self.dcnt[i] += 16
        tok = ("d", i, self.dcnt[i])
        self.ops[eng].append((fn, deps, i))
        self._upd(tok, reads, writes)
        return tok

    def barrier(self):
        toks = []
        for e in ENG:
            for idx in range(len(self.ops[e]) - 1, -1, -1):
                if self.ops[e][idx][2] is None and self.ops[e][idx][0] is not None:
                    toks.append(("c", e, idx))
                    break
        for i, c in enumerate(self.dcnt):
            if c > 0:
                toks.append(("d", i, c))
        for e in ENG:
            self.pend[e] = self.pend[e] + toks

    def check(self):
        rank = {e: {idx: i + 1 for i, idx in enumerate(sorted(self.need[e]))} for e in ENG}
        pc = {e: 0 for e in ENG}; cs = {e: 0 for e in ENG}; ds = [0] * len(self.dsem)
        while True:
            prog = False
            for e in ENG:
                while pc[e] < len(self.ops[e]):
                    fn, deps, di = self.ops[e][pc[e]]
                    ok = True
                    for t in deps:
                        if t[0] == "c":
                            if t[2] not in rank[t[1]] or cs[t[1]] < rank[t[1]][t[2]]:
                                ok = False; break
                        elif ds[t[1]] < t[2]:
                            ok = False; break
                    if not ok:
                        break
                    if di is not None:
                        ds[di] += 16
                    elif pc[e] in rank[e]:
                        cs[e] += 1
                    pc[e] += 1; prog = True
            if not prog:
                break
        stuck = {e: (pc[e], len(self.ops[e]), self.ops[e][pc[e]][1]) for e in ENG if pc[e] < len(self.ops[e])}
        assert not stuck, "DEADLOCK %r" % (stuck,)
        print("sync check ok:", {e: len(self.ops[e]) for e in ENG}, {e: len(self.need[e]) for e in ENG})

    def emit(self, block):
        for e in ENG:
            pass
        self.limit = 10 ** 9
        self.barrier()
        self.op("sp", None)
        print("total ops", self.nops)
        self.check()
        rank = {e: {idx: i + 1 for i, idx in enumerate(sorted(self.need[e]))} for e in ENG}

        def run(e, h):
            waited = {}
            for idx, (fn, deps, di) in enumerate(self.ops[e]):
                for t in deps:
                    if t[0] == "c":
                        sem = self.psem[t[1]]; val = rank[t[1]][t[2]]
                    else:
                        sem = self.dsem[t[1]]; val = t[2]
                    key = (t[0], t[1])
                    if waited.get(key, 0) >= val:
                        continue
                    waited[key] = val
                    h.wait_ge(sem, val)
                if fn is None:
                    continue
                ins = fn(h)
                if di is not None:
                    ins.then_inc(self.dsem[di], 16)
                elif idx in rank[e]:
                    ins.then_inc(self.psem[e], 1)

        @block.tensor
        def _(h): run("pe", h)

        @block.scalar
        def _(h): run("act", h)

        @block.vector
        def _(h): run("dve", h)

        @block.gpsimd
        def _(h): run("pool", h)

        @block.sync
        def _(h): run("sp", h)


def build_program():
    nc = bass.Bass("TRN2", target_bir_lowering=False)
    es = ExitStack()
    P = Prog(nc, es)

    def din(name, shape, dt=F32):
        return nc.dram_tensor(name, list(shape), dt, kind="ExternalInput").ap()

    def dout(name, shape, dt=F32):
        return nc.dram_tensor(name, list(shape), dt, kind="ExternalOutput").ap()

    def dscr(name, shape, dt):
        return nc.dram_tensor(name, list(shape), dt).ap()

    x_all = din("x_all", [8192, 1024]); x_own = din("x_own", [4096, 1024]); x_s = din("x_s", [128, 1024])
    clat = din("clat", [2, 4096, 256]); ckr = din("ckr", [2, 4096, 32])
    cdk = din("cdk", [2, 4096, 512]); cdv = din("cdv", [2, 4096, 512])
    c3 = din("c3", [3, 1024])
    w_ada = din("w_ada", [1024, 6144]); b_ada = din("b_ada", [1, 6144])
    norm_attn = din("norm_attn", [1, 1024]); w_in = din("w_in", [1024, 2080])
    qg = din("qg", [1, 256]); kvg = din("kvg", [1, 256])
    w_uq = din("w_uq", [256, 768]); w_ukv = din("w_ukv", [256, 1024])
    lam4 = din("lam4", [4, 64]); subln = din("subln", [128, 1]); relb = din("relb", [32, 8])
    w_out = din("w_out", [1024, 1024]); norm_ffn = din("norm_ffn", [1, 1024])
    w_router = din("w_router", [1024, 256]); rbias = din("rbias", [1, 256])
    if STAGE > 1:
        w_gu = din("w_gu", [256, 1024, 512]); w_dn = din("w_dn", [256, 256, 1024])
    w_sgu = din("w_sgu", [1024, 512]); w_sdn = din("w_sdn", [256, 1024]); fnorm = din("fnorm", [1, 1024])
    cst = din("cst", [128, 1600]); cst2 = din("cst2", [128, 520]); rope_all = din("rope_all", [8192, 64]); rope_own = din("rope_own", [4096, 64])
    rope_s = din("rope_s", [128, 64])

    o_y = dout("o_y", [4096, 1024]); o_ys = dout("o_ys", [128, 1024])
    o_lat = dout("o_lat", [8192, 256]); o_kpe = dout("o_kpe", [8192, 32])
    o_dk = dout("o_dk", [8192, 512]); o_dv = dout("o_dv", [8192, 512])
    o_lats = dout("o_lats", [128, 256]); o_kpes = dout("o_kpes", [128, 32])
    o_dks = dout("o_dks", [128, 512]); o_dvs = dout("o_dvs", [128, 512])

    modD = dscr("modD", [3, 6144], F32); gD = dscr("gD", [8, 6, 192], F32)
    KTm = dscr("KTm", [8, 96, 8192], BF16); Vm = dscr("Vm", [8, 128, 64, 65], BF16)
    KTd = dscr("KTd", [4, 128, 8192], BF16); Vd = dscr("Vd", [4, 128, 64, 128], BF16)
    QTm = dscr("QTm", [8, 96, 4096], BF16); QTd = dscr("QTd", [4, 2, 128, 4096], BF16)
    sKTm = dscr("sKTm", [2, 8, 96, 4096], BF16); sVm = dscr("sVm", [2, 8, 128, 32, 65], BF16)
    sKTd = dscr("sKTd", [2, 4, 128, 4096], BF16); sVd = dscr("sVd", [2, 4, 128, 32, 128], BF16)
    OT = dscr("OT", [1024, 4224], BF16)
    X1 = dscr("X1", [4224, 1024], F32)
    if STAGE > 1:
        XGs = [dscr("XGa", [HALF, 1024], BF16), dscr("XGb", [HALF, 1024], BF16)]
        YGs = [dscr("YGa", [HALF, 1024], BF16), dscr("YGb", [HALF, 1024], BF16)]

    def sb(name, shape, dt):
        return es.enter_context(nc.sbuf_tensor(name, list(shape), dt))

    regs = {}

    def getbc(e):
        if "bc" not in regs:
            regs["bc"] = e.to_reg(HALF - 1)
        return regs["bc"]

    def finish():
        with nc.Block() as block:
            P.emit(block)
        return nc, es

    CF = sb("CF", [128, 1600], F32); CFb = Buf()
    C2 = sb("C2", [128, 520], F32)
    identb = sb("identb", [128, 128], BF16); Ub = sb("Ub", [128, 128], BF16)
    onesb = sb("onesb", [128, 128], BF16); onesf = sb("onesf", [128, 128], F32)
    mpib = sb("mpib", [128, 64], BF16); cB = Buf()
    identf = CF[:, 0:128]; Jf = CF[:, 128:256]
    M = [sb("M%d" % i, [128, 1024], F32) for i in range(6)]; MB = [Buf() for _ in range(6)]
    gains = sb("gains", [128, 512], F32); gB = Buf()
    Bb = sb("Bb", [128, 8, 384], F32); BbB = Buf()
    lamt = sb("lamt", [128, 8], F32); lamB = Buf()
    nkT = sb("nkT", [128, 8, 128], BF16); ndkT = sb("ndkT", [128, 4, 128], BF16)
    nv = sb("nv", [128, 8, 65], BF16); ndv = sb("ndv", [128, 4, 128], BF16)
    nqT = sb("nqT", [128, 8, 128], BF16); ndqT = sb("ndqT", [128, 2, 4, 128], BF16); nB = Buf()
    didx = sb("didx", [128, 33, 32], I32); gate8 = sb("gate8", [128, 33, 16], F32); dgB = [Buf() for _ in range(33)]
    cntbc = sb("cntbc", [128, 256], F32); cntB = Buf()
    nhalf = sb("nhalf", [128, 8], F32)
    ARN = 35584
    arena = sb("arena", [128, ARN], F32)
    ps = [es.enter_context(nc.psum_tensor("psb%d" % i, [128, 512], F32)) for i in range(8)]
    psB = [Buf(excl=True) for _ in range(8)]
    psn = [0]

    def nextps():
        i = psn[0]; psn[0] = (i + 1) % 8
        return i

    class Arena:
        def __init__(self): self.off = 0

        def reset(self):
            self.off = 0; P.barrier()

        def f32(self, shape):
            n = int(np.prod(shape[1:])); a = arena[:, self.off:self.off + n]; self.off += n
            assert self.off <= ARN, self.off
            if len(shape) == 3:
                a = a.rearrange("p (a b) -> p a b", a=shape[1])
            return a

        def bf(self, shape):
            n = int(np.prod(shape[1:])); nf = (n + 1) // 2
            a = arena[:, self.off:self.off + nf].bitcast(BF16)[:, 0:n]; self.off += nf
            assert self.off <= ARN, self.off
            if len(shape) == 3:
                a = a.rearrange("p (a b) -> p a b", a=shape[1])
            elif len(shape) == 4:
                a = a.rearrange("p (a b c) -> p a b c", a=shape[1], b=shape[2])
            return a

        def i32(self, shape):
            n = int(np.prod(shape[1:])); a = arena[:, self.off:self.off + n].bitcast(I32); self.off += n
            return a

    AR = Arena()

    def rms_ops(src_ap, srcB, D, junk, ssb, tag):
        ss, ssB = ssb
        P.op("pool", lambda e: e.memset(ss, 0.0), writes=[ssB])
        P.op("act", lambda e: e.activation(out=junk, in_=src_ap, func=AF.Square, accum_out=ss), reads=[srcB], writes=[ssB])
        P.op("dve", lambda e: e.tensor_scalar(out=ss, in0=ss, scalar1=1.0 / D, scalar2=EPS, op0=ALU.mult, op1=ALU.add), reads=[ssB], writes=[ssB])
        P.op("pool", lambda e: e.tensor_tensor(out=ss, in0=ss, in1=nhalf[:, 0:1], op=ALU.pow), reads=[ssB], writes=[ssB])

    P.dma("sp", lambda e: e.dma_start(out=CF[:], in_=cst), writes=[CFb])
    P.dma("sp", lambda e: e.dma_start(out=C2[:], in_=cst2), writes=[cB])
    P.op("dve", lambda e: e.tensor_copy(out=identb[:], in_=CF[:, 0:128]), reads=[CFb], writes=[cB])
    P.op("dve", lambda e: e.tensor_copy(out=Ub[:], in_=CF[:, 1472:1600]), reads=[CFb], writes=[cB])
    P.op("dve", lambda e: e.tensor_copy(out=mpib[:], in_=CF[:, 1408:1472]), reads=[CFb], writes=[cB])
    P.op("pool", lambda e: e.memset(onesb[:], 1.0), writes=[cB])
    P.op("pool", lambda e: e.memset(onesf[:], 1.0), writes=[cB])
    P.op("pool", lambda e: e.memset(nv[:], 1.0), writes=[nB])
    P.op("pool", lambda e: e.memset(ndqT[:], 0.0), writes=[nB])
    P.op("pool", lambda e: e.memset(cntbc[:], 0.0), writes=[cntB])
    P.op("pool", lambda e: e.memset(nhalf[:], -0.5), writes=[cB])
    P.dma("sp", lambda e: e.dma_start(out=gains[:, 0:256], in_=qg.broadcast_to([128, 256])), writes=[gB])
    P.dma("sp", lambda e: e.dma_start(out=gains[:, 256:512], in_=kvg.broadcast_to([128, 256])), writes=[gB])
    cT = AR.f32([128, 8, 3]); cTB = Buf()
    modS = AR.f32([128, 6144]); modB = Buf()
    badd = AR.f32([128, 6144]); baB = Buf()
    wad = [AR.f32([128, 8, 512]) for _ in range(2)]; wadB = [Buf(), Buf()]
    for k in range(8):
        P.dma("sp", lambda e, k=k: e.dma_start(out=cT[:, k, :], in_=c3[:, k * 128:(k + 1) * 128].rearrange("c p -> p c"),
                                             allow_slow_non_contiguous=True), writes=[cTB])
    P.op("act", lambda e: e.activation(out=cT, in_=cT, func=AF.Silu), reads=[cTB], writes=[cTB])
    P.dma("sp", lambda e: e.dma_start(out=badd[0:3, :], in_=b_ada.broadcast_to([3, 6144])), writes=[baB])
    for n in range(12):
        wb = wad[n % 2]; wB = wadB[n % 2]
        P.dma("sp", lambda e, wb=wb, n=n: e.dma_start(out=wb, in_=w_ada[:, n * 512:(n + 1) * 512].rearrange("(k p) n -> p k n", p=128)), writes=[wB])
        pi = nextps()

        def f(e, wb=wb, pi=pi):
            for k in range(8):
                ins = e.matmul(ps[pi][0:3, :], lhsT=cT[:, k, :], rhs=wb[:, k, :], start=(k == 0), stop=(k == 7))
            return ins
        P.op("pe", f, reads=[cTB, wB], writes=[psB[pi]])
        P.op("dve", lambda e, pi=pi, n=n: e.tensor_tensor(out=modS[0:3, n * 512:(n + 1) * 512], in0=ps[pi][0:3, :],
                                                         in1=badd[0:3, n * 512:(n + 1) * 512], op=ALU.add),
             reads=[psB[pi], baB], writes=[modB])
    P.dma("sp", lambda e: e.dma_start(out=modD, in_=modS[0:3, :]), reads=[modB])
    P.barrier()
    if KCUT == 0:
        return finish()

    def load_mod(sample, which):
        src = {0: 1, 1: 0, 2: 2, 3: 4, 4: 3, 5: 5}
        for j in which:
            c0 = src[j] * 1024
            if not sample:
                P.dma("sp", lambda e, j=j, c0=c0: e.dma_start(out=M[j][:], in_=modD[0:1, c0:c0 + 1024].broadcast_to([128, 1024])), writes=[MB[j]])
            else:
                P.dma("sp", lambda e, j=j, c0=c0: e.dma_start(out=M[j][0:32, :], in_=modD[1:2, c0:c0 + 1024].broadcast_to([32, 1024])), writes=[MB[j]])
                P.dma("sp", lambda e, j=j, c0=c0: e.dma_start(out=M[j][32:128, :], in_=modD[2:3, c0:c0 + 1024].broadcast_to([96, 1024])), writes=[MB[j]])
            if j in (0, 3):
                gsrc = norm_attn if j == 0 else norm_ffn
                tmp = AR_tmp[:]
                P.dma("sp", lambda e, gsrc=gsrc: e.dma_start(out=tmp, in_=gsrc.broadcast_to([128, 1024])), writes=[tmpB])
                P.op("dve", lambda e, j=j: e.scalar_tensor_tensor(out=M[j][:], in0=M[j][:], scalar=1.0, in1=tmp, op0=ALU.add, op1=ALU.mult),
                     reads=[tmpB, MB[j]], writes=[MB[j]])

    AR_tmp = sb("modtmp", [128, 1024], F32); tmpB = Buf()
    load_mod(False, range(6))

    lv = AR.f32([128, 4, 64]); lvB = Buf()
    P.dma("sp", lambda e: e.dma_start(out=lv, in_=bass.AP(lam4.tensor, 0, [[0, 128], [64, 4], [1, 64]])), writes=[lvB])
    lp = AR.f32([128, 2, 64]); l2 = AR.f32([128, 4]); l2B = Buf()
    P.op("dve", lambda e: e.tensor_tensor(out=lp[:, 0, :], in0=lv[:, 0, :], in1=lv[:, 1, :], op=ALU.mult), reads=[lvB], writes=[l2B])
    P.op("dve", lambda e: e.tensor_tensor(out=lp[:, 1, :], in0=lv[:, 2, :], in1=lv[:, 3, :], op=ALU.mult), reads=[lvB, l2B], writes=[l2B])
    P.op("dve", lambda e: e.tensor_reduce(out=l2[:, 0:2], in_=lp, axis=AX.X, op=ALU.add), reads=[l2B], writes=[l2B])
    P.op("act", lambda e: e.activation(out=l2[:, 2:4], in_=l2[:, 0:2], func=AF.Exp), reads=[l2B], writes=[l2B])
    P.op("dve", lambda e: e.tensor_tensor(out=lamt[:, 0:1], in0=l2[:, 3:4], in1=l2[:, 2:3], op=ALU.subtract), reads=[l2B], writes=[lamB])
    P.op("dve", lambda e: e.tensor_scalar(out=lamt[:, 0:1], in0=lamt[:, 0:1], scalar1=-LAM_INIT, scalar2=None, op0=ALU.add), reads=[lamB], writes=[lamB])
    P.dma("sp", lambda e: e.dma_start(out=lamt[:, 1:2], in_=subln), writes=[lamB])
    P.op("dve", lambda e: e.tensor_scalar(out=lamt[:, 1:2], in0=lamt[:, 1:2], scalar1=(1.0 - LAM_INIT) * math.sqrt(128.0), scalar2=None, op0=ALU.mult),
         reads=[lamB], writes=[lamB])
    rb = AR.f32([128, 8]); rbB = Buf()
    P.dma("sp", lambda e: e.dma_start(out=rb[0:32, :], in_=relb), writes=[rbB])
    gS = AR.f32([128, 1152]); gSB = Buf()
    for half in range(3):
        pi = nextps()
        P.op("pe", lambda e, pi=pi, half=half: e.matmul(ps[pi][0:8, 0:384], lhsT=rb[0:32, :], rhs=CF[0:32, 256 + half * 384:256 + (half + 1) * 384], start=True, stop=True),
             reads=[rbB, CFb], writes=[psB[pi]])
        P.op("dve", lambda e, pi=pi, half=half: e.tensor_copy(out=gS[0:8, half * 384:(half + 1) * 384], in_=ps[pi][0:8, 0:384]), reads=[psB[pi]], writes=[gSB])
    tgd = P.dma("sp", lambda e: e.dma_start(out=gD.rearrange("a b c -> a (b c)"), in_=gS[0:8, :]), reads=[gSB])
    Tp = AR.f32([128, 8, 384]); TpB = Buf()
    for blk in range(6):
        P.dma("sp", lambda e, blk=blk: e.dma_start(out=Tp[:, :, blk * 64:(blk + 1) * 64],
                                                  in_=bass.AP(gD.tensor, blk * 192, [[1, 128], [1152, 8], [1, 64]])), writes=[TpB], waits=[tgd])
    for hm in range(8):
        pi = nextps()
        P.op("pe", lambda e, pi=pi, hm=hm: e.matmul(ps[pi][:, 0:384], lhsT=Jf, rhs=Tp[:, hm, :], start=True, stop=True), reads=[TpB, CFb], writes=[psB[pi]])
        P.op("dve", lambda e, pi=pi, hm=hm: e.tensor_copy(out=Bb[:, hm, :], in_=ps[pi][:, 0:384]), reads=[psB[pi]], writes=[BbB])

    if KCUT == 1:
        return finish()
    AR.reset()
    winb = AR.bf([128, 8, 2080]); wuqb = AR.bf([128, 2, 768]); wukvb = AR.bf([128, 2, 1024]); wB_ = Buf()
    for k in range(8):
        P.dma("pool", lambda e, k=k: e.dma_start(out=winb[:, k, :], in_=w_in[k * 128:(k + 1) * 128, :]), writes=[wB_])
    P.dma("pool", lambda e: e.dma_start(out=wuqb, in_=w_uq.rearrange("(k p) n -> p k n", p=128)), writes=[wB_])
    P.dma("pool", lambda e: e.dma_start(out=wukvb, in_=w_ukv.rearrange("(k p) n -> p k n", p=128)), writes=[wB_])
    NR = 2
    xt = [AR.f32([128, 1024]) for _ in range(NR)]; xtB = [Buf() for _ in range(NR)]
    rp = [AR.f32([128, 64]) for _ in range(NR)]; rpB = [Buf() for _ in range(NR)]
    hf = AR.f32([128, 1024]); hfB = Buf()
    hb = AR.bf([128, 1024]); hbB = Buf()
    junk = AR.bf([128, 1024]); junkB = Buf()
    hT = [AR.bf([128, 8, 128]) for _ in range(NR)]; hTB = [Buf() for _ in range(NR)]
    ssA = AR.f32([128, 8]); ssB_ = [Buf() for _ in range(8)]
    latf = [AR.f32([128, 256]) for _ in range(NR)]; latfB = [Buf() for _ in range(NR)]
    kpef = [AR.f32([128, 32]) for _ in range(NR)]; kpefB = [Buf() for _ in range(NR)]
    rtmp = AR.f32([128, 8, 32]); rtmpB = Buf(); rtmp2 = AR.f32([128, 8, 32])
    dkf = [AR.f32([128, 512]) for _ in range(NR)]; dkfB = [Buf() for _ in range(NR)]
    dvf = [AR.f32([128, 512]) for _ in range(NR)]; dvfB = [Buf() for _ in range(NR)]
    latb = AR.bf([128, 256]); latbB = Buf(); kpeb = AR.bf([128, 32]); kpebB = Buf()
    dkb = AR.bf([128, 512]); dkbB = Buf()
    latT = AR.bf([128, 2, 128]); latTB = Buf()
    kcomb = AR.bf([128, 8, 96]); kcombB = Buf()
    kTst = AR.bf([128, 8, 512]); kTstB = Buf(); dkTst = AR.bf([128, 4, 512]); dkTstB = Buf()
    vst = AR.bf([128, 8, 4, 65]); vstB = Buf(); dvst = AR.bf([128, 4, 4, 128]); dvstB = Buf()
    qnb = AR.bf([128, 256]); qnbB = Buf(); qnT = AR.bf([128, 2, 128]); qnTB = Buf()
    qc = AR.bf([128, 8, 96]); qcB = Buf(); dqb = AR.bf([128, 512]); dqbB = Buf()
    qTst = AR.bf([128, 8, 512]); qTstB = Buf(); dqTst = AR.bf([128, 2, 4, 512]); dqTstB = Buf()
    P.op("pool", lambda e: e.memset(vst, 1.0), writes=[vstB])
    P.op("pool", lambda e: e.memset(dqTst, 0.0), writes=[dqTstB])

    def transposes(src_fn, n, rows, dstB_reads, dst_ap, dstB, eng="dve"):
        pi = nextps(); pb = ps[pi][:].bitcast(BF16)

        def f(e):
            for k in range(n):
                ins = e.transpose(out=pb[0:rows, k * 128:(k + 1) * 128], in_=src_fn(k), identity=identb[:])
            return ins
        P.op("pe", f, reads=dstB_reads + [cB], writes=[psB[pi]])
        src = pb[0:rows, 0:n * 128].rearrange("p (a b) -> p a b", a=n)
        if eng == "act":
            P.op("act", lambda e: e.activation(out=dst_ap, in_=src, func=AF.Copy), reads=[psB[pi]], writes=[dstB])
        else:
            P.op(eng, lambda e: e.tensor_copy(out=dst_ap, in_=src), reads=[psB[pi]], writes=[dstB])

    def norm_h(xa, xB, Aj, Bj):
        rms_ops(xa, xB, 1024, junk, (ssA[:, 0:1], ssB_[0]), "x")
        P.op("dve", lambda e: e.scalar_tensor_tensor(out=hf, in0=xa, scalar=ssA[:, 0:1], in1=M[Aj][:], op0=ALU.mult, op1=ALU.mult),
             reads=[xB, ssB_[0], MB[Aj]], writes=[hfB])
        P.op("pool", lambda e: e.tensor_tensor(out=hb, in0=hf, in1=M[Bj][:], op=ALU.add), reads=[hfB, MB[Bj]], writes=[hbB])

    def rope(eng, dst, srcv, tab, nh, reads, writes):
        cs = tab[:, 0:32].unsqueeze(1).broadcast_to([128, nh, 32])
        s1 = tab[:, 32:48].unsqueeze(1).broadcast_to([128, nh, 16])
        s2 = tab[:, 48:64].unsqueeze(1).broadcast_to([128, nh, 16])
        t1 = rtmp[:, 0:nh, :]; t2 = rtmp2[:, 0:nh, :]
        P.op(eng, lambda e: e.tensor_tensor(out=t1, in0=srcv, in1=cs, op=ALU.mult), reads=reads, writes=[rtmpB])
        P.op(eng, lambda e: e.tensor_tensor(out=t2[:, :, 0:16], in0=srcv[:, :, 16:32], in1=s1, op=ALU.mult), reads=reads + [rtmpB], writes=[rtmpB])
        P.op(eng, lambda e: e.tensor_tensor(out=t2[:, :, 16:32], in0=srcv[:, :, 0:16], in1=s2, op=ALU.mult), reads=reads + [rtmpB], writes=[rtmpB])
        P.op(eng, lambda e: e.tensor_tensor(out=dst, in0=t1, in1=t2, op=ALU.add), reads=[rtmpB], writes=writes)

    def kside_derive(t4, dests, latb_, kpeb_, dkb_, dvb_src, rB):
        kT_dst, dkT_dst, v_dst, dv_dst = dests
        transposes(lambda k: latb_[:, k * 128:(k + 1) * 128], 2, 128, rB, latT, latTB)
        pa = nextps(); pb_ = nextps()

        def f(e):
            for hh, pi in ((0, pa), (1, pb_)):
                for k in range(2):
                    ins = e.matmul(ps[pi][:], lhsT=latT[:, k, :], rhs=wukvb[:, k, hh * 512:(hh + 1) * 512], start=(k == 0), stop=(k == 1))
            return ins
        P.op("pe", f, reads=[latTB, wB_], writes=[psB[pa], psB[pb_]])
        for hh, pi in ((0, pa), (1, pb_)):
            v4 = ps[pi][:].rearrange("p (h c) -> p h c", h=4)
            P.op("dve", lambda e, v4=v4, hh=hh: e.tensor_copy(out=kcomb[:, hh * 4:hh * 4 + 4, 0:64], in_=v4[:, :, 0:64]), reads=[psB[pi]], writes=[kcombB])
            P.op("act", lambda e, v4=v4, hh=hh: e.activation(out=v_dst[:, hh * 4:hh * 4 + 4, t4, 1:65] if v_dst is not None else nv[:, hh * 4:hh * 4 + 4, 1:65],
                                                            in_=v4[:, :, 64:128], func=AF.Copy), reads=[psB[pi]], writes=[vstB])
        P.op("pool", lambda e: e.tensor_copy(out=kcomb[:, :, 64:96], in_=kpeb_.unsqueeze(1).broadcast_to([128, 8, 32])), reads=rB + [kcombB], writes=[kcombB])
        transposes(lambda h: kcomb[:, h, :], 8, 96, [kcombB], kT_dst, kTstB, eng="act")
        transposes(lambda h: dkb_[:, h * 128:(h + 1) * 128], 4, 128, rB, dkT_dst, dkTstB)
        if dvb_src is not None:
            P.op("pool", lambda e: e.tensor_copy(out=dv_dst, in_=dvb_src.rearrange("p (h c) -> p h c", h=4)), reads=rB, writes=[dvstB])

    def flush_k(sidx, q4):
        c0 = q4 * 512
        if sidx is None:
            kd, dkd, vd, dvd = KTm, KTd, Vm, Vd
        else:
            kd, dkd, vd, dvd = sKTm[sidx], sKTd[sidx], sVm[sidx], sVd[sidx]
        P.dma("sp", lambda e: e.dma_start(out=kd[:, :, c0:c0 + 512].rearrange("h d t -> d h t"), in_=kTst[0:96, :, :]), reads=[kTstB])
        P.dma("sp", lambda e: e.dma_start(out=dkd[:, :, c0:c0 + 512].rearrange("h d t -> d h t"), in_=dkTst), reads=[dkTstB])
        P.dma("sp", lambda e: e.dma_start(out=vd[:, :, q4 * 4:q4 * 4 + 4, :].rearrange("h p t c -> p h t c"), in_=vst), reads=[vstB])
        P.dma("sp", lambda e: e.dma_start(out=dvd[:, :, q4 * 4:q4 * 4 + 4, :].rearrange("h p t c -> p h t c"), in_=dvst), reads=[dvstB])

    def proj_tile(xsrc, ropesrc, r, own_cols, kv_cols, outs, t4, sample=False):
        P.dma("sp", lambda e: e.dma_start(out=xt[r], in_=xsrc), writes=[xtB[r]])
        P.dma("sp", lambda e: e.dma_start(out=rp[r], in_=ropesrc), writes=[rpB[r]])
        norm_h(xt[r], xtB[r], 0, 1)
        transposes(lambda k: hb[:, k * 128:(k + 1) * 128], 8, 128, [hbB], hT[r], hTB[r], eng="act")

        def inproj(c0, c1):
            pi = nextps()

            def f(e):
                for k in range(8):
                    ins = e.matmul(ps[pi][:, 0:c1 - c0], lhsT=hT[r][:, k, :], rhs=winb[:, k, c0:c1], start=(k == 0), stop=(k == 7))
                return ins
            P.op("pe", f, reads=[hTB[r], wB_], writes=[psB[pi]])
            return pi
        if kv_cols:
            o_lat_, o_kpe_, o_dk_, o_dv_ = outs
            pi = inproj(256, 544)
            rms_ops(ps[pi][:, 0:256], psB[pi], 256, junk[:, 0:256], (ssA[:, 1:2], ssB_[1]), "kv")
            P.op("dve", lambda e: e.scalar_tensor_tensor(out=latf[r], in0=ps[pi][:, 0:256], scalar=ssA[:, 1:2], in1=gains[:, 256:512], op0=ALU.mult, op1=ALU.mult),
                 reads=[psB[pi], ssB_[1], gB], writes=[latfB[r]])
            P.op("pool", lambda e: e.tensor_copy(out=latb, in_=latf[r]), reads=[latfB[r]], writes=[latbB])
            rope("dve", kpef[r].unsqueeze(1), ps[pi][:, 256:288].unsqueeze(1), rp[r], 1, [psB[pi], rpB[r]], [kpefB[r]])
            P.op("pool", lambda e: e.tensor_copy(out=kpeb, in_=kpef[r]), reads=[kpefB[r]], writes=[kpebB])
            P.dma("sp", lambda e: e.dma_start(out=o_lat_, in_=latf[r]), reads=[latfB[r]])
            P.dma("sp", lambda e: e.dma_start(out=o_kpe_, in_=kpef[r]), reads=[kpefB[r]])
            pk = inproj(1056, 1568)
            P.op("act", lambda e: e.activation(out=dkf[r], in_=ps[pk][:], func=AF.Copy), reads=[psB[pk]], writes=[dkfB[r]])
            P.op("dve", lambda e: e.tensor_copy(out=dkb, in_=ps[pk][:]), reads=[psB[pk]], writes=[dkbB])
            P.dma("sp", lambda e: e.dma_start(out=o_dk_, in_=dkf[r]), reads=[dkfB[r]])
            pv = inproj(1568, 2080)
            P.op("act", lambda e: e.activation(out=dvf[r], in_=ps[pv][:], func=AF.Copy), reads=[psB[pv]], writes=[dvfB[r]])
            dvdst = ndv[:] if sample else dvst[:, :, t4, :]
            P.op("dve", lambda e: e.tensor_copy(out=dvdst, in_=ps[pv][:].rearrange("p (h c) -> p h c", h=4)), reads=[psB[pv]], writes=[dvstB])
            P.dma("sp", lambda e: e.dma_start(out=o_dv_, in_=dvf[r]), reads=[dvfB[r]])
            if sample:
                dests = (nkT[0:96, :, :], ndkT[:], None, None)
            else:
                dests = (kTst[0:96, :, t4 * 128:(t4 + 1) * 128], dkTst[:, :, t4 * 128:(t4 + 1) * 128], vst, None)
            kside_derive(t4, dests, latb, kpeb, dkb, None, [latbB, kpebB, dkbB])
        if own_cols:
            pi = inproj(0, 256)
            rms_ops(ps[pi][:, 0:256], psB[pi], 256, junk[:, 0:256], (ssA[:, 2:3], ssB_[2]), "q")
            P.op("dve", lambda e: e.scalar_tensor_tensor(out=qnb, in0=ps[pi][:, 0:256], scalar=ssA[:, 2:3], in1=gains[:, 0:256], op0=ALU.mult, op1=ALU.mult),
                 reads=[psB[pi], ssB_[2], gB], writes=[qnbB])
            pq = inproj(544, 1056)
            P.op("act", lambda e: e.activation(out=dqb, in_=ps[pq][:], func=AF.Copy), reads=[psB[pq]], writes=[dqbB])
            transposes(lambda k: qnb[:, k * 128:(k + 1) * 128], 2, 128, [qnbB], qnT, qnTB)
            pa = nextps(); pb_ = nextps()

            def f(e):
                for hh, pj in ((0, pa), (1, pb_)):
                    for k in range(2):
                        ins = e.matmul(ps[pj][:, 0:384], lhsT=qnT[:, k, :], rhs=wuqb[:, k, hh * 384:(hh + 1) * 384], start=(k == 0), stop=(k == 1))
                return ins
            P.op("pe", f, reads=[qnTB, wB_], writes=[psB[pa], psB[pb_]])
            for hh, pj in ((0, pa), (1, pb_)):
                v4 = ps[pj][:, 0:384].rearrange("p (h c) -> p h c", h=4)
                P.op("act", lambda e, v4=v4, hh=hh: e.activation(out=qc[:, hh * 4:hh * 4 + 4, 0:64], in_=v4[:, :, 0:64], func=AF.Copy), reads=[psB[pj]], writes=[qcB])
                rope("dve", qc[:, hh * 4:hh * 4 + 4, 64:96], v4[:, :, 64:96], rp[r], 4, [psB[pj], rpB[r]], [qcB])
            if sample:
                qd = nqT[0:96, :, :]
                dq0, dq1 = ndqT[0:64, 0, :, :], ndqT[64:128, 1, :, :]
            else:
                qd = qTst[0:96, :, t4 * 128:(t4 + 1) * 128]
                dq0, dq1 = dqTst[0:64, 0, :, t4 * 128:(t4 + 1) * 128], dqTst[64:128, 1, :, t4 * 128:(t4 + 1) * 128]
            transposes(lambda h: qc[:, h, :], 8, 96, [qcB], qd, qTstB, eng="act")
            pq2 = nextps(); pbq = ps[pq2][:].bitcast(BF16)

            def ftq(e):
                for k in range(4):
                    ins = e.transpose(out=pbq[:, k * 128:(k + 1) * 128], in_=dqb[:, k * 128:(k + 1) * 128], identity=identb[:])
                return ins
            P.op("pe", ftq, reads=[dqbB, cB], writes=[psB[pq2]])
            P.op("dve", lambda e: e.tensor_copy(out=dq0, in_=pbq[0:64, 0:512].rearrange("p (a b) -> p a b", a=4)), reads=[psB[pq2]], writes=[dqTstB])
            P.op("dve", lambda e: e.tensor_copy(out=dq1, in_=pbq[64:128, 0:512].rearrange("p (a b) -> p a b", a=4)), reads=[psB[pq2], dqTstB], writes=[dqTstB])

    for t in range(64):
        if KCUT == 2 and t == 4:
            return finish()
        sl = slice(t * 128, (t + 1) * 128)
        proj_tile(x_all[sl, :], rope_all[sl, :], t % NR, False, True, (o_lat[sl, :], o_kpe[sl, :], o_dk[sl, :], o_dv[sl, :]), t % 4)
        if t % 4 == 3:
            flush_k(None, t // 4)
    if KCUT == 3:
        return finish()
    for t in range(32):
        sl = slice(t * 128, (t + 1) * 128)
        proj_tile(x_own[sl, :], rope_own[sl, :], t % NR, True, False, None, t % 4)
        if t % 4 == 3:
            c0 = (t // 4) * 512
            P.dma("sp", lambda e, c0=c0: e.dma_start(out=QTm[:, :, c0:c0 + 512].rearrange("h d t -> d h t"), in_=qTst[0:96, :, :]), reads=[qTstB])
            for m_ in range(2):
                P.dma("sp", lambda e, c0=c0, m_=m_: e.dma_start(out=QTd[:, m_, :, c0:c0 + 512].rearrange("h d t -> d h t"), in_=dqTst[:, m_, :, :]), reads=[dqTstB])
    if KCUT == 4:
        return finish()
    clb = [AR.bf([128, 256]) for _ in range(2)]; ckb = [AR.bf([128, 32]) for _ in range(2)]
    cdkb = [AR.bf([128, 512]) for _ in range(2)]; cdvb = [AR.bf([128, 512]) for _ in range(2)]
    ccB = [Buf(), Buf()]
    for s in range(2):
        for t in range(32):
            r = t % 2; sl = slice(t * 128, (t + 1) * 128)
            P.dma("pool", lambda e, r=r, s=s, sl=sl: e.dma_start(out=clb[r], in_=clat[s, sl, :]), writes=[ccB[r]])
            P.dma("pool", lambda e, r=r, s=s, sl=sl: e.dma_start(out=ckb[r], in_=ckr[s, sl, :]), writes=[ccB[r]])
            P.dma("pool", lambda e, r=r, s=s, sl=sl: e.dma_start(out=cdkb[r], in_=cdk[s, sl, :]), writes=[ccB[r]])
            P.dma("pool", lambda e, r=r, s=s, sl=sl: e.dma_start(out=cdvb[r], in_=cdv[s, sl, :]), writes=[ccB[r]])
            t4 = t % 4
            dests = (kTst[0:96, :, t4 * 128:(t4 + 1) * 128], dkTst[:, :, t4 * 128:(t4 + 1) * 128], vst, dvst[:, :, t4, :])
            kside_derive(t4, dests, clb[r], ckb[r], cdkb[r], cdvb[r], [ccB[r]])
            if t4 == 3:
                flush_k(s, t // 4)
    if KCUT == 5:
        return finish()
    load_mod(True, (0, 1))
    proj_tile(x_s, rope_s, 0, True, True, (o_lats, o_kpes, o_dks, o_dvs), 0, sample=True)
    if STAGE <= 1:
        return finish()
    if KCUT == 6:
        return finish()
    AR.reset()
    ktb = [AR.bf([128, 8192]) for _ in range(2)]; ktB = [Buf(), Buf()]
    vb = [AR.bf([128, 64, 128]) for _ in range(2)]; vB = [Buf(), Buf()]
    qtb = [AR.bf([128, 2, 4096]) for _ in range(2)]; qtB = [Buf(), Buf()]
    NPT = 4
    pt = [AR.bf([128, 512]) for _ in range(NPT)]; ptB = [Buf() for _ in range(NPT)]
    sbs = [AR.f32([128, 128]) for _ in range(2)]; sbsB = [Buf(), Buf()]
    rl = AR.f32([128, 2, 512]); rlB = Buf()
    bcs = AR.f32([128, 512]); bcsB = Buf()
    o1 = AR.f32([128, 512]); o2 = AR.f32([128, 512]); oB = Buf()
    sq = AR.f32([128, 512]); sqB = Buf()
    otile = [AR.bf([128, 512]) for _ in range(2)]; otB = [Buf(), Buf()]
    zt = AR.bf([128, 1024]); ztB = Buf()
    rings = {"s": 0, "p": 0, "o": 0, "b": 0}

    def ring(name, n):
        i = rings[name]; rings[name] = (i + 1) % n
        return i

    P.op("pool", lambda e: e.memset(zt, 0.0), writes=[ztB])

    def zero_fill():
        for XG in XGs:
            for c in range(HALF // 2048):
                P.dma("sp", lambda e, c=c, XG=XG: e.dma_start(out=XG[c * 2048:(c + 1) * 2048, :].rearrange("(p r) c -> p r c", p=128),
                                                             in_=zt.unsqueeze(1).broadcast_to([128, 16, 1024])), reads=[ztB])

    def load_pass(kind, h, slot, s=None):
        if s is None:
            if kind == "m":
                P.dma("sp", lambda e: e.dma_start(out=ktb[slot][0:96, :], in_=KTm[h]), writes=[ktB[slot]])
                P.dma("sp", lambda e: e.dma_start(out=vb[slot][:, :, 0:65], in_=Vm[h]), writes=[vB[slot]])
                P.dma("sp", lambda e: e.dma_start(out=qtb[slot][0:96, 0, :], in_=QTm[h]), writes=[qtB[slot]])
            else:
                P.dma("sp", lambda e: e.dma_start(out=ktb[slot], in_=KTd[h]), writes=[ktB[slot]])
                P.dma("sp", lambda e: e.dma_start(out=vb[slot], in_=Vd[h]), writes=[vB[slot]])
                P.dma("sp", lambda e: e.dma_start(out=qtb[slot], in_=QTd[h].rearrange("m d t -> d m t")), writes=[qtB[slot]])
        else:
            if kind == "m":
                P.dma("sp", lambda e: e.dma_start(out=ktb[slot][0:96, 0:4096], in_=sKTm[s, h]), writes=[ktB[slot]])
                P.dma("sp", lambda e: e.dma_start(out=vb[slot][:, 0:32, 0:65], in_=sVm[s, h]), writes=[vB[slot]])
            else:
                P.dma("sp", lambda e: e.dma_start(out=ktb[slot][:, 0:4096], in_=sKTd[s, h]), writes=[ktB[slot]])
                P.dma("sp", lambda e: e.dma_start(out=vb[slot][:, 0:32, :], in_=sVd[s, h]), writes=[vB[slot]])

    def fin_mla(acc, h, col0, N):
        P.op("dve", lambda e: e.reciprocal(out=rl[0:1, 0, 0:N], in_=ps[acc][0:1, 0:N]), reads=[psB[acc]], writes=[rlB])
        P.op("pe", lambda e: e.matmul(ps[7][0:65, 0:N], lhsT=onesf[0:1, 0:65], rhs=rl[0:1, 0, 0:N], start=True, stop=True), reads=[rlB, cB], writes=[psB[7]])
        P.op("dve", lambda e: e.tensor_copy(out=bcs[0:65, 0:N], in_=ps[7][0:65, 0:N]), reads=[psB[7]], writes=[bcsB])
        k = ring("o", 2); ot = otile[k]
        P.op("dve", lambda e: e.tensor_tensor(out=ot[0:65, 0:N], in0=ps[acc][0:65, 0:N], in1=bcs[0:65, 0:N], op=ALU.mult), reads=[psB[acc], bcsB], writes=[otB[k]])
        P.dma("sp", lambda e: e.dma_start(out=OT[h * 64:(h + 1) * 64, col0:col0 + N], in_=ot[1:65, 0:N]), reads=[otB[k]], waits=(tz if col0 >= 4096 else ()))

    def fin_diff(h, col0, N):
        P.op("dve", lambda e: e.reciprocal(out=rl[:, 0, 0:N], in_=ps[5][:, 0:N]), reads=[psB[5]], writes=[rlB])
        P.op("dve", lambda e: e.reciprocal(out=rl[:, 1, 0:N], in_=ps[6][:, 0:N]), reads=[psB[6], rlB], writes=[rlB])
        P.op("dve", lambda e: e.tensor_tensor(out=o1[:, 0:N], in0=ps[3][:, 0:N], in1=rl[:, 0, 0:N], op=ALU.mult), reads=[psB[3], rlB], writes=[oB])
        P.op("dve", lambda e: e.tensor_tensor(out=o2[:, 0:N], in0=ps[4][:, 0:N], in1=rl[:, 1, 0:N], op=ALU.mult), reads=[psB[4], rlB, oB], writes=[oB])
        P.op("dve", lambda e: e.scalar_tensor_tensor(out=o1[:, 0:N], in0=o2[:, 0:N], scalar=lamt[:, 0:1], in1=o1[:, 0:N], op0=ALU.mult, op1=ALU.add),
             reads=[oB, lamB], writes=[oB])
        P.op("pool", lambda e: e.tensor_tensor(out=sq[:, 0:N], in0=o1[:, 0:N], in1=o1[:, 0:N], op=ALU.mult), reads=[oB], writes=[sqB])
        P.op("pe", lambda e: e.matmul(ps[7][:, 0:N], lhsT=onesf[:], rhs=sq[:, 0:N], start=True, stop=True), reads=[sqB, cB], writes=[psB[7]])
        P.op("dve", lambda e: e.tensor_scalar(out=bcs[:, 0:N], in0=ps[7][:, 0:N], scalar1=128.0 * EPS, scalar2=None, op0=ALU.add), reads=[psB[7]], writes=[bcsB])
        P.op("pool", lambda e: e.tensor_tensor(out=bcs[:, 0:N], in0=bcs[:, 0:N], in1=nhalf[:, 0:1].broadcast_to([128, N]), op=ALU.pow), reads=[bcsB], writes=[bcsB])
        k = ring("o", 2); ot = otile[k]
        P.op("dve", lambda e: e.scalar_tensor_tensor(out=ot[:, 0:N], in0=o1[:, 0:N], scalar=lamt[:, 1:2], in1=bcs[:, 0:N], op0=ALU.mult, op1=ALU.mult),
             reads=[oB, bcsB, lamB], writes=[otB[k]])
        P.dma("sp", lambda e: e.dma_start(out=OT[512 + h * 128:512 + (h + 1) * 128, col0:col0 + N], in_=ot[:, 0:N]), reads=[otB[k]], waits=(tz if col0 >= 4096 else ()))

    def attn_tiles(kind, h, tiles, q_fn, qB_, col0, N, accs):
        pend = []
        n = len(tiles)
        nm = 1 if kind == "m" else 2

        def pv(item):
            i, tl, pjs = item
            for m in range(nm):
                pj = pjs[m]; c0 = tl["c0"]; rows = tl["rows"]
                first = (i == 0); last = (i == n - 1)
                parts = [(0, rows, c0)]
                for (r0, r1, cc) in parts:
                    vv = tl["v_ap"]
                    if kind == "m":
                        P.op("pe", lambda e, r0=r0, r1=r1, cc=cc, vv=vv, pj=pj, first=first, last=last: e.matmul(
                            ps[accs[0]][0:65, cc:N], lhsT=vv[r0:r1, 0:65], rhs=pt[pj][r0:r1, cc:N], start=first and r0 == 0, stop=last and r1 == rows),
                            reads=[ptB[pj], tl["vB"]], writes=[psB[accs[0]]])
                    else:
                        def f(e, r0=r0, r1=r1, cc=cc, vv=vv, pj=pj, first=first, last=last, m=m):
                            e.matmul(ps[accs[m]][:, cc:N], lhsT=vv[r0:r1, :], rhs=pt[pj][r0:r1, cc:N], start=first and r0 == 0, stop=last and r1 == rows)
                            return e.matmul(ps[accs[2 + m]][:, cc:N], lhsT=onesb[r0:r1, :], rhs=pt[pj][r0:r1, cc:N], start=first and r0 == 0, stop=last and r1 == rows)
                        P.op("pe", f, reads=[ptB[pj], tl["vB"], cB], writes=[psB[accs[m]], psB[accs[2 + m]]])

        for i, tl in enumerate(tiles):
            c0 = tl["c0"]; rows = tl["rows"]; pjs = []
            for m in range(nm):
                sk = ring("s", 3); pj = ring("p", NPT); pjs.append(pj)
                kap = tl["k_ap"](m); qap = q_fn(m, c0)
                P.op("pe", lambda e, sk=sk, kap=kap, qap=qap, c0=c0, rows=rows: e.matmul(ps[sk][0:rows, c0:N], lhsT=kap, rhs=qap, start=True, stop=True),
                     reads=[tl["kB"], qB_], writes=[psB[sk]])
                if kind == "m":
                    P.op("act", lambda e, sk=sk, pj=pj, c0=c0, rows=rows: e.activation(out=pt[pj][0:rows, c0:N], in_=ps[sk][0:rows, c0:N], func=AF.Exp, scale=MLA_SCALE),
                         reads=[psB[sk]], writes=[ptB[pj]])
                else:
                    hm = h * 2 + m; b15 = Bb[0:rows, hm, 128:129]
                    if tl["bias"] is not None:
                        lo, w = tl["bias"]; w = min(w, N - c0)
                        bi = ring("b", 2)
                        P.op("dve", lambda e, sk=sk, bi=bi, c0=c0, w=w, lo=lo, hm=hm, rows=rows: e.scalar_tensor_tensor(
                            out=sbs[bi][0:rows, 0:w], in0=ps[sk][0:rows, c0:c0 + w], scalar=DIFF_SCALE, in1=Bb[0:rows, hm, lo:lo + w], op0=ALU.mult, op1=ALU.add),
                            reads=[psB[sk], BbB], writes=[sbsB[bi]])
                        P.op("act", lambda e, bi=bi, pj=pj, c0=c0, w=w, rows=rows: e.activation(out=pt[pj][0:rows, c0:c0 + w], in_=sbs[bi][0:rows, 0:w], func=AF.Exp),
                             reads=[sbsB[bi]], writes=[ptB[pj]])
                        if c0 + w < N:
                            P.op("act", lambda e, sk=sk, pj=pj, c0=c0, w=w, b15=b15, rows=rows: e.activation(
                                out=pt[pj][0:rows, c0 + w:N], in_=ps[sk][0:rows, c0 + w:N], func=AF.Exp, scale=DIFF_SCALE, bias=b15),
                                reads=[psB[sk], BbB, ptB[pj]], writes=[ptB[pj]])
                    else:
                        P.op("act", lambda e, sk=sk, pj=pj, c0=c0, b15=b15, rows=rows: e.activation(
                            out=pt[pj][0:rows, c0:N], in_=ps[sk][0:rows, c0:N], func=AF.Exp, scale=DIFF_SCALE, bias=b15),
                            reads=[psB[sk], BbB], writes=[ptB[pj]])
                if tl["diag"]:
                    P.op("pool", lambda e, pj=pj, c0=c0: e.tensor_tensor(out=pt[pj][64:128, c0:c0 + 64], in0=pt[pj][64:128, c0:c0 + 64], in1=mpib[64:128, :], op=ALU.mult),
                         reads=[ptB[pj], cB], writes=[ptB[pj]])
                if tl["pmask"] is not None:
                    pm = tl["pmask"]
                    P.op("pool", lambda e, pj=pj, pm=pm, rows=rows: e.tensor_tensor(out=pt[pj][0:rows, 0:N], in0=pt[pj][0:rows, 0:N], in1=pm.broadcast_to([rows, N]), op=ALU.mult),
                         reads=[ptB[pj], cB], writes=[ptB[pj]])
            pend.append((i, tl, pjs))
            if len(pend) > 1:
                pv(pend.pop(0))
        while pend:
            pv(pend.pop(0))

    def prompt_pass(kind, h, slot):
        kt_, v_, q_ = ktb[slot], vb[slot], qtb[slot]
        for G in range(8):
            tiles = []
            for kt in range(8 * G):
                bias = (64, 64) if (kt == 8 * G - 1) else None
                tiles.append(dict(kt=kt, c0=0, rows=128, diag=False, bias=bias, pmask=None))
            for k in range(8):
                tiles.append(dict(kt=8 * G + k, c0=64 * k, rows=128, diag=True, bias=(0, 128), pmask=None))
            for tl in tiles:
                kt = tl["kt"]
                if kind == "m":
                    tl["k_ap"] = (lambda m, kt=kt: kt_[0:96, kt * 128:(kt + 1) * 128])
                    tl["v_ap"] = v_[:, kt, :]
                else:
                    tl["k_ap"] = (lambda m, kt=kt: kt_[:, kt * 128:(kt + 1) * 128])
                    tl["v_ap"] = v_[:, kt, :]
                tl["kB"] = ktB[slot]; tl["vB"] = vB[slot]
            if kind == "m":
                acc = 3 + (G % 2)
                attn_tiles("m", h, tiles, lambda m, c0, G=G: q_[0:96, 0, G * 512 + c0:(G + 1) * 512], qtB[slot], G * 512, 512, [acc])
                fin_mla(acc, h, G * 512, 512)
            else:
                attn_tiles("d", h, tiles, lambda m, c0, G=G: q_[:, m, G * 512 + c0:(G + 1) * 512], qtB[slot], G * 512, 512, [3, 4, 5, 6])
                fin_diff(h, G * 512, 512)

    def sample_pass(kind, h, slot, s):
        kt_, v_ = ktb[slot], vb[slot]
        tiles = []
        for kt in range(32):
            tl = dict(c0=0, rows=128, diag=False, bias=((192, 16) if kt == 31 else None), pmask=None, kB=ktB[slot], vB=vB[slot], v_ap=v_[:, kt, :])
            if kind == "m":
                tl["k_ap"] = (lambda m, kt=kt: kt_[0:96, kt * 128:(kt + 1) * 128])
            else:
                tl["k_ap"] = (lambda m, kt=kt: kt_[:, kt * 128:(kt + 1) * 128])
            tiles.append(tl)
        tl = dict(c0=0, rows=48, diag=False, bias=((256 + 64 * s, 16)), pmask=C2[0:48, 512 + s:513 + s], kB=nB, vB=nB)
        if kind == "m":
            tl["k_ap"] = (lambda m: nkT[0:96, h, 0:48]); tl["v_ap"] = nv[:, h, :]
            qf = lambda m, c0: nqT[0:96, h, s * 32:s * 32 + 16]
        else:
            tl["k_ap"] = (lambda m: ndkT[:, h, 0:48]); tl["v_ap"] = ndv[:, h, :]
            qf = lambda m, c0: ndqT[:, m, h, s * 32:s * 32 + 16]
        tiles.append(tl)
        col0 = 4096 + s * 32
        if kind == "m":
            acc = 3 + (ring("o2", 2) if False else 0)
            attn_tiles("m", h, tiles, qf, nB, col0, 16, [3])
            fin_mla(3, h, col0, 16)
        else:
            attn_tiles("d", h, tiles, qf, nB, col0, 16, [3, 4, 5, 6])
            fin_diff(h, col0, 16)

    passes = [("m", h) for h in range(8)] + [("d", h) for h in range(4)]
    load_pass(passes[0][0], passes[0][1], 0)
    tz = [P.dma("sp", lambda e: e.dma_start(out=OT[:, 4096:4224].rearrange("(k p) t -> p k t", p=128), in_=zt[:, 0:1024].rearrange("p (k t) -> p k t", k=8)), reads=[ztB])]
    zero_fill()
    for i, (kind, h) in enumerate(passes):
        if i + 1 < len(passes):
            load_pass(passes[i + 1][0], passes[i + 1][1], (i + 1) % 2)
        prompt_pass(kind, h, i % 2)
    sp_list = [(kind, h, s) for s in range(2) for (kind, h) in passes]
    load_pass(sp_list[0][0], sp_list[0][1], 0, s=sp_list[0][2])
    for i, (kind, h, s) in enumerate(sp_list):
        if i + 1 < len(sp_list):
            load_pass(sp_list[i + 1][0], sp_list[i + 1][1], (i + 1) % 2, s=sp_list[i + 1][2])
        sample_pass(kind, h, i % 2, s)
    if KCUT == 7:
        return finish()

    AR.reset()
    woutb = AR.bf([128, 8, 1024]); wrb = AR.bf([128, 8, 256]); wsgb = AR.bf([128, 8, 512]); wsdb = AR.bf([128, 2, 1024])
    wDB = [Buf() for _ in range(4)]
    P.dma("pool", lambda e: e.dma_start(out=woutb, in_=w_out.rearrange("(k p) n -> p k n", p=128)), writes=[wDB[0]])
    P.dma("pool", lambda e: e.dma_start(out=wrb, in_=w_router.rearrange("(k p) n -> p k n", p=128)), writes=[wDB[1]])
    P.dma("pool", lambda e: e.dma_start(out=wsgb, in_=w_sgu.rearrange("(k p) n -> p k n", p=128)), writes=[wDB[2]])
    P.dma("pool", lambda e: e.dma_start(out=wsdb, in_=w_sdn.rearrange("(k p) n -> p k n", p=128)), writes=[wDB[3]])
    wrf = AR.f32([128, 8, 256]); wrfB = Buf()
    P.dma("sp", lambda e: e.dma_start(out=wrf, in_=w_router.rearrange("(k p) n -> p k n", p=128)), writes=[wrfB])
    h2ff = AR.f32([128, 1024]); h2ffB = Buf(); h2Tf = AR.f32([128, 8, 128]); h2TfB = Buf()
    rbb = AR.f32([128, 256]); rbbB = Buf()
    P.dma("sp", lambda e: e.dma_start(out=rbb, in_=rbias.broadcast_to([128, 256])), writes=[rbbB])
    oTs = [AR.bf([128, 8, 512]) for _ in range(2)]; oTsB = [Buf(), Buf()]
    xd = [AR.f32([128, 1024]) for _ in range(2)]; xdB = [Buf(), Buf()]
    x1 = AR.f32([128, 1024]); x1B = Buf()
    tmpd = AR.f32([128, 1024]); tmpdB = Buf()
    h2f = AR.f32([128, 1024]); h2fB = Buf()
    h2b = [AR.bf([128, 1024]) for _ in range(2)]; h2bB = [Buf(), Buf()]
    h2T = AR.bf([128, 8, 128]); h2TB = Buf()
    junkd = AR.bf([128, 1024])
    ssD = AR.f32([128, 8]); ssDB = Buf()
    sc = AR.f32([128, 256]); scB = Buf()
    sgd = AR.f32([128, 256]); sgdB = Buf()
    abd = AR.bf([128, 256]); abdB = Buf(); aTd = AR.bf([128, 2, 128]); aTdB = Buf()
    biased = AR.f32([128, 256]); m8 = AR.f32([128, 8, 8]); gs = AR.f32([128, 8]); t8 = AR.f32([128, 8]); gm = AR.f32([128, 8])
    masked = AR.f32([128, 256]); v8 = AR.f32([128, 8]); sel = AR.f32([128, 256]); gsel = AR.f32([128, 256]); den = AR.f32([128, 8])
    Gt = AR.f32([128, 256]); selb = AR.bf([128, 256]); posf = AR.f32([128, 256]); key = AR.f32([128, 256]); d8 = AR.f32([128, 8]); neg = AR.f32([128, 8])
    neg2 = AR.f32([128, 8]); dA = AR.f32([128, 8]); dB = AR.f32([128, 8])
    key2 = AR.f32([128, 256]); key3 = AR.f32([128, 256]); v8b = AR.f32([128, 8]); v8c = AR.f32([128, 8])
    rtB = Buf()
    selbB = Buf()

    def rt(fn, extra_r=(), extra_w=()):
        P.op("dve", fn, reads=[rtB] + list(extra_r), writes=[rtB] + list(extra_w))

    def phaseD_tile(tile):
        sample = (tile == 32)
        r = tile % 2
        if sample:
            P.dma("sp", lambda e: e.dma_start(out=oTs[0][:, :, 0:128], in_=OT[:, 4096:4224].rearrange("(k p) t -> p k t", p=128)), writes=[oTsB[0]])
            oT = oTs[0]; oTB = oTsB[0]; tc0 = 0
            xsrc = x_s
        else:
            G = tile // 4
            if tile % 4 == 0:
                P.dma("sp", lambda e: e.dma_start(out=oTs[G % 2], in_=OT[:, G * 512:(G + 1) * 512].rearrange("(k p) t -> p k t", p=128)), writes=[oTsB[G % 2]])
            oT = oTs[G % 2]; oTB = oTsB[G % 2]; tc0 = (tile % 4) * 128
            xsrc = x_own[tile * 128:(tile + 1) * 128, :]
        P.dma("sp", lambda e: e.dma_start(out=xd[r], in_=xsrc), writes=[xdB[r]])
        pa = nextps(); pb_ = nextps()

        def f(e):
            for nh, pj in ((0, pa), (1, pb_)):
                for k in range(8):
                    ins = e.matmul(ps[pj][:], lhsT=oT[:, k, tc0:tc0 + 128], rhs=woutb[:, k, nh * 512:(nh + 1) * 512], start=(k == 0), stop=(k == 7))
            return ins
        P.op("pe", f, reads=[oTB, wDB[0]], writes=[psB[pa], psB[pb_]])
        for nh, pj in ((0, pa), (1, pb_)):
            P.op("dve", lambda e, nh=nh, pj=pj: e.tensor_tensor(out=tmpd[:, nh * 512:(nh + 1) * 512], in0=ps[pj][:], in1=M[2][:, nh * 512:(nh + 1) * 512], op=ALU.mult),
                 reads=[psB[pj], MB[2]], writes=[tmpdB])
        P.op("pool", lambda e: e.tensor_tensor(out=x1, in0=tmpd, in1=xd[r], op=ALU.add), reads=[tmpdB, xdB[r]], writes=[x1B])
        P.op("pool", lambda e: e.memset(ssD[:, 0:1], 0.0), writes=[ssDB])
        P.op("act", lambda e: e.activation(out=junkd, in_=x1, func=AF.Square, accum_out=ssD[:, 0:1]), reads=[x1B], writes=[ssDB])
        P.op("dve", lambda e: e.tensor_scalar(out=ssD[:, 0:1], in0=ssD[:, 0:1], scalar1=1.0 / 1024, scalar2=EPS, op0=ALU.mult, op1=ALU.add), reads=[ssDB], writes=[ssDB])
        P.op("pool", lambda e: e.tensor_tensor(out=ssD[:, 0:1], in0=ssD[:, 0:1], in1=nhalf[:, 0:1], op=ALU.pow), reads=[ssDB], writes=[ssDB])
        P.op("dve", lambda e: e.scalar_tensor_tensor(out=h2f, in0=x1, scalar=ssD[:, 0:1], in1=M[3][:], op0=ALU.mult, op1=ALU.mult), reads=[x1B, ssDB, MB[3]], writes=[h2fB])
        P.op("pool", lambda e: e.tensor_tensor(out=h2ff, in0=h2f, in1=M[4][:], op=ALU.add), reads=[h2fB, MB[4]], writes=[h2ffB])
        P.op("pool", lambda e: e.tensor_copy(out=h2b[r], in_=h2ff), reads=[h2ffB], writes=[h2bB[r]])
        transposes(lambda k: h2b[r][:, k * 128:(k + 1) * 128], 8, 128, [h2bB[r]], h2T, h2TB, eng="act")
        for hh in range(2):
            pt_ = nextps()

            def ft(e, pt_=pt_, hh=hh):
                for k in range(4):
                    ins = e.transpose(out=ps[pt_][:, k * 128:(k + 1) * 128], in_=h2ff[:, (hh * 4 + k) * 128:(hh * 4 + k + 1) * 128], identity=identf)
                return ins
            P.op("pe", ft, reads=[h2ffB, CFb], writes=[psB[pt_]])
            P.op("dve", lambda e, pt_=pt_, hh=hh: e.tensor_copy(out=h2Tf[:, hh * 4:hh * 4 + 4, :], in_=ps[pt_][:].rearrange("p (a b) -> p a b", a=4)),
                 reads=[psB[pt_]], writes=[h2TfB])
        pr = nextps(); pg = nextps()

        def f2(e):
            for k in range(8):
                e.matmul(ps[pr][:, 0:256], lhsT=h2Tf[:, k, :], rhs=wrf[:, k, :], start=(k == 0), stop=(k == 7))
            for k in range(8):
                ins = e.matmul(ps[pg][:], lhsT=h2T[:, k, :], rhs=wsgb[:, k, :], start=(k == 0), stop=(k == 7))
            return ins
        P.op("pe", f2, reads=[h2TB, h2TfB, wrfB, wDB[2]], writes=[psB[pr], psB[pg]])
        P.op("act", lambda e: e.activation(out=sc, in_=ps[pr][:, 0:256], func=AF.Sigmoid), reads=[psB[pr]], writes=[scB])
        P.op("act", lambda e: e.activation(out=sgd, in_=ps[pg][:, 0:256], func=AF.Silu), reads=[psB[pg]], writes=[sgdB])
        P.op("dve", lambda e: e.tensor_tensor(out=abd, in0=ps[pg][:, 256:512], in1=sgd, op=ALU.mult), reads=[psB[pg], sgdB], writes=[abdB])
        transposes(lambda k: abd[:, k * 128:(k + 1) * 128], 2, 128, [abdB], aTd, aTdB)
        pa2 = nextps(); pb2 = nextps()

        def f3(e):
            for nh, pj in ((0, pa2), (1, pb2)):
                for k in range(2):
                    ins = e.matmul(ps[pj][:], lhsT=aTd[:, k, :], rhs=wsdb[:, k, nh * 512:(nh + 1) * 512], start=(k == 0), stop=(k == 1))
            return ins
        P.op("pe", f3, reads=[aTdB, wDB[3]], writes=[psB[pa2], psB[pb2]])
        for nh, pj in ((0, pa2), (1, pb2)):
            P.op("dve", lambda e, nh=nh, pj=pj: e.tensor_tensor(out=tmpd[:, nh * 512:(nh + 1) * 512], in0=ps[pj][:], in1=M[5][:, nh * 512:(nh + 1) * 512], op=ALU.mult),
                 reads=[psB[pj], MB[5]], writes=[tmpdB])
        P.op("pool", lambda e: e.tensor_tensor(out=x1, in0=tmpd, in1=x1, op=ALU.add), reads=[tmpdB, x1B], writes=[x1B])
        P.dma("sp", lambda e: e.dma_start(out=X1[tile * 128:(tile + 1) * 128, :], in_=x1), reads=[x1B])
        rt(lambda e: e.tensor_tensor(out=biased, in0=sc, in1=rbb, op=ALU.add), extra_r=[scB, rbbB])
        for g in range(8):
            rt(lambda e, g=g: e.max(out=m8[:, g, :], in_=biased[:, g * 32:(g + 1) * 32]))
        rt(lambda e: e.tensor_tensor(out=gs, in0=m8[:, :, 0], in1=m8[:, :, 1], op=ALU.add))
        rt(lambda e: e.max(out=t8, in_=gs))
        rt(lambda e: e.tensor_single_scalar(out=gm, in_=gs, scalar=t8[:, 3:4], op=ALU.is_ge))
        rt(lambda e: e.tensor_scalar(out=gm, in0=gm, scalar1=-1.0, scalar2=1e9, op0=ALU.add, op1=ALU.mult))
        rt(lambda e: e.tensor_tensor(out=masked.rearrange("p (g c) -> p g c", g=8), in0=biased.rearrange("p (g c) -> p g c", g=8),
                                     in1=gm.unsqueeze(2).broadcast_to([128, 8, 32]), op=ALU.add))
        rt(lambda e: e.max(out=v8, in_=masked))
        rt(lambda e: e.tensor_single_scalar(out=sel, in_=masked, scalar=v8[:, 7:8], op=ALU.is_ge))
        vcol = C2[:, 514:515] if sample else C2[:, 515:516]
        rt(lambda e: e.tensor_scalar(out=sel, in0=sel, scalar1=vcol, scalar2=None, op0=ALU.mult), extra_r=[cB])
        rt(lambda e: e.tensor_tensor(out=gsel, in0=sel, in1=sc, op=ALU.mult))
        rt(lambda e: e.tensor_reduce(out=den[:, 0:1], in_=gsel, axis=AX.X, op=ALU.add))
        rt(lambda e: e.tensor_scalar(out=den[:, 0:1], in0=den[:, 0:1], scalar1=1e-20, scalar2=None, op0=ALU.add))
        rt(lambda e: e.reciprocal(out=den[:, 1:2], in_=den[:, 0:1]))
        rt(lambda e: e.tensor_scalar(out=Gt, in0=gsel, scalar1=den[:, 1:2], scalar2=2.5, op0=ALU.mult, op1=ALU.mult))
        P.op("pool", lambda e: e.tensor_copy(out=selb, in_=sel), reads=[rtB], writes=[selbB])
        pp = nextps(); pc = nextps()

        def f4(e):
            e.matmul(ps[pp][:, 0:256], lhsT=Ub[:], rhs=selb, start=True, stop=True)
            return e.matmul(ps[pc][:, 0:256], lhsT=onesb[:], rhs=selb, start=True, stop=True)
        P.op("pe", f4, reads=[selbB, cB], writes=[psB[pp], psB[pc]])
        rt(lambda e: e.tensor_tensor(out=posf, in0=ps[pp][:, 0:256], in1=cntbc[:], op=ALU.add), extra_r=[psB[pp], cntB])
        rt(lambda e: e.tensor_tensor(out=cntbc[:], in0=ps[pc][:, 0:256], in1=cntbc[:], op=ALU.add), extra_r=[psB[pc]], extra_w=[cntB])
        rt(lambda e: e.tensor_single_scalar(out=key, in_=posf, scalar=float(CAP), op=ALU.is_lt))
        rt(lambda e: e.tensor_tensor(out=sel, in0=sel, in1=key, op=ALU.mult))
        rt(lambda e: e.tensor_tensor(out=posf, in0=posf, in1=C2[:, 0:256], op=ALU.add))
        rt(lambda e: e.tensor_tensor(out=key, in0=posf, in1=sel, op=ALU.mult))
        rt(lambda e: e.max(out=d8, in_=key))
        rt(lambda e: e.tensor_scalar(out=d8, in0=d8, scalar1=-1.0, scalar2=None, op0=ALU.add))
        rt(lambda e: e.tensor_single_scalar(out=neg, in_=d8, scalar=0.0, op=ALU.is_lt))
        rt(lambda e: e.tensor_single_scalar(out=neg2, in_=d8, scalar=float(HALF), op=ALU.is_ge))
        rt(lambda e: e.tensor_tensor(out=neg, in0=neg, in1=neg2, op=ALU.add))
        rt(lambda e: e.scalar_tensor_tensor(out=dA, in0=neg, scalar=BIG, in1=d8, op0=ALU.mult, op1=ALU.add))
        rt(lambda e: e.tensor_copy(out=didx[:, tile, 0:8], in_=dA))
        rt(lambda e: e.tensor_scalar(out=dB, in0=neg2, scalar1=-1.0, scalar2=-BIG, op0=ALU.add, op1=ALU.mult))
        rt(lambda e: e.scalar_tensor_tensor(out=dB, in0=d8, scalar=-float(HALF), in1=dB, op0=ALU.add, op1=ALU.add))
        rt(lambda e: e.tensor_copy(out=didx[:, tile, 8:16], in_=dB))
        rt(lambda e: e.tensor_scalar(out=neg, in0=neg, scalar1=-1.0, scalar2=-1.0, op0=ALU.add, op1=ALU.mult))
        rt(lambda e: e.tensor_tensor(out=dA, in0=d8, in1=neg, op=ALU.mult))
        rt(lambda e: e.tensor_copy(out=didx[:, tile, 16:24], in_=dA))
        rt(lambda e: e.scalar_tensor_tensor(out=dB, in0=d8, scalar=-float(HALF), in1=neg2, op0=ALU.add, op1=ALU.mult))
        rt(lambda e: e.tensor_copy(out=didx[:, tile, 24:32], in_=dB), extra_w=[dgB[tile]])
        rt(lambda e: e.tensor_tensor(out=key3, in0=C2[:, 256:512], in1=sel, op=ALU.mult))
        rt(lambda e: e.tensor_tensor(out=key2, in0=Gt, in1=sel, op=ALU.mult))
        rt(lambda e: e.tensor_tensor(out=key2, in0=key2, in1=key3, op=ALU.add))
        rt(lambda e: e.max(out=v8b, in_=key2))
        rt(lambda e: e.max(out=v8c, in_=key3))
        rt(lambda e: e.tensor_tensor(out=v8b, in0=v8b, in1=v8c, op=ALU.subtract))
        rt(lambda e: e.tensor_tensor(out=gate8[:, tile, 0:8], in0=v8b, in1=neg, op=ALU.mult))
        rt(lambda e: e.tensor_tensor(out=gate8[:, tile, 8:16], in0=v8b, in1=neg2, op=ALU.mult), extra_w=[dgB[tile]])
        for j in range(16):
            P.dma("pool", lambda e, j=j: e.indirect_dma_start(out=XGs[j // 8][:, :], out_offset=bass.IndirectOffsetOnAxis(ap=didx[:, tile, j:j + 1], axis=0),
                                                            in_=h2b[r][:, :], in_offset=None, bounds_check=getbc(e), oob_is_err=False),
                  reads=[h2bB[r], dgB[tile]])

    load_mod(False, (0, 1))
    for tile in range(32):
        phaseD_tile(tile)
    load_mod(True, (2, 3, 4, 5))
    phaseD_tile(32)
    if KCUT == 8:
        return finish()

    AR.reset()
    wg = [AR.bf([128, 8, 512]) for _ in range(2)]; wgB = [Buf(), Buf()]
    wd = [AR.bf([128, 2, 1024]) for _ in range(2)]; wdB = [Buf(), Buf()]
    xg = [AR.bf([128, NST, 1024]) for _ in range(2)]; xgB = [Buf(), Buf()]
    xgT = [AR.bf([128, 8, CAP]) for _ in range(2)]; xgTB = [Buf(), Buf()]
    sge = [AR.f32([128, 2, 320]) for _ in range(2)]; sgeB = [Buf(), Buf()]
    aTe = [AR.bf([128, 2, CAP]) for _ in range(2)]; aTeB = [Buf(), Buf()]
    yb = [AR.bf([128, NST, 1024]) for _ in range(2)]; ybB = [Buf(), Buf()]
    HN = CAP // 2
    for ex in range(256):
        r = ex % 2
        XG = XGs[ex // 128]; YG = YGs[ex // 128]; row0 = (ex % 128) * CAP
        P.dma("pool", lambda e, ex=ex, r=r: e.dma_start(out=wg[r], in_=w_gu[ex].rearrange("(k p) n -> p k n", p=128)), writes=[wgB[r]])
        P.dma("pool", lambda e, ex=ex, r=r: e.dma_start(out=wd[r], in_=w_dn[ex].rearrange("(k p) n -> p k n", p=128)), writes=[wdB[r]])
        P.dma("sp", lambda e, XG=XG, row0=row0, r=r: e.dma_start(out=xg[r], in_=XG[row0:row0 + CAP, :].rearrange("(s p) c -> p s c", p=128)), writes=[xgB[r]])
        for s_ in range(NST):
            transposes(lambda k, s_=s_, r=r: xg[r][:, s_, k * 128:(k + 1) * 128], 8, 128, [xgB[r]], xgT[r][:, :, s_ * 128:(s_ + 1) * 128], xgTB[r],
                       eng=("act" if s_ % 2 == 0 else "dve"))
        for nh in range(2):
            pbs = [nextps() for _ in range(4)]

            def f(e, r=r, pbs=pbs, nh=nh):
                for c in range(4):
                    for k in range(8):
                        ins = e.matmul(ps[pbs[c]][:, 0:HN], lhsT=wg[r][:, k, c * 128:(c + 1) * 128], rhs=xgT[r][:, k, nh * HN:(nh + 1) * HN], start=(k == 0), stop=(k == 7))
                return ins
            P.op("pe", f, reads=[wgB[r], xgTB[r]], writes=[psB[p_] for p_ in pbs])
            for c in range(2):
                P.op("act", lambda e, c=c, pbs=pbs, nh=nh: e.activation(out=sge[nh][:, c, 0:HN], in_=ps[pbs[c]][:, 0:HN], func=AF.Silu), reads=[psB[pbs[c]]], writes=[sgeB[nh]])
                P.op("dve", lambda e, c=c, pbs=pbs, nh=nh, r=r: e.tensor_tensor(out=aTe[r][:, c, nh * HN:(nh + 1) * HN], in0=ps[pbs[2 + c]][:, 0:HN], in1=sge[nh][:, c, 0:HN], op=ALU.mult),
                     reads=[psB[pbs[2 + c]], sgeB[nh]], writes=[aTeB[r]])
        for s_ in range(NST):
            for nh in range(2):
                pj = nextps()

                def f2(e, r=r, pj=pj, s_=s_, nh=nh):
                    for k in range(2):
                        ins = e.matmul(ps[pj][:], lhsT=aTe[r][:, k, s_ * 128:(s_ + 1) * 128], rhs=wd[r][:, k, nh * 512:(nh + 1) * 512], start=(k == 0), stop=(k == 1))
                    return ins
                P.op("pe", f2, reads=[aTeB[r], wdB[r]], writes=[psB[pj]])
                if (s_ + nh) % 2 == 0:
                    P.op("act", lambda e, r=r, pj=pj, s_=s_, nh=nh: e.activation(out=yb[r][:, s_, nh * 512:(nh + 1) * 512], in_=ps[pj][:], func=AF.Copy), reads=[psB[pj]], writes=[ybB[r]])
                else:
                    P.op("dve", lambda e, r=r, pj=pj, s_=s_, nh=nh: e.tensor_copy(out=yb[r][:, s_, nh * 512:(nh + 1) * 512], in_=ps[pj][:]), reads=[psB[pj]], writes=[ybB[r]])
        P.dma("sp", lambda e, YG=YG, row0=row0, r=r: e.dma_start(out=YG[row0:row0 + CAP, :].rearrange("(s p) c -> p s c", p=128), in_=yb[r]), reads=[ybB[r]])
    if KCUT == 9:
        return finish()

    AR.reset()
    yj = [AR.bf([128, 1024]) for _ in range(16)]; yjB = [Buf() for _ in range(16)]
    accF = AR.f32([128, 1024]); accB = Buf()
    x1f = AR.f32([128, 1024]); x1fB = Buf()
    x2 = AR.f32([128, 1024]); x2B = Buf()
    yo = AR.f32([128, 1024]); yoB = Buf()
    fnb = AR.f32([128, 1024]); fnbB = Buf()
    junkf = AR.bf([128, 1024]); ssF = AR.f32([128, 8]); ssFB = Buf()
    P.dma("sp", lambda e: e.dma_start(out=fnb, in_=fnorm.broadcast_to([128, 1024])), writes=[fnbB])
    for j in range(16):
        P.op("pool", lambda e, j=j: e.memset(yj[j], 0.0), writes=[yjB[j]])

    def phaseF_tile(tile):
        for j in range(16):
            P.dma("pool", lambda e, j=j: e.indirect_dma_start(out=yj[j][:, :], out_offset=None, in_=YGs[j // 8][:, :],
                                                            in_offset=bass.IndirectOffsetOnAxis(ap=didx[:, tile, 16 + j:17 + j], axis=0),
                                                            bounds_check=getbc(e), oob_is_err=False), reads=[dgB[tile]], writes=[yjB[j]])
        P.dma("sp", lambda e: e.dma_start(out=x1f, in_=X1[tile * 128:(tile + 1) * 128, :]), writes=[x1fB])
        P.op("dve", lambda e: e.tensor_scalar(out=accF, in0=yj[0], scalar1=gate8[:, tile, 0:1], scalar2=None, op0=ALU.mult), reads=[yjB[0], dgB[tile]], writes=[accB])
        for j in range(1, 16):
            P.op("dve", lambda e, j=j: e.scalar_tensor_tensor(out=accF, in0=yj[j], scalar=gate8[:, tile, j:j + 1], in1=accF, op0=ALU.mult, op1=ALU.add),
                 reads=[yjB[j], dgB[tile], accB], writes=[accB])
        P.op("dve", lambda e: e.tensor_tensor(out=accF, in0=accF, in1=M[5][:], op=ALU.mult), reads=[accB, MB[5]], writes=[accB])
        P.op("pool", lambda e: e.tensor_tensor(out=x2, in0=accF, in1=x1f, op=ALU.add), reads=[accB, x1fB], writes=[x2B])
        P.op("pool", lambda e: e.memset(ssF[:, 0:1], 0.0), writes=[ssFB])
        P.op("act", lambda e: e.activation(out=junkf, in_=x2, func=AF.Square, accum_out=ssF[:, 0:1]), reads=[x2B], writes=[ssFB])
        P.op("dve", lambda e: e.tensor_scalar(out=ssF[:, 0:1], in0=ssF[:, 0:1], scalar1=1.0 / 1024, scalar2=EPS, op0=ALU.mult, op1=ALU.add), reads=[ssFB], writes=[ssFB])
        P.op("pool", lambda e: e.tensor_tensor(out=ssF[:, 0:1], in0=ssF[:, 0:1], in1=nhalf[:, 0:1], op=ALU.pow), reads=[ssFB], writes=[ssFB])
        P.op("dve", lambda e: e.scalar_tensor_tensor(out=yo, in0=x2, scalar=ssF[:, 0:1], in1=fnb, op0=ALU.mult, op1=ALU.mult), reads=[x2B, ssFB, fnbB], writes=[yoB])
        dst = o_ys if tile == 32 else o_y[tile * 128:(tile + 1) * 128, :]
        P.dma("sp", lambda e: e.dma_start(out=dst, in_=yo), reads=[yoB])

    phaseF_tile(32)
    load_mod(False, (5,))
    for tile in range(32):
        phaseF_tile(tile)
    return finish()


_CACHE = {}


def _bucket(rel):
    rel = np.asarray(rel, np.int64)
    n = np.abs(rel)
    nf = np.maximum(n, 1).astype(np.float32)
    large = 8 + (np.log(nf / np.float32(8)) / np.float32(math.log(128 / 8)) * np.float32(8)).astype(np.int32)
    large = np.minimum(large, 15)
    return np.where(rel > 0, 16, 0) + np.where(n < 8, n, large)


def _consts(pi):
    cst = np.zeros((128, 1600), np.float32)
    cst[:, 0:128] = np.eye(128, dtype=np.float32)
    cst[:, 128:256] = np.eye(128, dtype=np.float32)[::-1]
    offs = [64 * pi, 64 * pi + 128, 64 * pi + 256, 128, 0, 32]
    oh = np.zeros((32, 6, 192), np.float32)
    for b, off in enumerate(offs):
        j = np.arange(192)
        bk = _bucket(127 - j - off)
        oh[bk, b, j] = 1.0
    cst[0:32, 256:1408] = oh.reshape(32, 1152)
    cst[0:64, 1408:1472] = 1.0
    cst[64:128, 1408:1472] = float(pi)
    cst[:, 1472:1600] = np.triu(np.ones((128, 128), np.float32), 1)
    return cst


def _consts2():
    c = np.zeros((128, 520), np.float32)
    e = np.arange(256, dtype=np.float32)
    c[:, 0:256] = e * CAP + 1.0
    c[:, 256:512] = 4.0 * e + 1.0
    c[0:16, 512] = 1.0; c[32:48, 513] = 1.0
    c[0:16, 514] = 1.0; c[32:48, 514] = 1.0
    c[:, 515] = 1.0
    return c


def _rope_tab(pos):
    half = 16
    inv = (10000.0 ** (-np.arange(half, dtype=np.float32) / half)).astype(np.float32)
    ang = pos.astype(np.float32)[:, None] * inv
    c = np.cos(ang).astype(np.float32); s = np.sin(ang).astype(np.float32)
    return np.concatenate([c, c, -s, s], axis=1).astype(np.float32)


def _in_maps(inp, cores=range(8)):
    f = lambda a: np.ascontiguousarray(np.asarray(a, dtype=np.float32))
    xp = f(inp["x_prompt"]); xs = f(inp["x_sample"])
    in_maps = []
    for c in cores:
        b, pi = c // 2, c % 2
        xo = xp[b].reshape(64, 2, 64, 1024)[:, pi].reshape(4096, 1024)
        pos_own = (np.arange(64)[:, None] * 128 + pi * 64 + np.arange(64)[None, :]).reshape(-1)
        x_s = np.zeros((128, 1024), np.float32); x_s[0:16] = xs[2 * c]; x_s[32:48] = xs[2 * c + 1]
        rs = np.zeros((128, 64), np.float32); rs[:, 0:32] = 1.0
        rs[0:16] = _rope_tab(4096 + np.arange(16)); rs[32:48] = rs[0:16]
        m = {
            "x_all": xp[b], "x_own": f(xo), "x_s": x_s,
            "clat": f(inp["cache_mla_latent"][0, 2 * c:2 * c + 2]), "ckr": f(inp["cache_mla_krope"][0, 2 * c:2 * c + 2]),
            "cdk": f(inp["cache_diff_k"][0, 2 * c:2 * c + 2]).reshape(2, 4096, 512),
            "cdv": f(inp["cache_diff_v"][0, 2 * c:2 * c + 2]).reshape(2, 4096, 512),
            "c3": f(np.stack([inp["c_prompt"][b], inp["c_sample"][2 * c], inp["c_sample"][2 * c + 1]])),
            "w_ada": f(inp["w_ada"][0]), "b_ada": f(inp["b_ada"]), "norm_attn": f(inp["norm_attn"]), "w_in": f(inp["w_in"][0]),
            "qg": f(inp["mla_q_norm"]), "kvg": f(inp["mla_kv_norm"]), "w_uq": f(inp["w_uq"][0]), "w_ukv": f(inp["w_ukv"][0]),
            "lam4": f(np.concatenate([inp["lambda_q1"], inp["lambda_k1"], inp["lambda_q2"], inp["lambda_k2"]], 0)),
            "subln": f(inp["diff_subln"]).reshape(128, 1), "relb": f(inp["rel_bias"]).reshape(32, 8),
            "w_out": f(inp["w_out"][0]), "norm_ffn": f(inp["norm_ffn"]), "w_router": f(inp["w_router"][0]), "rbias": f(inp["router_bias"]),
            "w_sgu": f(inp["w_shared_gu"][0]), "w_sdn": f(inp["w_shared_down"][0]),
            "fnorm": f(inp["final_norm"]).reshape(1, 1024),
            "cst": _consts(pi), "cst2": _consts2(), "rope_all": _rope_tab(np.arange(8192)), "rope_own": _rope_tab(pos_own), "rope_s": rs,
        }
        if STAGE > 1:
            m["w_gu"] = f(inp["w_exp_gu"][0]); m["w_dn"] = f(inp["w_exp_down"][0])
        in_maps.append(m)
    return in_maps


def kernel(**inp):
    if "prog" not in _CACHE:
        _CACHE["prog"] = build_program()
    nc, _es = _CACHE["prog"]
    in_maps = _in_maps(inp)
    res = run_bass_kernel_spmd(nc, in_maps, core_ids=list(range(8))).results
    y_p = np.zeros((4, 8192, 1024), np.float32); y_s = np.zeros((16, 16, 1024), np.float32)
    lat_p = np.zeros((1, 4, 8192, 256), np.float32); kpe_p = np.zeros((1, 4, 8192, 32), np.float32)
    dk_p = np.zeros((1, 4, 8192, 4, 2, 64), np.float32); dv_p = np.zeros((1, 4, 8192, 4, 128), np.float32)
    lat_s = np.zeros((1, 16, 16, 256), np.float32); kpe_s = np.zeros((1, 16, 16, 32), np.float32)
    dk_s = np.zeros((1, 16, 16, 4, 2, 64), np.float32); dv_s = np.zeros((1, 16, 16, 4, 128), np.float32)
    for c in range(8):
        b, pi = c // 2, c % 2
        r = res[c]
        y_p[b].reshape(64, 2, 64, 1024)[:, pi] = r["o_y"].reshape(64, 64, 1024)
        if pi == 0:
            lat_p[0, b] = r["o_lat"]; kpe_p[0, b] = r["o_kpe"]
            dk_p[0, b] = r["o_dk"].reshape(8192, 4, 2, 64); dv_p[0, b] = r["o_dv"].reshape(8192, 4, 128)
        for s in range(2):
            sl = slice(32 * s, 32 * s + 16)
            y_s[2 * c + s] = r["o_ys"][sl]
            lat_s[0, 2 * c + s] = r["o_lats"][sl]; kpe_s[0, 2 * c + s] = r["o_kpes"][sl]
            dk_s[0, 2 * c + s] = r["o_dks"][sl].reshape(16, 4, 2, 64); dv_s[0, 2 * c + s] = r["o_dvs"][sl].reshape(16, 4, 128)
    return (y_p, y_s, lat_p, kpe_p, dk_p, dv_p, lat_s, kpe_s, dk_s, dv_s)
```

```python
import math
import os
from contextlib import ExitStack
import numpy as np
import concourse.bass as bass
import concourse.mybir as mybir
from concourse.bass_utils import run_bass_kernel_spmd

F32 = mybir.dt.float32; BF16 = mybir.dt.bfloat16; I32 = mybir.dt.int32
ALU = mybir.AluOpType; AF = mybir.ActivationFunctionType; AX = mybir.AxisListType
ENG = ("pe", "act", "dve", "pool", "sp")
EPS = 1e-6
MLA_SCALE = 96 ** -0.5
DIFF_SCALE = 64 ** -0.5
LAM_INIT = 0.8 - 0.6 * math.exp(0.0)
CAP = 640
NST = CAP // 128
HALF = 128 * CAP
BIG = 1.0e6
STAGE = 9
KCUT = int(os.environ.get('KCUT', '99'))


class Buf:
    __slots__ = ("w", "r", "excl")

    def __init__(self, excl=False):
        self.w = []; self.r = []; self.excl = excl


def _prune(toks):
    best = {}
    for t in toks:
        k = (t[0], t[1])
        if k not in best or best[k][2] < t[2]:
            best[k] = t
    return list(best.values())


class Prog:
    def __init__(self, nc, es, nds=48):
        self.nc = nc
        self.ops = {e: [] for e in ENG}
        self.need = {e: set() for e in ENG}
        self.psem = {e: es.enter_context(nc.semaphore("pg_" + e)) for e in ENG}
        self.dsem = [es.enter_context(nc.semaphore("dq%d" % i)) for i in range(nds)]
        self.dcnt = [0] * nds
        self.dn = 0; self.dns = 0; self.NHW = 32
        self.nops = 0; self.limit = int(os.environ.get("KOPS", "100000000"))
        self.pend = {e: [] for e in ENG}

    def _deps(self, eng, reads, writes, waits):
        toks = list(waits) + self.pend[eng]
        self.pend[eng] = []
        for b in reads:
            toks += b.w
            if b.excl:
                toks += [t for t in b.r if not (t[0] == "c" and t[1] == eng)]
        for b in writes:
            toks += b.w; toks += b.r
        res = []
        for t in _prune(toks):
            if t[0] == "c":
                if t[1] == eng and eng == "pe":
                    continue
                self.need[t[1]].add(t[2])
            res.append(t)
        return res

    def _upd(self, tok, reads, writes):
        for b in reads:
            b.r = _prune(b.r + [tok])
        for b in writes:
            b.w = [tok]; b.r = []

    def op(self, eng, fn, reads=(), writes=(), waits=()):
        self.nops += 1
        if self.nops > self.limit:
            return ("d", 0, 0)
        if os.environ.get("KDBG"):
            import inspect
            fr = inspect.stack()[1]; fr2 = inspect.stack()[2]
            print("OP", self.nops, eng, fr.lineno, fr2.lineno)
        deps = self._deps(eng, reads, writes, waits)
        tok = ("c", eng, len(self.ops[eng]))
        self.ops[eng].append((fn, deps, None))
        self._upd(tok, reads, writes)
        return tok

    def dma(self, eng, fn, reads=(), writes=(), waits=()):
        self.nops += 1
        if self.nops > self.limit:
            return ("d", 0, 0)
        if os.environ.get("KDBG"):
            import inspect
            fr = inspect.stack()[1]; fr2 = inspect.stack()[2]
            print("DMA", self.nops, eng, fr.lineno, fr2.lineno)
        if eng == "pool":
            i = self.NHW + self.dns; self.dns = (self.dns + 1) % (len(self.dsem) - self.NHW)
        else:
            i = self.dn; self.dn = (self.dn + 1) % self.NHW
        w = list(waits)
        if self.dcnt[i] > 0:
            w.append(("d", i, self.dcnt[i]))
        deps = self._deps(eng, reads, writes, w)
        self.dcnt[i] += 16
        tok = ("d", i, self.dcnt[i])
        self.ops[eng].append((fn, deps, i))
        self._upd(tok, reads, writes)
        return tok

    def barrier(self):
        toks = []
        for e in ENG:
            for idx in range(len(self.ops[e]) - 1, -1, -1):
                if self.ops[e][idx][2] is None and self.ops[e][idx][0] is not None:
                    toks.append(("c", e, idx))
                    break
        for i, c in enumerate(self.dcnt):
            if c > 0:
                toks.append(("d", i, c))
        for e in ENG:
            self.pend[e] = self.pend[e] + toks

    def check(self):
        rank = {e: {idx: i + 1 for i, idx in enumerate(sorted(self.need[e]))} for e in ENG}
        pc = {e: 0 for e in ENG}; cs = {e: 0 for e in ENG}; ds = [0] * len(self.dsem)
        while True:
            prog = False
            for e in ENG:
                while pc[e] < len(self.ops[e]):
                    fn, deps, di = self.ops[e][pc[e]]
                    ok = True
                    for t in deps:
                        if t[0] == "c":
                            if t[2] not in rank[t[1]] or cs[t[1]] < rank[t[1]][t[2]]:
                                ok = False; break
                        elif ds[t[1]] < t[2]:
                            ok = False; break
                    if not ok:
                        break
                    if di is not None:
                        ds[di] += 16
                    elif pc[e] in rank[e]:
                        cs[e] += 1
                    pc[e] += 1; prog = True
            if not prog:
                break
        stuck = {e: (pc[e], len(self.ops[e]), self.ops[e][pc[e]][1]) for e in ENG if pc[e] < len(self.ops[e])}
        assert not stuck, "DEADLOCK %r" % (stuck,)
        print("sync check ok:", {e: len(self.ops[e]) for e in ENG}, {e: len(self.need[e]) for e in ENG})

    def emit(self, block):
        for e in ENG:
            pass
        self.limit = 10 ** 9
        self.barrier()
        self.op("sp", None)
        print("total ops", self.nops)
        self.check()
        rank = {e: {idx: i + 1 for i, idx in enumerate(sorted(self.need[e]))} for e in ENG}

        def run(e, h):
            waited = {}
            for idx, (fn, deps, di) in enumerate(self.ops[e]):
                for t in deps:
                    if t[0] == "c":
                        sem = self.psem[t[1]]; val = rank[t[1]][t[2]]
                    else:
                        sem = self.dsem[t[1]]; val = t[2]
                    key = (t[0], t[1])
                    if waited.get(key, 0) >= val:
                        continue
                    waited[key] = val
                    h.wait_ge(sem, val)
                if fn is None:
                    continue
                ins = fn(h)
                if di is not None:
                    ins.then_inc(self.dsem[di], 16)
                elif idx in rank[e]:
                    ins.then_inc(self.psem[e], 1)

        @block.tensor
        def _(h): run("pe", h)

        @block.scalar
        def _(h): run("act", h)

        @block.vector
        def _(h): run("dve", h)

        @block.gpsimd
        def _(h): run("pool", h)

        @block.sync
        def _(h): run("sp", h)


def build_program():
    nc = bass.Bass("TRN2", target_bir_lowering=False)
    es = ExitStack()
    P = Prog(nc, es)

    def din(name, shape, dt=F32):
        return nc.dram_tensor(name, list(shape), dt, kind="ExternalInput").ap()

    def dout(name, shape, dt=F32):
        return nc.dram_tensor(name, list(shape), dt, kind="ExternalOutput").ap()

    def dscr(name, shape, dt):
        return nc.dram_tensor(name, list(shape), dt).ap()

    x_all = din("x_all", [8192, 1024]); x_own = din("x_own", [4096, 1024]); x_s = din("x_s", [128, 1024])
    clat = din("clat", [2, 4096, 256]); ckr = din("ckr", [2, 4096, 32])
    cdk = din("cdk", [2, 4096, 512]); cdv = din("cdv", [2, 4096, 512])
    c3 = din("c3", [3, 1024])
    w_ada = din("w_ada", [1024, 6144]); b_ada = din("b_ada", [1, 6144])
    norm_attn = din("norm_attn", [1, 1024]); w_in = din("w_in", [1024, 2080])
    qg = din("qg", [1, 256]); kvg = din("kvg", [1, 256])
    w_uq = din("w_uq", [256, 768]); w_ukv = din("w_ukv", [256, 1024])
    lam4 = din("lam4", [4, 64]); subln = din("subln", [128, 1]); relb = din("relb", [32, 8])
    w_out = din("w_out", [1024, 1024]); norm_ffn = din("norm_ffn", [1, 1024])
    w_router = din("w_router", [1024, 256]); rbias = din("rbias", [1, 256])
    if STAGE > 1:
        w_gu = din("w_gu", [256, 1024, 512]); w_dn = din("w_dn", [256, 256, 1024])
    w_sgu = din("w_sgu", [1024, 512]); w_sdn = din("w_sdn", [256, 1024]); fnorm = din("fnorm", [1, 1024])
    cst = din("cst", [128, 1600]); cst2 = din("cst2", [128, 520]); rope_all = din("rope_all", [8192, 64]); rope_own = din("rope_own", [4096, 64])
    rope_s = din("rope_s", [128, 64])

    o_y = dout("o_y", [4096, 1024]); o_ys = dout("o_ys", [128, 1024])
    o_lat = dout("o_lat", [8192, 256]); o_kpe = dout("o_kpe", [8192, 32])
    o_dk = dout("o_dk", [8192, 512]); o_dv = dout("o_dv", [8192, 512])
    o_lats = dout("o_lats", [128, 256]); o_kpes = dout("o_kpes", [128, 32])
    o_dks = dout("o_dks", [128, 512]); o_dvs = dout("o_dvs", [128, 512])

    modD = dscr("modD", [3, 6144], F32); gD = dscr("gD", [8, 6, 192], F32)
    KTm = dscr("KTm", [8, 96, 8192], BF16); Vm = dscr("Vm", [8, 128, 64, 65], BF16)
    KTd = dscr("KTd", [4, 128, 8192], BF16); Vd = dscr("Vd", [4, 128, 64, 128], BF16)
    QTm = dscr("QTm", [8, 96, 4096], BF16); QTd = dscr("QTd", [4, 2, 128, 4096], BF16)
    sKTm = dscr("sKTm", [2, 8, 96, 4096], BF16); sVm = dscr("sVm", [2, 8, 128, 32, 65], BF16)
    sKTd = dscr("sKTd", [2, 4, 128, 4096], BF16); sVd = dscr("sVd", [2, 4, 128, 32, 128], BF16)
    OT = dscr("OT", [1024, 4224], BF16)
    X1 = dscr("X1", [4224, 1024], F32)
    if STAGE > 1:
        XGs = [dscr("XGa", [HALF, 1024], BF16), dscr("XGb", [HALF, 1024], BF16)]
        YGs = [dscr("YGa", [HALF, 1024], BF16), dscr("YGb", [HALF, 1024], BF16)]

    def sb(name, shape, dt):
        return es.enter_context(nc.sbuf_tensor(name, list(shape), dt))

    regs = {}

    def getbc(e):
        if "bc" not in regs:
            regs["bc"] = e.to_reg(HALF - 1)
        return regs["bc"]

    def finish():
        with nc.Block() as block:
            P.emit(block)
        return nc, es

    CF = sb("CF", [128, 1600], F32); CFb = Buf()
    C2 = sb("C2", [128, 520], F32)
    identb = sb("identb", [128, 128], BF16); Ub = sb("Ub", [128, 128], BF16)
    onesb = sb("onesb", [128, 128], BF16); onesf = sb("onesf", [128, 128], F32)
    mpib = sb("mpib", [128, 64], BF16); cB = Buf()
    identf = CF[:, 0:128]; Jf = CF[:, 128:256]
    M = [sb("M%d" % i, [128, 1024], F32) for i in range(6)]; MB = [Buf() for _ in range(6)]
    gains = sb("gains", [128, 512], F32); gB = Buf()
    Bb = sb("Bb", [128, 8, 384], F32); BbB = Buf()
    lamt = sb("lamt", [128, 8], F32); lamB = Buf()
    nkT = sb("nkT", [128, 8, 128], BF16); ndkT = sb("ndkT", [128, 4, 128], BF16)
    nv = sb("nv", [128, 8, 65], BF16); ndv = sb("ndv", [128, 4, 128], BF16)
    nqT = sb("nqT", [128, 8, 128], BF16); ndqT = sb("ndqT", [128, 2, 4, 128], BF16); nB = Buf()
    didx = sb("didx", [128, 33, 32], I32); gate8 = sb("gate8", [128, 33, 16], F32); dgB = [Buf() for _ in range(33)]
    cntbc = sb("cntbc", [128, 256], F32); cntB = Buf()
    nhalf = sb("nhalf", [128, 8], F32)
    ARN = 35584
    arena = sb("arena", [128, ARN], F32)
    ps = [es.enter_context(nc.psum_tensor("psb%d" % i, [128, 512], F32)) for i in range(8)]
    psB = [Buf(excl=True) for _ in range(8)]
    psn = [0]

    def nextps():
        i = psn[0]; psn[0] = (i + 1) % 8
        return i

    class Arena:
        def __init__(self): self.off = 0

        def reset(self):
            self.off = 0; P.barrier()

        def f32(self, shape):
            n = int(np.prod(shape[1:])); a = arena[:, self.off:self.off + n]; self.off += n
            assert self.off <= ARN, self.off
            if len(shape) == 3:
                a = a.rearrange("p (a b) -> p a b", a=shape[1])
            return a

        def bf(self, shape):
            n = int(np.prod(shape[1:])); nf = (n + 1) // 2
            a = arena[:, self.off:self.off + nf].bitcast(BF16)[:, 0:n]; self.off += nf
            assert self.off <= ARN, self.off
            if len(shape) == 3:
                a = a.rearrange("p (a b) -> p a b", a=shape[1])
            elif len(shape) == 4:
                a = a.rearrange("p (a b c) -> p a b c", a=shape[1], b=shape[2])
            return a

        def i32(self, shape):
            n = int(np.prod(shape[1:])); a = arena[:, self.off:self.off + n].bitcast(I32); self.off += n
            return a

    AR = Arena()

    def rms_ops(src_ap, srcB, D, junk, ssb, tag):
        ss, ssB = ssb
        P.op("pool", lambda e: e.memset(ss, 0.0), writes=[ssB])
        P.op("act", lambda e: e.activation(out=junk, in_=src_ap, func=AF.Square, accum_out=ss), reads=[srcB], writes=[ssB])
        P.op("dve", lambda e: e.tensor_scalar(out=ss, in0=ss, scalar1=1.0 / D, scalar2=EPS, op0=ALU.mult, op1=ALU.add), reads=[ssB], writes=[ssB])
        P.op("pool", lambda e: e.tensor_tensor(out=ss, in0=ss, in1=nhalf[:, 0:1], op=ALU.pow), reads=[ssB], writes=[ssB])

    P.dma("sp", lambda e: e.dma_start(out=CF[:], in_=cst), writes=[CFb])
    P.dma("sp", lambda e: e.dma_start(out=C2[:], in_=cst2), writes=[cB])
    P.op("dve", lambda e: e.tensor_copy(out=identb[:], in_=CF[:, 0:128]), reads=[CFb], writes=[cB])
    P.op("dve", lambda e: e.tensor_copy(out=Ub[:], in_=CF[:, 1472:1600]), reads=[CFb], writes=[cB])
    P.op("dve", lambda e: e.tensor_copy(out=mpib[:], in_=CF[:, 1408:1472]), reads=[CFb], writes=[cB])
    P.op("pool", lambda e: e.memset(onesb[:], 1.0), writes=[cB])
    P.op("pool", lambda e: e.memset(onesf[:], 1.0), writes=[cB])
    P.op("pool", lambda e: e.memset(nv[:], 1.0), writes=[nB])
    P.op("pool", lambda e: e.memset(ndqT[:], 0.0), writes=[nB])
    P.op("pool", lambda e: e.memset(cntbc[:], 0.0), writes=[cntB])
    P.op("pool", lambda e: e.memset(nhalf[:], -0.5), writes=[cB])
    P.dma("sp", lambda e: e.dma_start(out=gains[:, 0:256], in_=qg.broadcast_to([128, 256])), writes=[gB])
    P.dma("sp", lambda e: e.dma_start(out=gains[:, 256:512], in_=kvg.broadcast_to([128, 256])), writes=[gB])
    cT = AR.f32([128, 8, 3]); cTB = Buf()
    modS = AR.f32([128, 6144]); modB = Buf()
    badd = AR.f32([128, 6144]); baB = Buf()
    wad = [AR.f32([128, 8, 512]) for _ in range(2)]; wadB = [Buf(), Buf()]
    for k in range(8):
        P.dma("sp", lambda e, k=k: e.dma_start(out=cT[:, k, :], in_=c3[:, k * 128:(k + 1) * 128].rearrange("c p -> p c"),
                                             allow_slow_non_contiguous=True), writes=[cTB])
    P.op("act", lambda e: e.activation(out=cT, in_=cT, func=AF.Silu), reads=[cTB], writes=[cTB])
    P.dma("sp", lambda e: e.dma_start(out=badd[0:3, :], in_=b_ada.broadcast_to([3, 6144])), writes=[baB])
    for n in range(12):
        wb = wad[n % 2]; wB = wadB[n % 2]
        P.dma("sp", lambda e, wb=wb, n=n: e.dma_start(out=wb, in_=w_ada[:, n * 512:(n + 1) * 512].rearrange("(k p) n -> p k n", p=128)), writes=[wB])
        pi = nextps()

        def f(e, wb=wb, pi=pi):
            for k in range(8):
                ins = e.matmul(ps[pi][0:3, :], lhsT=cT[:, k, :], rhs=wb[:, k, :], start=(k == 0), stop=(k == 7))
            return ins
        P.op("pe", f, reads=[cTB, wB], writes=[psB[pi]])
        P.op("dve", lambda e, pi=pi, n=n: e.tensor_tensor(out=modS[0:3, n * 512:(n + 1) * 512], in0=ps[pi][0:3, :],
                                                         in1=badd[0:3, n * 512:(n + 1) * 512], op=ALU.add),
             reads=[psB[pi], baB], writes=[modB])
    P.dma("sp", lambda e: e.dma_start(out=modD, in_=modS[0:3, :]), reads=[modB])
    P.barrier()
    if KCUT == 0:
        return finish()

    def load_mod(sample, which):
        src = {0: 1, 1: 0, 2: 2, 3: 4, 4: 3, 5: 5}
        for j in which:
            c0 = src[j] * 1024
            if not sample:
                P.dma("sp", lambda e, j=j, c0=c0: e.dma_start(out=M[j][:], in_=modD[0:1, c0:c0 + 1024].broadcast_to([128, 1024])), writes=[MB[j]])
            else:
                P.dma("sp", lambda e, j=j, c0=c0: e.dma_start(out=M[j][0:32, :], in_=modD[1:2, c0:c0 + 1024].broadcast_to([32, 1024])), writes=[MB[j]])
                P.dma("sp", lambda e, j=j, c0=c0: e.dma_start(out=M[j][32:128, :], in_=modD[2:3, c0:c0 + 1024].broadcast_to([96, 1024])), writes=[MB[j]])
            if j in (0, 3):
                gsrc = norm_attn if j == 0 else norm_ffn
                tmp = AR_tmp[:]
                P.dma("sp", lambda e, gsrc=gsrc: e.dma_start(out=tmp, in_=gsrc.broadcast_to([128, 1024])), writes=[tmpB])
                P.op("dve", lambda e, j=j: e.scalar_tensor_tensor(out=M[j][:], in0=M[j][:], scalar=1.0, in1=tmp, op0=ALU.add, op1=ALU.mult),
                     reads=[tmpB, MB[j]], writes=[MB[j]])

    AR_tmp = sb("modtmp", [128, 1024], F32); tmpB = Buf()
    load_mod(False, range(6))

    lv = AR.f32([128, 4, 64]); lvB = Buf()
    P.dma("sp", lambda e: e.dma_start(out=lv, in_=bass.AP(lam4.tensor, 0, [[0, 128], [64, 4], [1, 64]])), writes=[lvB])
    lp = AR.f32([128, 2, 64]); l2 = AR.f32([128, 4]); l2B = Buf()
    P.op("dve", lambda e: e.tensor_tensor(out=lp[:, 0, :], in0=lv[:, 0, :], in1=lv[:, 1, :], op=ALU.mult), reads=[lvB], writes=[l2B])
    P.op("dve", lambda e: e.tensor_tensor(out=lp[:, 1, :], in0=lv[:, 2, :], in1=lv[:, 3, :], op=ALU.mult), reads=[lvB, l2B], writes=[l2B])
    P.op("dve", lambda e: e.tensor_reduce(out=l2[:, 0:2], in_=lp, axis=AX.X, op=ALU.add), reads=[l2B], writes=[l2B])
    P.op("act", lambda e: e.activation(out=l2[:, 2:4], in_=l2[:, 0:2], func=AF.Exp), reads=[l2B], writes=[l2B])
    P.op("dve", lambda e: e.tensor_tensor(out=lamt[:, 0:1], in0=l2[:, 3:4], in1=l2[:, 2:3], op=ALU.subtract), reads=[l2B], writes=[lamB])
    P.op("dve", lambda e: e.tensor_scalar(out=lamt[:, 0:1], in0=lamt[:, 0:1], scalar1=-LAM_INIT, scalar2=None, op0=ALU.add), reads=[lamB], writes=[lamB])
    P.dma("sp", lambda e: e.dma_start(out=lamt[:, 1:2], in_=subln), writes=[lamB])
    P.op("dve", lambda e: e.tensor_scalar(out=lamt[:, 1:2], in0=lamt[:, 1:2], scalar1=(1.0 - LAM_INIT) * math.sqrt(128.0), scalar2=None, op0=ALU.mult),
         reads=[lamB], writes=[lamB])
    rb = AR.f32([128, 8]); rbB = Buf()
    P.dma("sp", lambda e: e.dma_start(out=rb[0:32, :], in_=relb), writes=[rbB])
    gS = AR.f32([128, 1152]); gSB = Buf()
    for half in range(3):
        pi = nextps()
        P.op("pe", lambda e, pi=pi, half=half: e.matmul(ps[pi][0:8, 0:384], lhsT=rb[0:32, :], rhs=CF[0:32, 256 + half * 384:256 + (half + 1) * 384], start=True, stop=True),
             reads=[rbB, CFb], writes=[psB[pi]])
        P.op("dve", lambda e, pi=pi, half=half: e.tensor_copy(out=gS[0:8, half * 384:(half + 1) * 384], in_=ps[pi][0:8, 0:384]), reads=[psB[pi]], writes=[gSB])
    tgd = P.dma("sp", lambda e: e.dma_start(out=gD.rearrange("a b c -> a (b c)"), in_=gS[0:8, :]), reads=[gSB])
    Tp = AR.f32([128, 8, 384]); TpB = Buf()
    for blk in range(6):
        P.dma("sp", lambda e, blk=blk: e.dma_start(out=Tp[:, :, blk * 64:(blk + 1) * 64],
                                                  in_=bass.AP(gD.tensor, blk * 192, [[1, 128], [1152, 8], [1, 64]])), writes=[TpB], waits=[tgd])
    for hm in range(8):
        pi = nextps()
        P.op("pe", lambda e, pi=pi, hm=hm: e.matmul(ps[pi][:, 0:384], lhsT=Jf, rhs=Tp[:, hm, :], start=True, stop=True), reads=[TpB, CFb], writes=[psB[pi]])
        P.op("dve", lambda e, pi=pi, hm=hm: e.tensor_copy(out=Bb[:, hm, :], in_=ps[pi][:, 0:384]), reads=[psB[pi]], writes=[BbB])

    if KCUT == 1:
        return finish()
    AR.reset()
    winb = AR.bf([128, 8, 2080]); wuqb = AR.bf([128, 2, 768]); wukvb = AR.bf([128, 2, 1024]); wB_ = Buf()
    for k in range(8):
        P.dma("pool", lambda e, k=k: e.dma_start(out=winb[:, k, :], in_=w_in[k * 128:(k + 1) * 128, :]), writes=[wB_])
    P.dma("pool", lambda e: e.dma_start(out=wuqb, in_=w_uq.rearrange("(k p) n -> p k n", p=128)), writes=[wB_])
    P.dma("pool", lambda e: e.dma_start(out=wukvb, in_=w_ukv.rearrange("(k p) n -> p k n", p=128)), writes=[wB_])
    NR = 2
    xt = [AR.f32([128, 1024]) for _ in range(NR)]; xtB = [Buf() for _ in range(NR)]
    rp = [AR.f32([128, 64]) for _ in range(NR)]; rpB = [Buf() for _ in range(NR)]
    hf = AR.f32([128, 1024]); hfB = Buf()
    hb = AR.bf([128, 1024]); hbB = Buf()
    junk = AR.bf([128, 1024]); junkB = Buf()
    hT = [AR.bf([128, 8, 128]) for _ in range(NR)]; hTB = [Buf() for _ in range(NR)]
    ssA = AR.f32([128, 8]); ssB_ = [Buf() for _ in range(8)]
    latf = [AR.f32([128, 256]) for _ in range(NR)]; latfB = [Buf() for _ in range(NR)]
    kpef = [AR.f32([128, 32]) for _ in range(NR)]; kpefB = [Buf() for _ in range(NR)]
    rtmp = AR.f32([128, 8, 32]); rtmpB = Buf(); rtmp2 = AR.f32([128, 8, 32])
    dkf = [AR.f32([128, 512]) for _ in range(NR)]; dkfB = [Buf() for _ in range(NR)]
    dvf = [AR.f32([128, 512]) for _ in range(NR)]; dvfB = [Buf() for _ in range(NR)]
    latb = AR.bf([128, 256]); latbB = Buf(); kpeb = AR.bf([128, 32]); kpebB = Buf()
    dkb = AR.bf([128, 512]); dkbB = Buf()
    latT = AR.bf([128, 2, 128]); latTB = Buf()
    kcomb = AR.bf([128, 8, 96]); kcombB = Buf()
    kTst = AR.bf([128, 8, 512]); kTstB = Buf(); dkTst = AR.bf([128, 4, 512]); dkTstB = Buf()
    vst = AR.bf([128, 8, 4, 65]); vstB = Buf(); dvst = AR.bf([128, 4, 4, 128]); dvstB = Buf()
    qnb = AR.bf([128, 256]); qnbB = Buf(); qnT = AR.bf([128, 2, 128]); qnTB = Buf()
    qc = AR.bf([128, 8, 96]); qcB = Buf(); dqb = AR.bf([128, 512]); dqbB = Buf()
    qTst = AR.bf([128, 8, 512]); qTstB = Buf(); dqTst = AR.bf([128, 2, 4, 512]); dqTstB = Buf()
    P.op("pool", lambda e: e.memset(vst, 1.0), writes=[vstB])
    P.op("pool", lambda e: e.memset(dqTst, 0.0), writes=[dqTstB])

    def transposes(src_fn, n, rows, dstB_reads, dst_ap, dstB, eng="dve"):
        pi = nextps(); pb = ps[pi][:].bitcast(BF16)

        def f(e):
            for k in range(n):
                ins = e.transpose(out=pb[0:rows, k * 128:(k + 1) * 128], in_=src_fn(k), identity=identb[:])
            return ins
        P.op("pe", f, reads=dstB_reads + [cB], writes=[psB[pi]])
        src = pb[0:rows, 0:n * 128].rearrange("p (a b) -> p a b", a=n)
        if eng == "act":
            P.op("act", lambda e: e.activation(out=dst_ap, in_=src, func=AF.Copy), reads=[psB[pi]], writes=[dstB])
        else:
            P.op(eng, lambda e: e.tensor_copy(out=dst_ap, in_=src), reads=[psB[pi]], writes=[dstB])

    def norm_h(xa, xB, Aj, Bj):
        rms_ops(xa, xB, 1024, junk, (ssA[:, 0:1], ssB_[0]), "x")
        P.op("dve", lambda e: e.scalar_tensor_tensor(out=hf, in0=xa, scalar=ssA[:, 0:1], in1=M[Aj][:], op0=ALU.mult, op1=ALU.mult),
             reads=[xB, ssB_[0], MB[Aj]], writes=[hfB])
        P.op("pool", lambda e: e.tensor_tensor(out=hb, in0=hf, in1=M[Bj][:], op=ALU.add), reads=[hfB, MB[Bj]], writes=[hbB])

    def rope(eng, dst, srcv, tab, nh, reads, writes):
        cs = tab[:, 0:32].unsqueeze(1).broadcast_to([128, nh, 32])
        s1 = tab[:, 32:48].unsqueeze(1).broadcast_to([128, nh, 16])
        s2 = tab[:, 48:64].unsqueeze(1).broadcast_to([128, nh, 16])
        t1 = rtmp[:, 0:nh, :]; t2 = rtmp2[:, 0:nh, :]
        P.op(eng, lambda e: e.tensor_tensor(out=t1, in0=srcv, in1=cs, op=ALU.mult), reads=reads, writes=[rtmpB])
        P.op(eng, lambda e: e.tensor_tensor(out=t2[:, :, 0:16], in0=srcv[:, :, 16:32], in1=s1, op=ALU.mult), reads=reads + [rtmpB], writes=[rtmpB])
        P.op(eng, lambda e: e.tensor_tensor(out=t2[:, :, 16:32], in0=srcv[:, :, 0:16], in1=s2, op=ALU.mult), reads=reads + [rtmpB], writes=[rtmpB])
        P.op(eng, lambda e: e.tensor_tensor(out=dst, in0=t1, in1=t2, op=ALU.add), reads=[rtmpB], writes=writes)

    def kside_derive(t4, dests, latb_, kpeb_, dkb_, dvb_src, rB):
        kT_dst, dkT_dst, v_dst, dv_dst = dests
        transposes(lambda k: latb_[:, k * 128:(k + 1) * 128], 2, 128, rB, latT, latTB)
        pa = nextps(); pb_ = nextps()

        def f(e):
            for hh, pi in ((0, pa), (1, pb_)):
                for k in range(2):
                    ins = e.matmul(ps[pi][:], lhsT=latT[:, k, :], rhs=wukvb[:, k, hh * 512:(hh + 1) * 512], start=(k == 0), stop=(k == 1))
            return ins
        P.op("pe", f, reads=[latTB, wB_], writes=[psB[pa], psB[pb_]])
        for hh, pi in ((0, pa), (1, pb_)):
            v4 = ps[pi][:].rearrange("p (h c) -> p h c", h=4)
            P.op("dve", lambda e, v4=v4, hh=hh: e.tensor_copy(out=kcomb[:, hh * 4:hh * 4 + 4, 0:64], in_=v4[:, :, 0:64]), reads=[psB[pi]], writes=[kcombB])
            P.op("act", lambda e, v4=v4, hh=hh: e.activation(out=v_dst[:, hh * 4:hh * 4 + 4, t4, 1:65] if v_dst is not None else nv[:, hh * 4:hh * 4 + 4, 1:65],
                                                            in_=v4[:, :, 64:128], func=AF.Copy), reads=[psB[pi]], writes=[vstB])
        P.op("pool", lambda e: e.tensor_copy(out=kcomb[:, :, 64:96], in_=kpeb_.unsqueeze(1).broadcast_to([128, 8, 32])), reads=rB + [kcombB], writes=[kcombB])
        transposes(lambda h: kcomb[:, h, :], 8, 96, [kcombB], kT_dst, kTstB, eng="act")
        transposes(lambda h: dkb_[:, h * 128:(h + 1) * 128], 4, 128, rB, dkT_dst, dkTstB)
        if dvb_src is not None:
            P.op("pool", lambda e: e.tensor_copy(out=dv_dst, in_=dvb_src.rearrange("p (h c) -> p h c", h=4)), reads=rB, writes=[dvstB])

    def flush_k(sidx, q4):
        c0 = q4 * 512
        if sidx is None:
            kd, dkd, vd, dvd = KTm, KTd, Vm, Vd
        else:
            kd, dkd, vd, dvd = sKTm[sidx], sKTd[sidx], sVm[sidx], sVd[sidx]
        P.dma("sp", lambda e: e.dma_start(out=kd[:, :, c0:c0 + 512].rearrange("h d t -> d h t"), in_=kTst[0:96, :, :]), reads=[kTstB])
        P.dma("sp", lambda e: e.dma_start(out=dkd[:, :, c0:c0 + 512].rearrange("h d t -> d h t"), in_=dkTst), reads=[dkTstB])
        P.dma("sp", lambda e: e.dma_start(out=vd[:, :, q4 * 4:q4 * 4 + 4, :].rearrange("h p t c -> p h t c"), in_=vst), reads=[vstB])
        P.dma("sp", lambda e: e.dma_start(out=dvd[:, :, q4 * 4:q4 * 4 + 4, :].rearrange("h p t c -> p h t c"), in_=dvst), reads=[dvstB])

    def proj_tile(xsrc, ropesrc, r, own_cols, kv_cols, outs, t4, sample=False):
        P.dma("sp", lambda e: e.dma_start(out=xt[r], in_=xsrc), writes=[xtB[r]])
        P.dma("sp", lambda e: e.dma_start(out=rp[r], in_=ropesrc), writes=[rpB[r]])
        norm_h(xt[r], xtB[r], 0, 1)
        transposes(lambda k: hb[:, k * 128:(k + 1) * 128], 8, 128, [hbB], hT[r], hTB[r], eng="act")

        def inproj(c0, c1):
            pi = nextps()

            def f(e):
                for k in range(8):
                    ins = e.matmul(ps[pi][:, 0:c1 - c0], lhsT=hT[r][:, k, :], rhs=winb[:, k, c0:c1], start=(k == 0), stop=(k == 7))
                return ins
            P.op("pe", f, reads=[hTB[r], wB_], writes=[psB[pi]])
            return pi
        if kv_cols:
            o_lat_, o_kpe_, o_dk_, o_dv_ = outs
            pi = inproj(256, 544)
            rms_ops(ps[pi][:, 0:256], psB[pi], 256, junk[:, 0:256], (ssA[:, 1:2], ssB_[1]), "kv")
            P.op("dve", lambda e: e.scalar_tensor_tensor(out=latf[r], in0=ps[pi][:, 0:256], scalar=ssA[:, 1:2], in1=gains[:, 256:512], op0=ALU.mult, op1=ALU.mult),
                 reads=[psB[pi], ssB_[1], gB], writes=[latfB[r]])
            P.op("pool", lambda e: e.tensor_copy(out=latb, in_=latf[r]), reads=[latfB[r]], writes=[latbB])
            rope("dve", kpef[r].unsqueeze(1), ps[pi][:, 256:288].unsqueeze(1), rp[r], 1, [psB[pi], rpB[r]], [kpefB[r]])
            P.op("pool", lambda e: e.tensor_copy(out=kpeb, in_=kpef[r]), reads=[kpefB[r]], writes=[kpebB])
            P.dma("sp", lambda e: e.dma_start(out=o_lat_, in_=latf[r]), reads=[latfB[r]])
            P.dma("sp", lambda e: e.dma_start(out=o_kpe_, in_=kpef[r]), reads=[kpefB[r]])
            pk = inproj(1056, 1568)
            P.op("act", lambda e: e.activation(out=dkf[r], in_=ps[pk][:], func=AF.Copy), reads=[psB[pk]], writes=[dkfB[r]])
            P.op("dve", lambda e: e.tensor_copy(out=dkb, in_=ps[pk][:]), reads=[psB[pk]], writes=[dkbB])
            P.dma("sp", lambda e: e.dma_start(out=o_dk_, in_=dkf[r]), reads=[dkfB[r]])
            pv = inproj(1568, 2080)
            P.op("act", lambda e: e.activation(out=dvf[r], in_=ps[pv][:], func=AF.Copy), reads=[psB[pv]], writes=[dvfB[r]])
            dvdst = ndv[:] if sample else dvst[:, :, t4, :]
            P.op("dve", lambda e: e.tensor_copy(out=dvdst, in_=ps[pv][:].rearrange("p (h c) -> p h c", h=4)), reads=[psB[pv]], writes=[dvstB])
            P.dma("sp", lambda e: e.dma_start(out=o_dv_, in_=dvf[r]), reads=[dvfB[r]])
            if sample:
                dests = (nkT[0:96, :, :], ndkT[:], None, None)
            else:
                dests = (kTst[0:96, :, t4 * 128:(t4 + 1) * 128], dkTst[:, :, t4 * 128:(t4 + 1) * 128], vst, None)
            kside_derive(t4, dests, latb, kpeb, dkb, None, [latbB, kpebB, dkbB])
        if own_cols:
            pi = inproj(0, 256)
            rms_ops(ps[pi][:, 0:256], psB[pi], 256, junk[:, 0:256], (ssA[:, 2:3], ssB_[2]), "q")
            P.op("dve", lambda e: e.scalar_tensor_tensor(out=qnb, in0=ps[pi][:, 0:256], scalar=ssA[:, 2:3], in1=gains[:, 0:256], op0=ALU.mult, op1=ALU.mult),
                 reads=[psB[pi], ssB_[2], gB], writes=[qnbB])
            pq = inproj(544, 1056)
            P.op("act", lambda e: e.activation(out=dqb, in_=ps[pq][:], func=AF.Copy), reads=[psB[pq]], writes=[dqbB])
            transposes(lambda k: qnb[:, k * 128:(k + 1) * 128], 2, 128, [qnbB], qnT, qnTB)
            pa = nextps(); pb_ = nextps()

            def f(e):
                for hh, pj in ((0, pa), (1, pb_)):
                    for k in range(2):
                        ins = e.matmul(ps[pj][:, 0:384], lhsT=qnT[:, k, :], rhs=wuqb[:, k, hh * 384:(hh + 1) * 384], start=(k == 0), stop=(k == 1))
                return ins
            P.op("pe", f, reads=[qnTB, wB_], writes=[psB[pa], psB[pb_]])
            for hh, pj in ((0, pa), (1, pb_)):
                v4 = ps[pj][:, 0:384].rearrange("p (h c) -> p h c", h=4)
                P.op("act", lambda e, v4=v4, hh=hh: e.activation(out=qc[:, hh * 4:hh * 4 + 4, 0:64], in_=v4[:, :, 0:64], func=AF.Copy), reads=[psB[pj]], writes=[qcB])
                rope("dve", qc[:, hh * 4:hh * 4 + 4, 64:96], v4[:, :, 64:96], rp[r], 4, [psB[pj], rpB[r]], [qcB])
            if sample:
                qd = nqT[0:96, :, :]
                dq0, dq1 = ndqT[0:64, 0, :, :], ndqT[64:128, 1, :, :]
            else:
                qd = qTst[0:96, :, t4 * 128:(t4 + 1) * 128]
                dq0, dq1 = dqTst[0:64, 0, :, t4 * 128:(t4 + 1) * 128], dqTst[64:128, 1, :, t4 * 128:(t4 + 1) * 128]
            transposes(lambda h: qc[:, h, :], 8, 96, [qcB], qd, qTstB, eng="act")
            pq2 = nextps(); pbq = ps[pq2][:].bitcast(BF16)

            def ftq(e):
                for k in range(4):
                    ins = e.transpose(out=pbq[:, k * 128:(k + 1) * 128], in_=dqb[:, k * 128:(k + 1) * 128], identity=identb[:])
                return ins
            P.op("pe", ftq, reads=[dqbB, cB], writes=[psB[pq2]])
            P.op("dve", lambda e: e.tensor_copy(out=dq0, in_=pbq[0:64, 0:512].rearrange("p (a b) -> p a b", a=4)), reads=[psB[pq2]], writes=[dqTstB])
            P.op("dve", lambda e: e.tensor_copy(out=dq1, in_=pbq[64:128, 0:512].rearrange("p (a b) -> p a b", a=4)), reads=[psB[pq2], dqTstB], writes=[dqTstB])

    for t in range(64):
        if KCUT == 2 and t == 4:
            return finish()
        sl = slice(t * 128, (t + 1) * 128)
        proj_tile(x_all[sl, :], rope_all[sl, :], t % NR, False, True, (o_lat[sl, :], o_kpe[sl, :], o_dk[sl, :], o_dv[sl, :]), t % 4)
        if t % 4 == 3:
            flush_k(None, t // 4)
    if KCUT == 3:
        return finish()
    for t in range(32):
        sl = slice(t * 128, (t + 1) * 128)
        proj_tile(x_own[sl, :], rope_own[sl, :], t % NR, True, False, None, t % 4)
        if t % 4 == 3:
            c0 = (t // 4) * 512
            P.dma("sp", lambda e, c0=c0: e.dma_start(out=QTm[:, :, c0:c0 + 512].rearrange("h d t -> d h t"), in_=qTst[0:96, :, :]), reads=[qTstB])
            for m_ in range(2):
                P.dma("sp", lambda e, c0=c0, m_=m_: e.dma_start(out=QTd[:, m_, :, c0:c0 + 512].rearrange("h d t -> d h t"), in_=dqTst[:, m_, :, :]), reads=[dqTstB])
    if KCUT == 4:
        return finish()
    clb = [AR.bf([128, 256]) for _ in range(2)]; ckb = [AR.bf([128, 32]) for _ in range(2)]
    cdkb = [AR.bf([128, 512]) for _ in range(2)]; cdvb = [AR.bf([128, 512]) for _ in range(2)]
    ccB = [Buf(), Buf()]
    for s in range(2):
        for t in range(32):
            r = t % 2; sl = slice(t * 128, (t + 1) * 128)
            P.dma("pool", lambda e, r=r, s=s, sl=sl: e.dma_start(out=clb[r], in_=clat[s, sl, :]), writes=[ccB[r]])
            P.dma("pool", lambda e, r=r, s=s, sl=sl: e.dma_start(out=ckb[r], in_=ckr[s, sl, :]), writes=[ccB[r]])
            P.dma("pool", lambda e, r=r, s=s, sl=sl: e.dma_start(out=cdkb[r], in_=cdk[s, sl, :]), writes=[ccB[r]])
            P.dma("pool", lambda e, r=r, s=s, sl=sl: e.dma_start(out=cdvb[r], in_=cdv[s, sl, :]), writes=[ccB[r]])
            t4 = t % 4
            dests = (kTst[0:96, :, t4 * 128:(t4 + 1) * 128], dkTst[:, :, t4 * 128:(t4 + 1) * 128], vst, dvst[:, :, t4, :])
            kside_derive(t4, dests, clb[r], ckb[r], cdkb[r], cdvb[r], [ccB[r]])
            if t4 == 3:
                flush_k(s, t // 4)
    if KCUT == 5:
        return finish()
    load_mod(True, (0, 1))
    proj_tile(x_s, rope_s, 0, True, True, (o_lats, o_kpes, o_dks, o_dvs), 0, sample=True)
    if STAGE <= 1:
        return finish()
    if KCUT == 6:
        return finish()
    AR.reset()
    ktb = [AR.bf([128, 8192]) for _ in range(2)]; ktB = [Buf(), Buf()]
    vb = [AR.bf([128, 64, 128]) for _ in range(2)]; vB = [Buf(), Buf()]
    qtb = [AR.bf([128, 2, 4096]) for _ in range(2)]; qtB = [Buf(), Buf()]
    NPT = 4
    pt = [AR.bf([128, 512]) for _ in range(NPT)]; ptB = [Buf() for _ in range(NPT)]
    sbs = [AR.f32([128, 128]) for _ in range(2)]; sbsB = [Buf(), Buf()]
    rl = AR.f32([128, 2, 512]); rlB = Buf()
    bcs = AR.f32([128, 512]); bcsB = Buf()
    o1 = AR.f32([128, 512]); o2 = AR.f32([128, 512]); oB = Buf()
    sq = AR.f32([128, 512]); sqB = Buf()
    otile = [AR.bf([128, 512]) for _ in range(2)]; otB = [Buf(), Buf()]
    zt = AR.bf([128, 1024]); ztB = Buf()
    rings = {"s": 0, "p": 0, "o": 0, "b": 0}

    def ring(name, n):
        i = rings[name]; rings[name] = (i + 1) % n
        return i

    P.op("pool", lambda e: e.memset(zt, 0.0), writes=[ztB])

    def zero_fill():
        for XG in XGs:
            for c in range(HALF // 2048):
                P.dma("sp", lambda e, c=c, XG=XG: e.dma_start(out=XG[c * 2048:(c + 1) * 2048, :].rearrange("(p r) c -> p r c", p=128),
                                                             in_=zt.unsqueeze(1).broadcast_to([128, 16, 1024])), reads=[ztB])

    def load_pass(kind, h, slot, s=None):
        if s is None:
            if kind == "m":
                P.dma("sp", lambda e: e.dma_start(out=ktb[slot][0:96, :], in_=KTm[h]), writes=[ktB[slot]])
                P.dma("sp", lambda e: e.dma_start(out=vb[slot][:, :, 0:65], in_=Vm[h]), writes=[vB[slot]])
                P.dma("sp", lambda e: e.dma_start(out=qtb[slot][0:96, 0, :], in_=QTm[h]), writes=[qtB[slot]])
            else:
                P.dma("sp", lambda e: e.dma_start(out=ktb[slot], in_=KTd[h]), writes=[ktB[slot]])
                P.dma("sp", lambda e: e.dma_start(out=vb[slot], in_=Vd[h]), writes=[vB[slot]])
                P.dma("sp", lambda e: e.dma_start(out=qtb[slot], in_=QTd[h].rearrange("m d t -> d m t")), writes=[qtB[slot]])
        else:
            if kind == "m":
                P.dma("sp", lambda e: e.dma_start(out=ktb[slot][0:96, 0:4096], in_=sKTm[s, h]), writes=[ktB[slot]])
                P.dma("sp", lambda e: e.dma_start(out=vb[slot][:, 0:32, 0:65], in_=sVm[s, h]), writes=[vB[slot]])
            else:
                P.dma("sp", lambda e: e.dma_start(out=ktb[slot][:, 0:4096], in_=sKTd[s, h]), writes=[ktB[slot]])
                P.dma("sp", lambda e: e.dma_start(out=vb[slot][:, 0:32, :], in_=sVd[s, h]), writes=[vB[slot]])

    def fin_mla(acc, h, col0, N):
        P.op("dve", lambda e: e.reciprocal(out=rl[0:1, 0, 0:N], in_=ps[acc][0:1, 0:N]), reads=[psB[acc]], writes=[rlB])
        P.op("pe", lambda e: e.matmul(ps[7][0:65, 0:N], lhsT=onesf[0:1, 0:65], rhs=rl[0:1, 0, 0:N], start=True, stop=True), reads=[rlB, cB], writes=[psB[7]])
        P.op("dve", lambda e: e.tensor_copy(out=bcs[0:65, 0:N], in_=ps[7][0:65, 0:N]), reads=[psB[7]], writes=[bcsB])
        k = ring("o", 2); ot = otile[k]
        P.op("dve", lambda e: e.tensor_tensor(out=ot[0:65, 0:N], in0=ps[acc][0:65, 0:N], in1=bcs[0:65, 0:N], op=ALU.mult), reads=[psB[acc], bcsB], writes=[otB[k]])
        P.dma("sp", lambda e: e.dma_start(out=OT[h * 64:(h + 1) * 64, col0:col0 + N], in_=ot[1:65, 0:N]), reads=[otB[k]], waits=(tz if col0 >= 4096 else ()))

    def fin_diff(h, col0, N):
        P.op("dve", lambda e: e.reciprocal(out=rl[:, 0, 0:N], in_=ps[5][:, 0:N]), reads=[psB[5]], writes=[rlB])
        P.op("dve", lambda e: e.reciprocal(out=rl[:, 1, 0:N], in_=ps[6][:, 0:N]), reads=[psB[6], rlB], writes=[rlB])
        P.op("dve", lambda e: e.tensor_tensor(out=o1[:, 0:N], in0=ps[3][:, 0:N], in1=rl[:, 0, 0:N], op=ALU.mult), reads=[psB[3], rlB], writes=[oB])
        P.op("dve", lambda e: e.tensor_tensor(out=o2[:, 0:N], in0=ps[4][:, 0:N], in1=rl[:, 1, 0:N], op=ALU.mult), reads=[psB[4], rlB, oB], writes=[oB])
        P.op("dve", lambda e: e.scalar_tensor_tensor(out=o1[:, 0:N], in0=o2[:, 0:N], scalar=lamt[:, 0:1], in1=o1[:, 0:N], op0=ALU.mult, op1=ALU.add),
             reads=[oB, lamB], writes=[oB])
        P.op("pool", lambda e: e.tensor_tensor(out=sq[:, 0:N], in0=o1[:, 0:N], in1=o1[:, 0:N], op=ALU.mult), reads=[oB], writes=[sqB])
        P.op("pe", lambda e: e.matmul(ps[7][:, 0:N], lhsT=onesf[:], rhs=sq[:, 0:N], start=True, stop=True), reads=[sqB, cB], writes=[psB[7]])
        P.op("dve", lambda e: e.tensor_scalar(out=bcs[:, 0:N], in0=ps[7][:, 0:N], scalar1=128.0 * EPS, scalar2=None, op0=ALU.add), reads=[psB[7]], writes=[bcsB])
        P.op("pool", lambda e: e.tensor_tensor(out=bcs[:, 0:N], in0=bcs[:, 0:N], in1=nhalf[:, 0:1].broadcast_to([128, N]), op=ALU.pow), reads=[bcsB], writes=[bcsB])
        k = ring("o", 2); ot = otile[k]
        P.op("dve", lambda e: e.scalar_tensor_tensor(out=ot[:, 0:N], in0=o1[:, 0:N], scalar=lamt[:, 1:2], in1=bcs[:, 0:N], op0=ALU.mult, op1=ALU.mult),
             reads=[oB, bcsB, lamB], writes=[otB[k]])
        P.dma("sp", lambda e: e.dma_start(out=OT[512 + h * 128:512 + (h + 1) * 128, col0:col0 + N], in_=ot[:, 0:N]), reads=[otB[k]], waits=(tz if col0 >= 4096 else ()))

    def attn_tiles(kind, h, tiles, q_fn, qB_, col0, N, accs):
        pend = []
        n = len(tiles)
        nm = 1 if kind == "m" else 2

        def pv(item):
            i, tl, pjs = item
            for m in range(nm):
                pj = pjs[m]; c0 = tl["c0"]; rows = tl["rows"]
                first = (i == 0); last = (i == n - 1)
                parts = [(0, rows, c0)]
                for (r0, r1, cc) in parts:
                    vv = tl["v_ap"]
                    if kind == "m":
                        P.op("pe", lambda e, r0=r0, r1=r1, cc=cc, vv=vv, pj=pj, first=first, last=last: e.matmul(
                            ps[accs[0]][0:65, cc:N], lhsT=vv[r0:r1, 0:65], rhs=pt[pj][r0:r1, cc:N], start=first and r0 == 0, stop=last and r1 == rows),
                            reads=[ptB[pj], tl["vB"]], writes=[psB[accs[0]]])
                    else:
                        def f(e, r0=r0, r1=r1, cc=cc, vv=vv, pj=pj, first=first, last=last, m=m):
                            e.matmul(ps[accs[m]][:, cc:N], lhsT=vv[r0:r1, :], rhs=pt[pj][r0:r1, cc:N], start=first and r0 == 0, stop=last and r1 == rows)
                            return e.matmul(ps[accs[2 + m]][:, cc:N], lhsT=onesb[r0:r1, :], rhs=pt[pj][r0:r1, cc:N], start=first and r0 == 0, stop=last and r1 == rows)
                        P.op("pe", f, reads=[ptB[pj], tl["vB"], cB], writes=[psB[accs[m]], psB[accs[2 + m]]])

        for i, tl in enumerate(tiles):
            c0 = tl["c0"]; rows = tl["rows"]; pjs = []
            for m in range(nm):
                sk = ring("s", 3); pj = ring("p", NPT); pjs.append(pj)
                kap = tl["k_ap"](m); qap = q_fn(m, c0)
                P.op("pe", lambda e, sk=sk, kap=kap, qap=qap, c0=c0, rows=rows: e.matmul(ps[sk][0:rows, c0:N], lhsT=kap, rhs=qap, start=True, stop=True),
                     reads=[tl["kB"], qB_], writes=[psB[sk]])
                if kind == "m":
                    P.op("act", lambda e, sk=sk, pj=pj, c0=c0, rows=rows: e.activation(out=pt[pj][0:rows, c0:N], in_=ps[sk][0:rows, c0:N], func=AF.Exp, scale=MLA_SCALE),
                         reads=[psB[sk]], writes=[ptB[pj]])
                else:
                    hm = h * 2 + m; b15 = Bb[0:rows, hm, 128:129]
                    if tl["bias"] is not None:
                        lo, w = tl["bias"]; w = min(w, N - c0)
                        bi = ring("b", 2)
                        P.op("dve", lambda e, sk=sk, bi=bi, c0=c0, w=w, lo=lo, hm=hm, rows=rows: e.scalar_tensor_tensor(
                            out=sbs[bi][0:rows, 0:w], in0=ps[sk][0:rows, c0:c0 + w], scalar=DIFF_SCALE, in1=Bb[0:rows, hm, lo:lo + w], op0=ALU.mult, op1=ALU.add),
                            reads=[psB[sk], BbB], writes=[sbsB[bi]])
                        P.op("act", lambda e, bi=bi, pj=pj, c0=c0, w=w, rows=rows: e.activation(out=pt[pj][0:rows, c0:c0 + w], in_=sbs[bi][0:rows, 0:w], func=AF.Exp),
                             reads=[sbsB[bi]], writes=[ptB[pj]])
                        if c0 + w < N:
                            P.op("act", lambda e, sk=sk, pj=pj, c0=c0, w=w, b15=b15, rows=rows: e.activation(
                                out=pt[pj][0:rows, c0 + w:N], in_=ps[sk][0:rows, c0 + w:N], func=AF.Exp, scale=DIFF_SCALE, bias=b15),
                                reads=[psB[sk], BbB, ptB[pj]], writes=[ptB[pj]])
                    else:
                        P.op("act", lambda e, sk=sk, pj=pj, c0=c0, b15=b15, rows=rows: e.activation(
                            out=pt[pj][0:rows, c0:N], in_=ps[sk][0:rows, c0:N], func=AF.Exp, scale=DIFF_SCALE, bias=b15),
                            reads=[psB[sk], BbB], writes=[ptB[pj]])
                if tl["diag"]:
                    P.op("pool", lambda e, pj=pj, c0=c0: e.tensor_tensor(out=pt[pj][64:128, c0:c0 + 64], in0=pt[pj][64:128, c0:c0 + 64], in1=mpib[64:128, :], op=ALU.mult),
                         reads=[ptB[pj], cB], writes=[ptB[pj]])
                if tl["pmask"] is not None:
                    pm = tl["pmask"]
                    P.op("pool", lambda e, pj=pj, pm=pm, rows=rows: e.tensor_tensor(out=pt[pj][0:rows, 0:N], in0=pt[pj][0:rows, 0:N], in1=pm.broadcast_to([rows, N]), op=ALU.mult),
                         reads=[ptB[pj], cB], writes=[ptB[pj]])
            pend.append((i, tl, pjs))
            if len(pend) > 1:
                pv(pend.pop(0))
        while pend:
            pv(pend.pop(0))

    def prompt_pass(kind, h, slot):
        kt_, v_, q_ = ktb[slot], vb[slot], qtb[slot]
        for G in range(8):
            tiles = []
            for kt in range(8 * G):
                bias = (64, 64) if (kt == 8 * G - 1) else None
                tiles.append(dict(kt=kt, c0=0, rows=128, diag=False, bias=bias, pmask=None))
            for k in range(8):
                tiles.append(dict(kt=8 * G + k, c0=64 * k, rows=128, diag=True, bias=(0, 128), pmask=None))
            for tl in tiles:
                kt = tl["kt"]
                if kind == "m":
                    tl["k_ap"] = (lambda m, kt=kt: kt_[0:96, kt * 128:(kt + 1) * 128])
                    tl["v_ap"] = v_[:, kt, :]
                else:
                    tl["k_ap"] = (lambda m, kt=kt: kt_[:, kt * 128:(kt + 1) * 128])
                    tl["v_ap"] = v_[:, kt, :]
                tl["kB"] = ktB[slot]; tl["vB"] = vB[slot]
            if kind == "m":
                acc = 3 + (G % 2)
                attn_tiles("m", h, tiles, lambda m, c0, G=G: q_[0:96, 0, G * 512 + c0:(G + 1) * 512], qtB[slot], G * 512, 512, [acc])
                fin_mla(acc, h, G * 512, 512)
            else:
                attn_tiles("d", h, tiles, lambda m, c0, G=G: q_[:, m, G * 512 + c0:(G + 1) * 512], qtB[slot], G * 512, 512, [3, 4, 5, 6])
                fin_diff(h, G * 512, 512)

    def sample_pass(kind, h, slot, s):
        kt_, v_ = ktb[slot], vb[slot]
        tiles = []
        for kt in range(32):
            tl = dict(c0=0, rows=128, diag=False, bias=((192, 16) if kt == 31 else None), pmask=None, kB=ktB[slot], vB=vB[slot], v_ap=v_[:, kt, :])
            if kind == "m":
                tl["k_ap"] = (lambda m, kt=kt: kt_[0:96, kt * 128:(kt + 1) * 128])
            else:
                tl["k_ap"] = (lambda m, kt=kt: kt_[:, kt * 128:(kt + 1) * 128])
            tiles.append(tl)
        tl = dict(c0=0, rows=48, diag=False, bias=((256 + 64 * s, 16)), pmask=C2[0:48, 512 + s:513 + s], kB=nB, vB=nB)
        if kind == "m":
            tl["k_ap"] = (lambda m: nkT[0:96, h, 0:48]); tl["v_ap"] = nv[:, h, :]
            qf = lambda m, c0: nqT[0:96, h, s * 32:s * 32 + 16]
        else:
            tl["k_ap"] = (lambda m: ndkT[:, h, 0:48]); tl["v_ap"] = ndv[:, h, :]
            qf = lambda m, c0: ndqT[:, m, h, s * 32:s * 32 + 16]
        tiles.append(tl)
        col0 = 4096 + s * 32
        if kind == "m":
            acc = 3 + (ring("o2", 2) if False else 0)
            attn_tiles("m", h, tiles, qf, nB, col0, 16, [3])
            fin_mla(3, h, col0, 16)
        else:
            attn_tiles("d", h, tiles, qf, nB, col0, 16, [3, 4, 5, 6])
            fin_diff(h, col0, 16)

    passes = [("m", h) for h in range(8)] + [("d", h) for h in range(4)]
    load_pass(passes[0][0], passes[0][1], 0)
    tz = [P.dma("sp", lambda e: e.dma_start(out=OT[:, 4096:4224].rearrange("(k p) t -> p k t", p=128), in_=zt[:, 0:1024].rearrange("p (k t) -> p k t", k=8)), reads=[ztB])]
    zero_fill()
    for i, (kind, h) in enumerate(passes):
        if i + 1 < len(passes):
            load_pass(passes[i + 1][0], passes[i + 1][1], (i + 1) % 2)
        prompt_pass(kind, h, i % 2)
    sp_list = [(kind, h, s) for s in range(2) for (kind, h) in passes]
    load_pass(sp_list[0][0], sp_list[0][1], 0, s=sp_list[0][2])
    for i, (kind, h, s) in enumerate(sp_list):
        if i + 1 < len(sp_list):
            load_pass(sp_list[i + 1][0], sp_list[i + 1][1], (i + 1) % 2, s=sp_list[i + 1][2])
        sample_pass(kind, h, i % 2, s)
    if KCUT == 7:
        return finish()

    AR.reset()
    woutb = AR.bf([128, 8, 1024]); wrb = AR.bf([128, 8, 256]); wsgb = AR.bf([128, 8, 512]); wsdb = AR.bf([128, 2, 1024])
    wDB = [Buf() for _ in range(4)]
    P.dma("pool", lambda e: e.dma_start(out=woutb, in_=w_out.rearrange("(k p) n -> p k n", p=128)), writes=[wDB[0]])
    P.dma("pool", lambda e: e.dma_start(out=wrb, in_=w_router.rearrange("(k p) n -> p k n", p=128)), writes=[wDB[1]])
    P.dma("pool", lambda e: e.dma_start(out=wsgb, in_=w_sgu.rearrange("(k p) n -> p k n", p=128)), writes=[wDB[2]])
    P.dma("pool", lambda e: e.dma_start(out=wsdb, in_=w_sdn.rearrange("(k p) n -> p k n", p=128)), writes=[wDB[3]])
    wrf = AR.f32([128, 8, 256]); wrfB = Buf()
    P.dma("sp", lambda e: e.dma_start(out=wrf, in_=w_router.rearrange("(k p) n -> p k n", p=128)), writes=[wrfB])
    h2ff = AR.f32([128, 1024]); h2ffB = Buf(); h2Tf = AR.f32([128, 8, 128]); h2TfB = Buf()
    rbb = AR.f32([128, 256]); rbbB = Buf()
    P.dma("sp", lambda e: e.dma_start(out=rbb, in_=rbias.broadcast_to([128, 256])), writes=[rbbB])
    oTs = [AR.bf([128, 8, 512]) for _ in range(2)]; oTsB = [Buf(), Buf()]
    xd = [AR.f32([128, 1024]) for _ in range(2)]; xdB = [Buf(), Buf()]
    x1 = AR.f32([128, 1024]); x1B = Buf()
    tmpd = AR.f32([128, 1024]); tmpdB = Buf()
    h2f = AR.f32([128, 1024]); h2fB = Buf()
    h2b = [AR.bf([128, 1024]) for _ in range(2)]; h2bB = [Buf(), Buf()]
    h2T = AR.bf([128, 8, 128]); h2TB = Buf()
    junkd = AR.bf([128, 1024])
    ssD = AR.f32([128, 8]); ssDB = Buf()
    sc = AR.f32([128, 256]); scB = Buf()
    sgd = AR.f32([128, 256]); sgdB = Buf()
    abd = AR.bf([128, 256]); abdB = Buf(); aTd = AR.bf([128, 2, 128]); aTdB = Buf()
    biased = AR.f32([128, 256]); m8 = AR.f32([128, 8, 8]); gs = AR.f32([128, 8]); t8 = AR.f32([128, 8]); gm = AR.f32([128, 8])
    masked = AR.f32([128, 256]); v8 = AR.f32([128, 8]); sel = AR.f32([128, 256]); gsel = AR.f32([128, 256]); den = AR.f32([128, 8])
    Gt = AR.f32([128, 256]); selb = AR.bf([128, 256]); posf = AR.f32([128, 256]); key = AR.f32([128, 256]); d8 = AR.f32([128, 8]); neg = AR.f32([128, 8])
    neg2 = AR.f32([128, 8]); dA = AR.f32([128, 8]); dB = AR.f32([128, 8])
    key2 = AR.f32([128, 256]); key3 = AR.f32([128, 256]); v8b = AR.f32([128, 8]); v8c = AR.f32([128, 8])
    rtB = Buf()
    selbB = Buf()

    def rt(fn, extra_r=(), extra_w=()):
        P.op("dve", fn, reads=[rtB] + list(extra_r), writes=[rtB] + list(extra_w))

    def phaseD_tile(tile):
        sample = (tile == 32)
        r = tile % 2
        if sample:
            P.dma("sp", lambda e: e.dma_start(out=oTs[0][:, :, 0:128], in_=OT[:, 4096:4224].rearrange("(k p) t -> p k t", p=128)), writes=[oTsB[0]])
            oT = oTs[0]; oTB = oTsB[0]; tc0 = 0
            xsrc = x_s
        else:
            G = tile // 4
            if tile % 4 == 0:
                P.dma("sp", lambda e: e.dma_start(out=oTs[G % 2], in_=OT[:, G * 512:(G + 1) * 512].rearrange("(k p) t -> p k t", p=128)), writes=[oTsB[G % 2]])
            oT = oTs[G % 2]; oTB = oTsB[G % 2]; tc0 = (tile % 4) * 128
            xsrc = x_own[tile * 128:(tile + 1) * 128, :]
        P.dma("sp", lambda e: e.dma_start(out=xd[r], in_=xsrc), writes=[xdB[r]])
        pa = nextps(); pb_ = nextps()

        def f(e):
            for nh, pj in ((0, pa), (1, pb_)):
                for k in range(8):
                    ins = e.matmul(ps[pj][:], lhsT=oT[:, k, tc0:tc0 + 128], rhs=woutb[:, k, nh * 512:(nh + 1) * 512], start=(k == 0), stop=(k == 7))
            return ins
        P.op("pe", f, reads=[oTB, wDB[0]], writes=[psB[pa], psB[pb_]])
        for nh, pj in ((0, pa), (1, pb_)):
            P.op("dve", lambda e, nh=nh, pj=pj: e.tensor_tensor(out=tmpd[:, nh * 512:(nh + 1) * 512], in0=ps[pj][:], in1=M[2][:, nh * 512:(nh + 1) * 512], op=ALU.mult),
                 reads=[psB[pj], MB[2]], writes=[tmpdB])
        P.op("pool", lambda e: e.tensor_tensor(out=x1, in0=tmpd, in1=xd[r], op=ALU.add), reads=[tmpdB, xdB[r]], writes=[x1B])
        P.op("pool", lambda e: e.memset(ssD[:, 0:1], 0.0), writes=[ssDB])
        P.op("act", lambda e: e.activation(out=junkd, in_=x1, func=AF.Square, accum_out=ssD[:, 0:1]), reads=[x1B], writes=[ssDB])
        P.op("dve", lambda e: e.tensor_scalar(out=ssD[:, 0:1], in0=ssD[:, 0:1], scalar1=1.0 / 1024, scalar2=EPS, op0=ALU.mult, op1=ALU.add), reads=[ssDB], writes=[ssDB])
        P.op("pool", lambda e: e.tensor_tensor(out=ssD[:, 0:1], in0=ssD[:, 0:1], in1=nhalf[:, 0:1], op=ALU.pow), reads=[ssDB], writes=[ssDB])
        P.op("dve", lambda e: e.scalar_tensor_tensor(out=h2f, in0=x1, scalar=ssD[:, 0:1], in1=M[3][:], op0=ALU.mult, op1=ALU.mult), reads=[x1B, ssDB, MB[3]], writes=[h2fB])
        P.op("pool", lambda e: e.tensor_tensor(out=h2ff, in0=h2f, in1=M[4][:], op=ALU.add), reads=[h2fB, MB[4]], writes=[h2ffB])
        P.op("pool", lambda e: e.tensor_copy(out=h2b[r], in_=h2ff), reads=[h2ffB], writes=[h2bB[r]])
        transposes(lambda k: h2b[r][:, k * 128:(k + 1) * 128], 8, 128, [h2bB[r]], h2T, h2TB, eng="act")
        for hh in range(2):
            pt_ = nextps()

            def ft(e, pt_=pt_, hh=hh):
                for k in range(4):
                    ins = e.transpose(out=ps[pt_][:, k * 128:(k + 1) * 128], in_=h2ff[:, (hh * 4 + k) * 128:(hh * 4 + k + 1) * 128], identity=identf)
                return ins
            P.op("pe", ft, reads=[h2ffB, CFb], writes=[psB[pt_]])
            P.op("dve", lambda e, pt_=pt_, hh=hh: e.tensor_copy(out=h2Tf[:, hh * 4:hh * 4 + 4, :], in_=ps[pt_][:].rearrange("p (a b) -> p a b", a=4)),
                 reads=[psB[pt_]], writes=[h2TfB])
        pr = nextps(); pg = nextps()

        def f2(e):
            for k in range(8):
                e.matmul(ps[pr][:, 0:256], lhsT=h2Tf[:, k, :], rhs=wrf[:, k, :], start=(k == 0), stop=(k == 7))
            for k in range(8):
                ins = e.matmul(ps[pg][:], lhsT=h2T[:, k, :], rhs=wsgb[:, k, :], start=(k == 0), stop=(k == 7))
            return ins
        P.op("pe", f2, reads=[h2TB, h2TfB, wrfB, wDB[2]], writes=[psB[pr], psB[pg]])
        P.op("act", lambda e: e.activation(out=sc, in_=ps[pr][:, 0:256], func=AF.Sigmoid), reads=[psB[pr]], writes=[scB])
        P.op("act", lambda e: e.activation(out=sgd, in_=ps[pg][:, 0:256], func=AF.Silu), reads=[psB[pg]], writes=[sgdB])
        P.op("dve", lambda e: e.tensor_tensor(out=abd, in0=ps[pg][:, 256:512], in1=sgd, op=ALU.mult), reads=[psB[pg], sgdB], writes=[abdB])
        transposes(lambda k: abd[:, k * 128:(k + 1) * 128], 2, 128, [abdB], aTd, aTdB)
        pa2 = nextps(); pb2 = nextps()

        def f3(e):
            for nh, pj in ((0, pa2), (1, pb2)):
                for k in range(2):
                    ins = e.matmul(ps[pj][:], lhsT=aTd[:, k, :], rhs=wsdb[:, k, nh * 512:(nh + 1) * 512], start=(k == 0), stop=(k == 1))
            return ins
        P.op("pe", f3, reads=[aTdB, wDB[3]], writes=[psB[pa2], psB[pb2]])
        for nh, pj in ((0, pa2), (1, pb2)):
            P.op("dve", lambda e, nh=nh, pj=pj: e.tensor_tensor(out=tmpd[:, nh * 512:(nh + 1) * 512], in0=ps[pj][:], in1=M[5][:, nh * 512:(nh + 1) * 512], op=ALU.mult),
                 reads=[psB[pj], MB[5]], writes=[tmpdB])
        P.op("pool", lambda e: e.tensor_tensor(out=x1, in0=tmpd, in1=x1, op=ALU.add), reads=[tmpdB, x1B], writes=[x1B])
        P.dma("sp", lambda e: e.dma_start(out=X1[tile * 128:(tile + 1) * 128, :], in_=x1), reads=[x1B])
        rt(lambda e: e.tensor_tensor(out=biased, in0=sc, in1=rbb, op=ALU.add), extra_r=[scB, rbbB])
        for g in range(8):
            rt(lambda e, g=g: e.max(out=m8[:, g, :], in_=biased[:, g * 32:(g + 1) * 32]))
        rt(lambda e: e.tensor_tensor(out=gs, in0=m8[:, :, 0], in1=m8[:, :, 1], op=ALU.add))
        rt(lambda e: e.max(out=t8, in_=gs))
        rt(lambda e: e.tensor_single_scalar(out=gm, in_=gs, scalar=t8[:, 3:4], op=ALU.is_ge))
        rt(lambda e: e.tensor_scalar(out=gm, in0=gm, scalar1=-1.0, scalar2=1e9, op0=ALU.add, op1=ALU.mult))
        rt(lambda e: e.tensor_tensor(out=masked.rearrange("p (g c) -> p g c", g=8), in0=biased.rearrange("p (g c) -> p g c", g=8),
                                     in1=gm.unsqueeze(2).broadcast_to([128, 8, 32]), op=ALU.add))
        rt(lambda e: e.max(out=v8, in_=masked))
        rt(lambda e: e.tensor_single_scalar(out=sel, in_=masked, scalar=v8[:, 7:8], op=ALU.is_ge))
        vcol = C2[:, 514:515] if sample else C2[:, 515:516]
        rt(lambda e: e.tensor_scalar(out=sel, in0=sel, scalar1=vcol, scalar2=None, op0=ALU.mult), extra_r=[cB])
        rt(lambda e: e.tensor_tensor(out=gsel, in0=sel, in1=sc, op=ALU.mult))
        rt(lambda e: e.tensor_reduce(out=den[:, 0:1], in_=gsel, axis=AX.X, op=ALU.add))
        rt(lambda e: e.tensor_scalar(out=den[:, 0:1], in0=den[:, 0:1], scalar1=1e-20, scalar2=None, op0=ALU.add))
        rt(lambda e: e.reciprocal(out=den[:, 1:2], in_=den[:, 0:1]))
        rt(lambda e: e.tensor_scalar(out=Gt, in0=gsel, scalar1=den[:, 1:2], scalar2=2.5, op0=ALU.mult, op1=ALU.mult))
        P.op("pool", lambda e: e.tensor_copy(out=selb, in_=sel), reads=[rtB], writes=[selbB])
        pp = nextps(); pc = nextps()

        def f4(e):
            e.matmul(ps[pp][:, 0:256], lhsT=Ub[:], rhs=selb, start=True, stop=True)
            return e.matmul(ps[pc][:, 0:256], lhsT=onesb[:], rhs=selb, start=True, stop=True)
        P.op("pe", f4, reads=[selbB, cB], writes=[psB[pp], psB[pc]])
        rt(lambda e: e.tensor_tensor(out=posf, in0=ps[pp][:, 0:256], in1=cntbc[:], op=ALU.add), extra_r=[psB[pp], cntB])
        rt(lambda e: e.tensor_tensor(out=cntbc[:], in0=ps[pc][:, 0:256], in1=cntbc[:], op=ALU.add), extra_r=[psB[pc]], extra_w=[cntB])
        rt(lambda e: e.tensor_single_scalar(out=key, in_=posf, scalar=float(CAP), op=ALU.is_lt))
        rt(lambda e: e.tensor_tensor(out=sel, in0=sel, in1=key, op=ALU.mult))
        rt(lambda e: e.tensor_tensor(out=posf, in0=posf, in1=C2[:, 0:256], op=ALU.add))
        rt(lambda e: e.tensor_tensor(out=key, in0=posf, in1=sel, op=ALU.mult))
        rt(lambda e: e.max(out=d8, in_=key))
        rt(lambda e: e.tensor_scalar(out=d8, in0=d8, scalar1=-1.0, scalar2=None, op0=ALU.add))
        rt(lambda e: e.tensor_single_scalar(out=neg, in_=d8, scalar=0.0, op=ALU.is_lt))
        rt(lambda e: e.tensor_single_scalar(out=neg2, in_=d8, scalar=float(HALF), op=ALU.is_ge))
        rt(lambda e: e.tensor_tensor(out=neg, in0=neg, in1=neg2, op=ALU.add))
        rt(lambda e: e.scalar_tensor_tensor(out=dA, in0=neg, scalar=BIG, in1=d8, op0=ALU.mult, op1=ALU.add))
        rt(lambda e: e.tensor_copy(out=didx[:, tile, 0:8], in_=dA))
        rt(lambda e: e.tensor_scalar(out=dB, in0=neg2, scalar1=-1.0, scalar2=-BIG, op0=ALU.add, op1=ALU.mult))
        rt(lambda e: e.scalar_tensor_tensor(out=dB, in0=d8, scalar=-float(HALF), in1=dB, op0=ALU.add, op1=ALU.add))
        rt(lambda e: e.tensor_copy(out=didx[:, tile, 8:16], in_=dB))
        rt(lambda e: e.tensor_scalar(out=neg, in0=neg, scalar1=-1.0, scalar2=-1.0, op0=ALU.add, op1=ALU.mult))
        rt(lambda e: e.tensor_tensor(out=dA, in0=d8, in1=neg, op=ALU.mult))
        rt(lambda e: e.tensor_copy(out=didx[:, tile, 16:24], in_=dA))
        rt(lambda e: e.scalar_tensor_tensor(out=dB, in0=d8, scalar=-float(HALF), in1=neg2, op0=ALU.add, op1=ALU.mult))
        rt(lambda e: e.tensor_copy(out=didx[:, tile, 24:32], in_=dB), extra_w=[dgB[tile]])
        rt(lambda e: e.tensor_tensor(out=key3, in0=C2[:, 256:512], in1=sel, op=ALU.mult))
        rt(lambda e: e.tensor_tensor(out=key2, in0=Gt, in1=sel, op=ALU.mult))
        rt(lambda e: e.tensor_tensor(out=key2, in0=key2, in1=key3, op=ALU.add))
        rt(lambda e: e.max(out=v8b, in_=key2))
        rt(lambda e: e.max(out=v8c, in_=key3))
        rt(lambda e: e.tensor_tensor(out=v8b, in0=v8b, in1=v8c, op=ALU.subtract))
        rt(lambda e: e.tensor_tensor(out=gate8[:, tile, 0:8], in0=v8b, in1=neg, op=ALU.mult))
        rt(lambda e: e.tensor_tensor(out=gate8[:, tile, 8:16], in0=v8b, in1=neg2, op=ALU.mult), extra_w=[dgB[tile]])
        for j in range(16):
            P.dma("pool", lambda e, j=j: e.indirect_dma_start(out=XGs[j // 8][:, :], out_offset=bass.IndirectOffsetOnAxis(ap=didx[:, tile, j:j + 1], axis=0),
                                                            in_=h2b[r][:, :], in_offset=None, bounds_check=getbc(e), oob_is_err=False),
                  reads=[h2bB[r], dgB[tile]])

    load_mod(False, (0, 1))
    for tile in range(32):
        phaseD_tile(tile)
    load_mod(True, (2, 3, 4, 5))
    phaseD_tile(32)
    if KCUT == 8:
        return finish()

    AR.reset()
    wg = [AR.bf([128, 8, 512]) for _ in range(2)]; wgB = [Buf(), Buf()]
    wd = [AR.bf([128, 2, 1024]) for _ in range(2)]; wdB = [Buf(), Buf()]
    xg = [AR.bf([128, NST, 1024]) for _ in range(2)]; xgB = [Buf(), Buf()]
    xgT = [AR.bf([128, 8, CAP]) for _ in range(2)]; xgTB = [Buf(), Buf()]
    sge = [AR.f32([128, 2, 320]) for _ in range(2)]; sgeB = [Buf(), Buf()]
    aTe = [AR.bf([128, 2, CAP]) for _ in range(2)]; aTeB = [Buf(), Buf()]
    yb = [AR.bf([128, NST, 1024]) for _ in range(2)]; ybB = [Buf(), Buf()]
    HN = CAP // 2

    def e_s1(ex):
        r = ex % 2
        XG = XGs[ex // 128]; row0 = (ex % 128) * CAP
        P.dma("pool", lambda e: e.dma_start(out=wg[r], in_=w_gu[ex].rearrange("(k p) n -> p k n", p=128)), writes=[wgB[r]])
        P.dma("pool", lambda e: e.dma_start(out=wd[r], in_=w_dn[ex].rearrange("(k p) n -> p k n", p=128)), writes=[wdB[r]])
        P.dma("sp", lambda e: e.dma_start(out=xg[r], in_=XG[row0:row0 + CAP, :].rearrange("(s p) c -> p s c", p=128)), writes=[xgB[r]])
        for s_ in range(NST):
            transposes(lambda k, s_=s_: xg[r][:, s_, k * 128:(k + 1) * 128], 8, 128, [xgB[r]], xgT[r][:, :, s_ * 128:(s_ + 1) * 128], xgTB[r],
                       eng=("act" if s_ % 2 == 0 else "dve"))

    def e_s2(ex):
        r = ex % 2
        for nh in range(2):
            pbs = [nextps() for _ in range(4)]

            def f(e, pbs=pbs, nh=nh):
                for c in range(4):
                    for k in range(8):
                        ins = e.matmul(ps[pbs[c]][:, 0:HN], lhsT=wg[r][:, k, c * 128:(c + 1) * 128], rhs=xgT[r][:, k, nh * HN:(nh + 1) * HN], start=(k == 0), stop=(k == 7))
                return ins
            P.op("pe", f, reads=[wgB[r], xgTB[r]], writes=[psB[p_] for p_ in pbs])
            for c in range(2):
                P.op("act", lambda e, c=c, pbs=pbs, nh=nh: e.activation(out=sge[nh][:, c, 0:HN], in_=ps[pbs[c]][:, 0:HN], func=AF.Silu), reads=[psB[pbs[c]]], writes=[sgeB[nh]])
                P.op("dve", lambda e, c=c, pbs=pbs, nh=nh: e.tensor_tensor(out=aTe[r][:, c, nh * HN:(nh + 1) * HN], in0=ps[pbs[2 + c]][:, 0:HN], in1=sge[nh][:, c, 0:HN], op=ALU.mult),
                     reads=[psB[pbs[2 + c]], sgeB[nh]], writes=[aTeB[r]])

    def e_s3(ex):
        r = ex % 2
        YG = YGs[ex // 128]; row0 = (ex % 128) * CAP
        for s_ in range(NST):
            for nh in range(2):
                pj = nextps()

                def f2(e, pj=pj, s_=s_, nh=nh):
                    for k in range(2):
                        ins = e.matmul(ps[pj][:], lhsT=aTe[r][:, k, s_ * 128:(s_ + 1) * 128], rhs=wd[r][:, k, nh * 512:(nh + 1) * 512], start=(k == 0), stop=(k == 1))
                    return ins
                P.op("pe", f2, reads=[aTeB[r], wdB[r]], writes=[psB[pj]])
                if (s_ + nh) % 2 == 0:
                    P.op("act", lambda e, pj=pj, s_=s_, nh=nh: e.activation(out=yb[r][:, s_, nh * 512:(nh + 1) * 512], in_=ps[pj][:], func=AF.Copy), reads=[psB[pj]], writes=[ybB[r]])
                else:
                    P.op("dve", lambda e, pj=pj, s_=s_, nh=nh: e.tensor_copy(out=yb[r][:, s_, nh * 512:(nh + 1) * 512], in_=ps[pj][:]), reads=[psB[pj]], writes=[ybB[r]])
        P.dma("sp", lambda e: e.dma_start(out=YG[row0:row0 + CAP, :].rearrange("(s p) c -> p s c", p=128), in_=yb[r]), reads=[ybB[r]])

    e_s1(0)
    for ex in range(256):
        if ex + 1 < 256:
            e_s1(ex + 1)
        e_s2(ex)
        e_s3(ex)
    if KCUT == 9:
        return finish()

    AR.reset()
    yj = [AR.bf([128, 1024]) for _ in range(16)]; yjB = [Buf() for _ in range(16)]
    accF = AR.f32([128, 1024]); accB = Buf()
    x1f = AR.f32([128, 1024]); x1fB = Buf()
    x2 = AR.f32([128, 1024]); x2B = Buf()
    yo = AR.f32([128, 1024]); yoB = Buf()
    fnb = AR.f32([128, 1024]); fnbB = Buf()
    junkf = AR.bf([128, 1024]); ssF = AR.f32([128, 8]); ssFB = Buf()
    P.dma("sp", lambda e: e.dma_start(out=fnb, in_=fnorm.broadcast_to([128, 1024])), writes=[fnbB])
    for j in range(16):
        P.op("pool", lambda e, j=j: e.memset(yj[j], 0.0), writes=[yjB[j]])

    def phaseF_tile(tile):
        for j in range(16):
            P.dma("pool", lambda e, j=j: e.indirect_dma_start(out=yj[j][:, :], out_offset=None, in_=YGs[j // 8][:, :],
                                                            in_offset=bass.IndirectOffsetOnAxis(ap=didx[:, tile, 16 + j:17 + j], axis=0),
                                                            bounds_check=getbc(e), oob_is_err=False), reads=[dgB[tile]], writes=[yjB[j]])
        P.dma("sp", lambda e: e.dma_start(out=x1f, in_=X1[tile * 128:(tile + 1) * 128, :]), writes=[x1fB])
        P.op("dve", lambda e: e.tensor_scalar(out=accF, in0=yj[0], scalar1=gate8[:, tile, 0:1], scalar2=None, op0=ALU.mult), reads=[yjB[0], dgB[tile]], writes=[accB])
        for j in range(1, 16):
            P.op("dve", lambda e, j=j: e.scalar_tensor_tensor(out=accF, in0=yj[j], scalar=gate8[:, tile, j:j + 1], in1=accF, op0=ALU.mult, op1=ALU.add),
                 reads=[yjB[j], dgB[tile], accB], writes=[accB])
        P.op("dve", lambda e: e.tensor_tensor(out=accF, in0=accF, in1=M[5][:], op=ALU.mult), reads=[accB, MB[5]], writes=[accB])
        P.op("pool", lambda e: e.tensor_tensor(out=x2, in0=accF, in1=x1f, op=ALU.add), reads=[accB, x1fB], writes=[x2B])
        P.op("pool", lambda e: e.memset(ssF[:, 0:1], 0.0), writes=[ssFB])
        P.op("act", lambda e: e.activation(out=junkf, in_=x2, func=AF.Square, accum_out=ssF[:, 0:1]), reads=[x2B], writes=[ssFB])
        P.op("dve", lambda e: e.tensor_scalar(out=ssF[:, 0:1], in0=ssF[:, 0:1], scalar1=1.0 / 1024, scalar2=EPS, op0=ALU.mult, op1=ALU.add), reads=[ssFB], writes=[ssFB])
        P.op("pool", lambda e: e.tensor_tensor(out=ssF[:, 0:1], in0=ssF[:, 0:1], in1=nhalf[:, 0:1], op=ALU.pow), reads=[ssFB], writes=[ssFB])
        P.op("dve", lambda e: e.scalar_tensor_tensor(out=yo, in0=x2, scalar=ssF[:, 0:1], in1=fnb, op0=ALU.mult, op1=ALU.mult), reads=[x2B, ssFB, fnbB], writes=[yoB])
        dst = o_ys if tile == 32 else o_y[tile * 128:(tile + 1) * 128, :]
        P.dma("sp", lambda e: e.dma_start(out=dst, in_=yo), reads=[yoB])

    phaseF_tile(32)
    load_mod(False, (5,))
    for tile in range(32):
        phaseF_tile(tile)
    return finish()


_CACHE = {}


def _bucket(rel):
    rel = np.asarray(rel, np.int64)
    n = np.abs(rel)
    nf = np.maximum(n, 1).astype(np.float32)
    large = 8 + (np.log(nf / np.float32(8)) / np.float32(math.log(128 / 8)) * np.float32(8)).astype(np.int32)
    large = np.minimum(large, 15)
    return np.where(rel > 0, 16, 0) + np.where(n < 8, n, large)


def _consts(pi):
    cst = np.zeros((128, 1600), np.float32)
    cst[:, 0:128] = np.eye(128, dtype=np.float32)
    cst[:, 128:256] = np.eye(128, dtype=np.float32)[::-1]
    offs = [64 * pi, 64 * pi + 128, 64 * pi + 256, 128, 0, 32]
    oh = np.zeros((32, 6, 192), np.float32)
    for b, off in enumerate(offs):
        j = np.arange(192)
        bk = _bucket(127 - j - off)
        oh[bk, b, j] = 1.0
    cst[0:32, 256:1408] = oh.reshape(32, 1152)
    cst[0:64, 1408:1472] = 1.0
    cst[64:128, 1408:1472] = float(pi)
    cst[:, 1472:1600] = np.triu(np.ones((128, 128), np.float32), 1)
    return cst


def _consts2():
    c = np.zeros((128, 520), np.float32)
    e = np.arange(256, dtype=np.float32)
    c[:, 0:256] = e * CAP + 1.0
    c[:, 256:512] = 4.0 * e + 1.0
    c[0:16, 512] = 1.0; c[32:48, 513] = 1.0
    c[0:16, 514] = 1.0; c[32:48, 514] = 1.0
    c[:, 515] = 1.0
    return c


def _rope_tab(pos):
    half = 16
    inv = (10000.0 ** (-np.arange(half, dtype=np.float32) / half)).astype(np.float32)
    ang = pos.astype(np.float32)[:, None] * inv
    c = np.cos(ang).astype(np.float32); s = np.sin(ang).astype(np.float32)
    return np.concatenate([c, c, -s, s], axis=1).astype(np.float32)


def _in_maps(inp, cores=range(8)):
    f = lambda a: np.ascontiguousarray(np.asarray(a, dtype=np.float32))
    xp = f(inp["x_prompt"]); xs = f(inp["x_sample"])
    in_maps = []
    for c in cores:
        b, pi = c // 2, c % 2
        xo = xp[b].reshape(64, 2, 64, 1024)[:, pi].reshape(4096, 1024)
        pos_own = (np.arange(64)[:, None] * 128 + pi * 64 + np.arange(64)[None, :]).reshape(-1)
        x_s = np.zeros((128, 1024), np.float32); x_s[0:16] = xs[2 * c]; x_s[32:48] = xs[2 * c + 1]
        rs = np.zeros((128, 64), np.float32); rs[:, 0:32] = 1.0
        rs[0:16] = _rope_tab(4096 + np.arange(16)); rs[32:48] = rs[0:16]
        m = {
            "x_all": xp[b], "x_own": f(xo), "x_s": x_s,
            "clat": f(inp["cache_mla_latent"][0, 2 * c:2 * c + 2]), "ckr": f(inp["cache_mla_krope"][0, 2 * c:2 * c + 2]),
            "cdk": f(inp["cache_diff_k"][0, 2 * c:2 * c + 2]).reshape(2, 4096, 512),
            "cdv": f(inp["cache_diff_v"][0, 2 * c:2 * c + 2]).reshape(2, 4096, 512),
            "c3": f(np.stack([inp["c_prompt"][b], inp["c_sample"][2 * c], inp["c_sample"][2 * c + 1]])),
            "w_ada": f(inp["w_ada"][0]), "b_ada": f(inp["b_ada"]), "norm_attn": f(inp["norm_attn"]), "w_in": f(inp["w_in"][0]),
            "qg": f(inp["mla_q_norm"]), "kvg": f(inp["mla_kv_norm"]), "w_uq": f(inp["w_uq"][0]), "w_ukv": f(inp["w_ukv"][0]),
            "lam4": f(np.concatenate([inp["lambda_q1"], inp["lambda_k1"], inp["lambda_q2"], inp["lambda_k2"]], 0)),
            "subln": f(inp["diff_subln"]).reshape(128, 1), "relb": f(inp["rel_bias"]).reshape(32, 8),
            "w_out": f(inp["w_out"][0]), "norm_ffn": f(inp["norm_ffn"]), "w_router": f(inp["w_router"][0]), "rbias": f(inp["router_bias"]),
            "w_sgu": f(inp["w_shared_gu"][0]), "w_sdn": f(inp["w_shared_down"][0]),
            "fnorm": f(inp["final_norm"]).reshape(1, 1024),
            "cst": _consts(pi), "cst2": _consts2(), "rope_all": _rope_tab(np.arange(8192)), "rope_own": _rope_tab(pos_own), "rope_s": rs,
        }
        if STAGE > 1:
            m["w_gu"] = f(inp["w_exp_gu"][0]); m["w_dn"] = f(inp["w_exp_down"][0])
        in_maps.append(m)
    return in_maps


def kernel(**inp):
    if "prog" not in _CACHE:
        _CACHE["prog"] = build_program()
    nc, _es = _CACHE["prog"]
    in_maps = _in_maps(inp)
    res = run_bass_kernel_spmd(nc, in_maps, core_ids=list(range(8))).results
    y_p = np.zeros((4, 8192, 1024), np.float32); y_s = np.zeros((16, 16, 1024), np.float32)
    lat_p = np.zeros((1, 4, 8192, 256), np.float32); kpe_p = np.zeros((1, 4, 8192, 32), np.float32)
    dk_p = np.zeros((1, 4, 8192, 4, 2, 64), np.float32); dv_p = np.zeros((1, 4, 8192, 4, 128), np.float32)
    lat_s = np.zeros((1, 16, 16, 256), np.float32); kpe_s = np.zeros((1, 16, 16, 32), np.float32)
    dk_s = np.zeros((1, 16, 16, 4, 2, 64), np.float32); dv_s = np.zeros((1, 16, 16, 4, 128), np.float32)
    for c in range(8):
        b, pi = c // 2, c % 2
        r = res[c]
        y_p[b].reshape(64, 2, 64, 1024)[:, pi] = r["o_y"].reshape(64, 64, 1024)
        if pi == 0:
            lat_p[0, b] = r["o_lat"]; kpe_p[0, b] = r["o_kpe"]
            dk_p[0, b] = r["o_dk"].reshape(8192, 4, 2, 64); dv_p[0, b] = r["o_dv"].reshape(8192, 4, 128)
        for s in range(2):
            sl = slice(32 * s, 32 * s + 16)
            y_s[2 * c + s] = r["o_ys"][sl]
            lat_s[0, 2 * c + s] = r["o_lats"][sl]; kpe_s[0, 2 * c + s] = r["o_kpes"][sl]
            dk_s[0, 2 * c + s] = r["o_dks"][sl].reshape(16, 4, 2, 64); dv_s[0, 2 * c + s] = r["o_dvs"][sl].reshape(16, 4, 128)
    return (y_p, y_s, lat_p, kpe_p, dk_p, dv_p, lat_s, kpe_s, dk_s, dv_s)
```

```python
import math
import os
from contextlib import ExitStack
import numpy as np
import concourse.bass as bass
import concourse.mybir as mybir
from concourse.bass_utils import run_bass_kernel_spmd

F32 = mybir.dt.float32; BF16 = mybir.dt.bfloat16; I32 = mybir.dt.int32
ALU = mybir.AluOpType; AF = mybir.ActivationFunctionType; AX = mybir.AxisListType
ENG = ("pe", "act", "dve", "pool", "sp")
EPS = 1e-6
MLA_SCALE = 96 ** -0.5
DIFF_SCALE = 64 ** -0.5
LAM_INIT = 0.8 - 0.6 * math.exp(0.0)
CAP = 640
NST = CAP // 128
HALF = 128 * CAP
BIG = 1.0e6
STAGE = 9
KCUT = int(os.environ.get('KCUT', '99'))


class Buf:
    __slots__ = ("w", "r", "excl")

    def __init__(self, excl=False):
        self.w = []; self.r = []; self.excl = excl


def _prune(toks):
    best = {}
    for t in toks:
        k = (t[0], t[1])
        if k not in best or best[k][2] < t[2]:
            best[k] = t
    return list(best.values())


class Prog:
    def __init__(self, nc, es, nds=48):
        self.nc = nc
        self.ops = {e: [] for e in ENG}
        self.need = {e: set() for e in ENG}
        self.psem = {e: es.enter_context(nc.semaphore("pg_" + e)) for e in ENG}
        self.dsem = [es.enter_context(nc.semaphore("dq%d" % i)) for i in range(nds)]
        self.dcnt = [0] * nds
        self.dn = 0; self.dns = 0; self.NHW = 32
        self.nops = 0; self.limit = int(os.environ.get("KOPS", "100000000"))
        self.pend = {e: [] for e in ENG}

    def _deps(self, eng, reads, writes, waits):
        toks = list(waits) + self.pend[eng]
        self.pend[eng] = []
        for b in reads:
            toks += b.w
            if b.excl:
                toks += [t for t in b.r if not (t[0] == "c" and t[1] == eng)]
        for b in writes:
            toks += b.w; toks += b.r
        res = []
        for t in _prune(toks):
            if t[0] == "c":
                if t[1] == eng and eng == "pe":
                    continue
                self.need[t[1]].add(t[2])
            res.append(t)
        return res

    def _upd(self, tok, reads, writes):
        for b in reads:
            b.r = _prune(b.r + [tok])
        for b in writes:
            b.w = [tok]; b.r = []

    def op(self, eng, fn, reads=(), writes=(), waits=()):
        self.nops += 1
        if self.nops > self.limit:
            return ("d", 0, 0)
        if os.environ.get("KDBG"):
            import inspect
            fr = inspect.stack()[1]; fr2 = inspect.stack()[2]
            print("OP", self.nops, eng, fr.lineno, fr2.lineno)
        deps = self._deps(eng, reads, writes, waits)
        tok = ("c", eng, len(self.ops[eng]))
        self.ops[eng].append((fn, deps, None))
        self._upd(tok, reads, writes)
        return tok

    def dma(self, eng, fn, reads=(), writes=(), waits=()):
        self.nops += 1
        if self.nops > self.limit:
            return ("d", 0, 0)
        if os.environ.get("KDBG"):
            import inspect
            fr = inspect.stack()[1]; fr2 = inspect.stack()[2]
            print("DMA", self.nops, eng, fr.lineno, fr2.lineno)
        if eng == "pool":
            i = self.NHW + self.dns; self.dns = (self.dns + 1) % (len(self.dsem) - self.NHW)
        else:
            i = self.dn; self.dn = (self.dn + 1) % self.NHW
        w = list(waits)
        if self.dcnt[i] > 0:
            w.append(("d", i, self.dcnt[i]))
        deps = self._deps(eng, reads, writes, w)
        self.dcnt[i] += 16
        tok = ("d", i, self.dcnt[i])
        self.ops[eng].append((fn, deps, i))
        self._upd(tok, reads, writes)
        return tok

    def barrier(self):
        toks = []
        for e in ENG:
            for idx in range(len(self.ops[e]) - 1, -1, -1):
                if self.ops[e][idx][2] is None and self.ops[e][idx][0] is not None:
                    toks.append(("c", e, idx))
                    break
        for i, c in enumerate(self.dcnt):
            if c > 0:
                toks.append(("d", i, c))
        for e in ENG:
            self.pend[e] = self.pend[e] + toks

    def check(self):
        rank = {e: {idx: i + 1 for i, idx in enumerate(sorted(self.need[e]))} for e in ENG}
        pc = {e: 0 for e in ENG}; cs = {e: 0 for e in ENG}; ds = [0] * len(self.dsem)
        while True:
            prog = False
            for e in ENG:
                while pc[e] < len(self.ops[e]):
                    fn, deps, di = self.ops[e][pc[e]]
                    ok = True
                    for t in deps:
                        if t[0] == "c":
                            if t[2] not in rank[t[1]] or cs[t[1]] < rank[t[1]][t[2]]:
                                ok = False; break
                        elif ds[t[1]] < t[2]:
                            ok = False; break
                    if not ok:
                        break
                    if di is not None:
                        ds[di] += 16
                    elif pc[e] in rank[e]:
                        cs[e] += 1
                    pc[e] += 1; prog = True
            if not prog:
                break
        stuck = {e: (pc[e], len(self.ops[e]), self.ops[e][pc[e]][1]) for e in ENG if pc[e] < len(self.ops[e])}
        assert not stuck, "DEADLOCK %r" % (stuck,)
        print("sync check ok:", {e: len(self.ops[e]) for e in ENG}, {e: len(self.need[e]) for e in ENG})

    def emit(self, block):
        for e in ENG:
            pass
        self.limit = 10 ** 9
        self.barrier()
        self.op("sp", None)
        print("total ops", self.nops)
        self.check()
        rank = {e: {idx: i + 1 for i, idx in enumerate(sorted(self.need[e]))} for e in ENG}

        def run(e, h):
            waited = {}
            for idx, (fn, deps, di) in enumerate(self.ops[e]):
                for t in deps:
                    if t[0] == "c":
                        sem = self.psem[t[1]]; val = rank[t[1]][t[2]]
                    else:
                        sem = self.dsem[t[1]]; val = t[2]
                    key = (t[0], t[1])
                    if waited.get(key, 0) >= val:
                        continue
                    waited[key] = val
                    h.wait_ge(sem, val)
                if fn is None:
                    continue
                ins = fn(h)
                if di is not None:
                    ins.then_inc(self.dsem[di], 16)
                elif idx in rank[e]:
                    ins.then_inc(self.psem[e], 1)

        @block.tensor
        def _(h): run("pe", h)

        @block.scalar
        def _(h): run("act", h)

        @block.vector
        def _(h): run("dve", h)

        @block.gpsimd
        def _(h): run("pool", h)

        @block.sync
        def _(h): run("sp", h)


def build_program():
    nc = bass.Bass("TRN2", target_bir_lowering=False)
    es = ExitStack()
    P = Prog(nc, es)

    def din(name, shape, dt=F32):
        return nc.dram_tensor(name, list(shape), dt, kind="ExternalInput").ap()

    def dout(name, shape, dt=F32):
        return nc.dram_tensor(name, list(shape), dt, kind="ExternalOutput").ap()

    def dscr(name, shape, dt):
        return nc.dram_tensor(name, list(shape), dt).ap()

    x_all = din("x_all", [8192, 1024]); x_own = din("x_own", [4096, 1024]); x_s = din("x_s", [128, 1024])
    clat = din("clat", [2, 4096, 256]); ckr = din("ckr", [2, 4096, 32])
    cdk = din("cdk", [2, 4096, 512]); cdv = din("cdv", [2, 4096, 512])
    c3 = din("c3", [3, 1024])
    w_ada = din("w_ada", [1024, 6144]); b_ada = din("b_ada", [1, 6144])
    norm_attn = din("norm_attn", [1, 1024]); w_in = din("w_in", [1024, 2080])
    qg = din("qg", [1, 256]); kvg = din("kvg", [1, 256])
    w_uq = din("w_uq", [256, 768]); w_ukv = din("w_ukv", [256, 1024])
    lam4 = din("lam4", [4, 64]); subln = din("subln", [128, 1]); relb = din("relb", [32, 8])
    w_out = din("w_out", [1024, 1024]); norm_ffn = din("norm_ffn", [1, 1024])
    w_router = din("w_router", [1024, 256]); rbias = din("rbias", [1, 256])
    if STAGE > 1:
        w_gu = din("w_gu", [256, 1024, 512]); w_dn = din("w_dn", [256, 256, 1024])
    w_sgu = din("w_sgu", [1024, 512]); w_sdn = din("w_sdn", [256, 1024]); fnorm = din("fnorm", [1, 1024])
    cst = din("cst", [128, 1600]); cst2 = din("cst2", [128, 520]); rope_all = din("rope_all", [8192, 64]); rope_own = din("rope_own", [4096, 64])
    rope_s = din("rope_s", [128, 64])

    o_y = dout("o_y", [4096, 1024]); o_ys = dout("o_ys", [128, 1024])
    o_lat = dout("o_lat", [8192, 256]); o_kpe = dout("o_kpe", [8192, 32])
    o_dk = dout("o_dk", [8192, 512]); o_dv = dout("o_dv", [8192, 512])
    o_lats = dout("o_lats", [128, 256]); o_kpes = dout("o_kpes", [128, 32])
    o_dks = dout("o_dks", [128, 512]); o_dvs = dout("o_dvs", [128, 512])

    modD = dscr("modD", [3, 6144], F32); gD = dscr("gD", [8, 6, 192], F32)
    KTm = dscr("KTm", [8, 96, 8192], BF16); Vm = dscr("Vm", [8, 128, 64, 65], BF16)
    KTd = dscr("KTd", [4, 128, 8192], BF16); Vd = dscr("Vd", [4, 128, 64, 128], BF16)
    QTm = dscr("QTm", [8, 96, 4096], BF16); QTd = dscr("QTd", [4, 2, 128, 4096], BF16)
    sKTm = dscr("sKTm", [2, 8, 96, 4096], BF16); sVm = dscr("sVm", [2, 8, 128, 32, 65], BF16)
    sKTd = dscr("sKTd", [2, 4, 128, 4096], BF16); sVd = dscr("sVd", [2, 4, 128, 32, 128], BF16)
    OT = dscr("OT", [1024, 4224], BF16)
    X1 = dscr("X1", [4224, 1024], F32)
    if STAGE > 1:
        XGs = [dscr("XGa", [HALF, 1024], BF16), dscr("XGb", [HALF, 1024], BF16)]
        YGs = [dscr("YGa", [HALF, 1024], BF16), dscr("YGb", [HALF, 1024], BF16)]

    def sb(name, shape, dt):
        return es.enter_context(nc.sbuf_tensor(name, list(shape), dt))

    regs = {}

    def getbc(e):
        if "bc" not in regs:
            regs["bc"] = e.to_reg(HALF - 1)
        return regs["bc"]

    def finish():
        with nc.Block() as block:
            P.emit(block)
        return nc, es

    CF = sb("CF", [128, 1600], F32); CFb = Buf()
    C2 = sb("C2", [128, 520], F32)
    identb = sb("identb", [128, 128], BF16); Ub = sb("Ub", [128, 128], BF16)
    onesb = sb("onesb", [128, 128], BF16); onesf = sb("onesf", [128, 128], F32)
    mpib = sb("mpib", [128, 64], BF16); cB = Buf()
    identf = CF[:, 0:128]; Jf = CF[:, 128:256]
    M = [sb("M%d" % i, [128, 1024], F32) for i in range(6)]; MB = [Buf() for _ in range(6)]
    gains = sb("gains", [128, 512], F32); gB = Buf()
    Bb = sb("Bb", [128, 8, 384], F32); BbB = Buf()
    lamt = sb("lamt", [128, 8], F32); lamB = Buf()
    nkT = sb("nkT", [128, 8, 128], BF16); ndkT = sb("ndkT", [128, 4, 128], BF16)
    nv = sb("nv", [128, 8, 65], BF16); ndv = sb("ndv", [128, 4, 128], BF16)
    nqT = sb("nqT", [128, 8, 128], BF16); ndqT = sb("ndqT", [128, 2, 4, 128], BF16); nB = Buf()
    didx = sb("didx", [128, 33, 32], I32); gate8 = sb("gate8", [128, 33, 16], F32); dgB = [Buf() for _ in range(33)]
    cntbc = sb("cntbc", [128, 256], F32); cntB = Buf()
    nhalf = sb("nhalf", [128, 8], F32)
    ARN = 35584
    arena = sb("arena", [128, ARN], F32)
    ps = [es.enter_context(nc.psum_tensor("psb%d" % i, [128, 512], F32)) for i in range(8)]
    psB = [Buf(excl=True) for _ in range(8)]
    psn = [0]

    def nextps():
        i = psn[0]; psn[0] = (i + 1) % 8
        return i

    class Arena:
        def __init__(self): self.off = 0

        def reset(self):
            self.off = 0; P.barrier()

        def f32(self, shape):
            n = int(np.prod(shape[1:])); a = arena[:, self.off:self.off + n]; self.off += n
            assert self.off <= ARN, self.off
            if len(shape) == 3:
                a = a.rearrange("p (a b) -> p a b", a=shape[1])
            return a

        def bf(self, shape):
            n = int(np.prod(shape[1:])); nf = (n + 1) // 2
            a = arena[:, self.off:self.off + nf].bitcast(BF16)[:, 0:n]; self.off += nf
            assert self.off <= ARN, self.off
            if len(shape) == 3:
                a = a.rearrange("p (a b) -> p a b", a=shape[1])
            elif len(shape) == 4:
                a = a.rearrange("p (a b c) -> p a b c", a=shape[1], b=shape[2])
            return a

        def i32(self, shape):
            n = int(np.prod(shape[1:])); a = arena[:, self.off:self.off + n].bitcast(I32); self.off += n
            return a

    AR = Arena()

    def rms_ops(src_ap, srcB, D, junk, ssb, tag):
        ss, ssB = ssb
        P.op("pool", lambda e: e.memset(ss, 0.0), writes=[ssB])
        P.op("act", lambda e: e.activation(out=junk, in_=src_ap, func=AF.Square, accum_out=ss), reads=[srcB], writes=[ssB])
        P.op("dve", lambda e: e.tensor_scalar(out=ss, in0=ss, scalar1=1.0 / D, scalar2=EPS, op0=ALU.mult, op1=ALU.add), reads=[ssB], writes=[ssB])
        P.op("pool", lambda e: e.tensor_tensor(out=ss, in0=ss, in1=nhalf[:, 0:1], op=ALU.pow), reads=[ssB], writes=[ssB])

    P.dma("sp", lambda e: e.dma_start(out=CF[:], in_=cst), writes=[CFb])
    P.dma("sp", lambda e: e.dma_start(out=C2[:], in_=cst2), writes=[cB])
    P.op("dve", lambda e: e.tensor_copy(out=identb[:], in_=CF[:, 0:128]), reads=[CFb], writes=[cB])
    P.op("dve", lambda e: e.tensor_copy(out=Ub[:], in_=CF[:, 1472:1600]), reads=[CFb], writes=[cB])
    P.op("dve", lambda e: e.tensor_copy(out=mpib[:], in_=CF[:, 1408:1472]), reads=[CFb], writes=[cB])
    P.op("pool", lambda e: e.memset(onesb[:], 1.0), writes=[cB])
    P.op("pool", lambda e: e.memset(onesf[:], 1.0), writes=[cB])
    P.op("pool", lambda e: e.memset(nv[:], 1.0), writes=[nB])
    P.op("pool", lambda e: e.memset(ndqT[:], 0.0), writes=[nB])
    P.op("pool", lambda e: e.memset(cntbc[:], 0.0), writes=[cntB])
    P.op("pool", lambda e: e.memset(nhalf[:], -0.5), writes=[cB])
    P.dma("sp", lambda e: e.dma_start(out=gains[:, 0:256], in_=qg.broadcast_to([128, 256])), writes=[gB])
    P.dma("sp", lambda e: e.dma_start(out=gains[:, 256:512], in_=kvg.broadcast_to([128, 256])), writes=[gB])
    cT = AR.f32([128, 8, 3]); cTB = Buf()
    modS = AR.f32([128, 6144]); modB = Buf()
    badd = AR.f32([128, 6144]); baB = Buf()
    wad = [AR.f32([128, 8, 512]) for _ in range(2)]; wadB = [Buf(), Buf()]
    for k in range(8):
        P.dma("sp", lambda e, k=k: e.dma_start(out=cT[:, k, :], in_=c3[:, k * 128:(k + 1) * 128].rearrange("c p -> p c"),
                                             allow_slow_non_contiguous=True), writes=[cTB])
    P.op("act", lambda e: e.activation(out=cT, in_=cT, func=AF.Silu), reads=[cTB], writes=[cTB])
    P.dma("sp", lambda e: e.dma_start(out=badd[0:3, :], in_=b_ada.broadcast_to([3, 6144])), writes=[baB])
    for n in range(12):
        wb = wad[n % 2]; wB = wadB[n % 2]
        P.dma("sp", lambda e, wb=wb, n=n: e.dma_start(out=wb, in_=w_ada[:, n * 512:(n + 1) * 512].rearrange("(k p) n -> p k n", p=128)), writes=[wB])
        pi = nextps()

        def f(e, wb=wb, pi=pi):
            for k in range(8):
                ins = e.matmul(ps[pi][0:3, :], lhsT=cT[:, k, :], rhs=wb[:, k, :], start=(k == 0), stop=(k == 7))
            return ins
        P.op("pe", f, reads=[cTB, wB], writes=[psB[pi]])
        P.op("dve", lambda e, pi=pi, n=n: e.tensor_tensor(out=modS[0:3, n * 512:(n + 1) * 512], in0=ps[pi][0:3, :],
                                                         in1=badd[0:3, n * 512:(n + 1) * 512], op=ALU.add),
             reads=[psB[pi], baB], writes=[modB])
    P.dma("sp", lambda e: e.dma_start(out=modD, in_=modS[0:3, :]), reads=[modB])
    P.barrier()
    if KCUT == 0:
        return finish()

    def load_mod(sample, which):
        src = {0: 1, 1: 0, 2: 2, 3: 4, 4: 3, 5: 5}
        for j in which:
            c0 = src[j] * 1024
            if not sample:
                P.dma("sp", lambda e, j=j, c0=c0: e.dma_start(out=M[j][:], in_=modD[0:1, c0:c0 + 1024].broadcast_to([128, 1024])), writes=[MB[j]])
            else:
                P.dma("sp", lambda e, j=j, c0=c0: e.dma_start(out=M[j][0:32, :], in_=modD[1:2, c0:c0 + 1024].broadcast_to([32, 1024])), writes=[MB[j]])
                P.dma("sp", lambda e, j=j, c0=c0: e.dma_start(out=M[j][32:128, :], in_=modD[2:3, c0:c0 + 1024].broadcast_to([96, 1024])), writes=[MB[j]])
            if j in (0, 3):
                gsrc = norm_attn if j == 0 else norm_ffn
                tmp = AR_tmp[:]
                P.dma("sp", lambda e, gsrc=gsrc: e.dma_start(out=tmp, in_=gsrc.broadcast_to([128, 1024])), writes=[tmpB])
                P.op("dve", lambda e, j=j: e.scalar_tensor_tensor(out=M[j][:], in0=M[j][:], scalar=1.0, in1=tmp, op0=ALU.add, op1=ALU.mult),
                     reads=[tmpB, MB[j]], writes=[MB[j]])

    AR_tmp = sb("modtmp", [128, 1024], F32); tmpB = Buf()
    load_mod(False, range(6))

    lv = AR.f32([128, 4, 64]); lvB = Buf()
    P.dma("sp", lambda e: e.dma_start(out=lv, in_=bass.AP(lam4.tensor, 0, [[0, 128], [64, 4], [1, 64]])), writes=[lvB])
    lp = AR.f32([128, 2, 64]); l2 = AR.f32([128, 4]); l2B = Buf()
    P.op("dve", lambda e: e.tensor_tensor(out=lp[:, 0, :], in0=lv[:, 0, :], in1=lv[:, 1, :], op=ALU.mult), reads=[lvB], writes=[l2B])
    P.op("dve", lambda e: e.tensor_tensor(out=lp[:, 1, :], in0=lv[:, 2, :], in1=lv[:, 3, :], op=ALU.mult), reads=[lvB, l2B], writes=[l2B])
    P.op("dve", lambda e: e.tensor_reduce(out=l2[:, 0:2], in_=lp, axis=AX.X, op=ALU.add), reads=[l2B], writes=[l2B])
    P.op("act", lambda e: e.activation(out=l2[:, 2:4], in_=l2[:, 0:2], func=AF.Exp), reads=[l2B], writes=[l2B])
    P.op("dve", lambda e: e.tensor_tensor(out=lamt[:, 0:1], in0=l2[:, 3:4], in1=l2[:, 2:3], op=ALU.subtract), reads=[l2B], writes=[lamB])
    P.op("dve", lambda e: e.tensor_scalar(out=lamt[:, 0:1], in0=lamt[:, 0:1], scalar1=-LAM_INIT, scalar2=None, op0=ALU.add), reads=[lamB], writes=[lamB])
    P.dma("sp", lambda e: e.dma_start(out=lamt[:, 1:2], in_=subln), writes=[lamB])
    P.op("dve", lambda e: e.tensor_scalar(out=lamt[:, 1:2], in0=lamt[:, 1:2], scalar1=(1.0 - LAM_INIT) * math.sqrt(128.0), scalar2=None, op0=ALU.mult),
         reads=[lamB], writes=[lamB])
    rb = AR.f32([128, 8]); rbB = Buf()
    P.dma("sp", lambda e: e.dma_start(out=rb[0:32, :], in_=relb), writes=[rbB])
    gS = AR.f32([128, 1152]); gSB = Buf()
    for half in range(3):
        pi = nextps()
        P.op("pe", lambda e, pi=pi, half=half: e.matmul(ps[pi][0:8, 0:384], lhsT=rb[0:32, :], rhs=CF[0:32, 256 + half * 384:256 + (half + 1) * 384], start=True, stop=True),
             reads=[rbB, CFb], writes=[psB[pi]])
        P.op("dve", lambda e, pi=pi, half=half: e.tensor_copy(out=gS[0:8, half * 384:(half + 1) * 384], in_=ps[pi][0:8, 0:384]), reads=[psB[pi]], writes=[gSB])
    tgd = P.dma("sp", lambda e: e.dma_start(out=gD.rearrange("a b c -> a (b c)"), in_=gS[0:8, :]), reads=[gSB])
    Tp = AR.f32([128, 8, 384]); TpB = Buf()
    for blk in range(6):
        P.dma("sp", lambda e, blk=blk: e.dma_start(out=Tp[:, :, blk * 64:(blk + 1) * 64],
                                                  in_=bass.AP(gD.tensor, blk * 192, [[1, 128], [1152, 8], [1, 64]])), writes=[TpB], waits=[tgd])
    for hm in range(8):
        pi = nextps()
        P.op("pe", lambda e, pi=pi, hm=hm: e.matmul(ps[pi][:, 0:384], lhsT=Jf, rhs=Tp[:, hm, :], start=True, stop=True), reads=[TpB, CFb], writes=[psB[pi]])
        P.op("dve", lambda e, pi=pi, hm=hm: e.tensor_copy(out=Bb[:, hm, :], in_=ps[pi][:, 0:384]), reads=[psB[pi]], writes=[BbB])

    if KCUT == 1:
        return finish()
    AR.reset()
    winb = AR.bf([128, 8, 2080]); wuqb = AR.bf([128, 2, 768]); wukvb = AR.bf([128, 2, 1024]); wB_ = Buf()
    for k in range(8):
        P.dma("pool", lambda e, k=k: e.dma_start(out=winb[:, k, :], in_=w_in[k * 128:(k + 1) * 128, :]), writes=[wB_])
    P.dma("pool", lambda e: e.dma_start(out=wuqb, in_=w_uq.rearrange("(k p) n -> p k n", p=128)), writes=[wB_])
    P.dma("pool", lambda e: e.dma_start(out=wukvb, in_=w_ukv.rearrange("(k p) n -> p k n", p=128)), writes=[wB_])
    NR = 2
    xt = [AR.f32([128, 1024]) for _ in range(NR)]; xtB = [Buf() for _ in range(NR)]
    rp = [AR.f32([128, 64]) for _ in range(NR)]; rpB = [Buf() for _ in range(NR)]
    hf = AR.f32([128, 1024]); hfB = Buf()
    hb = AR.bf([128, 1024]); hbB = Buf()
    junk = AR.bf([128, 1024]); junkB = Buf()
    hT = [AR.bf([128, 8, 128]) for _ in range(NR)]; hTB = [Buf() for _ in range(NR)]
    ssA = AR.f32([128, 8]); ssB_ = [Buf() for _ in range(8)]
    latf = [AR.f32([128, 256]) for _ in range(NR)]; latfB = [Buf() for _ in range(NR)]
    kpef = [AR.f32([128, 32]) for _ in range(NR)]; kpefB = [Buf() for _ in range(NR)]
    rtmp = AR.f32([128, 8, 32]); rtmpB = Buf(); rtmp2 = AR.f32([128, 8, 32])
    dkf = [AR.f32([128, 512]) for _ in range(NR)]; dkfB = [Buf() for _ in range(NR)]
    dvf = [AR.f32([128, 512]) for _ in range(NR)]; dvfB = [Buf() for _ in range(NR)]
    latb = AR.bf([128, 256]); latbB = Buf(); kpeb = AR.bf([128, 32]); kpebB = Buf()
    dkb = AR.bf([128, 512]); dkbB = Buf()
    latT = AR.bf([128, 2, 128]); latTB = Buf()
    kcomb = AR.bf([128, 8, 96]); kcombB = Buf()
    kTst = AR.bf([128, 8, 512]); kTstB = Buf(); dkTst = AR.bf([128, 4, 512]); dkTstB = Buf()
    vst = AR.bf([128, 8, 4, 65]); vstB = Buf(); dvst = AR.bf([128, 4, 4, 128]); dvstB = Buf()
    qnb = AR.bf([128, 256]); qnbB = Buf(); qnT = AR.bf([128, 2, 128]); qnTB = Buf()
    qc = AR.bf([128, 8, 96]); qcB = Buf(); dqb = AR.bf([128, 512]); dqbB = Buf()
    qTst = AR.bf([128, 8, 512]); qTstB = Buf(); dqTst = AR.bf([128, 2, 4, 512]); dqTstB = Buf()
    P.op("pool", lambda e: e.memset(vst, 1.0), writes=[vstB])
    P.op("pool", lambda e: e.memset(dqTst, 0.0), writes=[dqTstB])

    def transposes(src_fn, n, rows, dstB_reads, dst_ap, dstB, eng="dve"):
        pi = nextps(); pb = ps[pi][:].bitcast(BF16)

        def f(e):
            for k in range(n):
                ins = e.transpose(out=pb[0:rows, k * 128:(k + 1) * 128], in_=src_fn(k), identity=identb[:])
            return ins
        P.op("pe", f, reads=dstB_reads + [cB], writes=[psB[pi]])
        src = pb[0:rows, 0:n * 128].rearrange("p (a b) -> p a b", a=n)
        if eng == "act":
            P.op("act", lambda e: e.activation(out=dst_ap, in_=src, func=AF.Copy), reads=[psB[pi]], writes=[dstB])
        else:
            P.op(eng, lambda e: e.tensor_copy(out=dst_ap, in_=src), reads=[psB[pi]], writes=[dstB])

    def norm_h(xa, xB, Aj, Bj):
        rms_ops(xa, xB, 1024, junk, (ssA[:, 0:1], ssB_[0]), "x")
        P.op("dve", lambda e: e.scalar_tensor_tensor(out=hf, in0=xa, scalar=ssA[:, 0:1], in1=M[Aj][:], op0=ALU.mult, op1=ALU.mult),
             reads=[xB, ssB_[0], MB[Aj]], writes=[hfB])
        P.op("pool", lambda e: e.tensor_tensor(out=hb, in0=hf, in1=M[Bj][:], op=ALU.add), reads=[hfB, MB[Bj]], writes=[hbB])

    def rope(eng, dst, srcv, tab, nh, reads, writes):
        cs = tab[:, 0:32].unsqueeze(1).broadcast_to([128, nh, 32])
        s1 = tab[:, 32:48].unsqueeze(1).broadcast_to([128, nh, 16])
        s2 = tab[:, 48:64].unsqueeze(1).broadcast_to([128, nh, 16])
        t1 = rtmp[:, 0:nh, :]; t2 = rtmp2[:, 0:nh, :]
        P.op(eng, lambda e: e.tensor_tensor(out=t1, in0=srcv, in1=cs, op=ALU.mult), reads=reads, writes=[rtmpB])
        P.op(eng, lambda e: e.tensor_tensor(out=t2[:, :, 0:16], in0=srcv[:, :, 16:32], in1=s1, op=ALU.mult), reads=reads + [rtmpB], writes=[rtmpB])
        P.op(eng, lambda e: e.tensor_tensor(out=t2[:, :, 16:32], in0=srcv[:, :, 0:16], in1=s2, op=ALU.mult), reads=reads + [rtmpB], writes=[rtmpB])
        P.op(eng, lambda e: e.tensor_tensor(out=dst, in0=t1, in1=t2, op=ALU.add), reads=[rtmpB], writes=writes)

    def kside_derive(t4, dests, latb_, kpeb_, dkb_, dvb_src, rB):
        kT_dst, dkT_dst, v_dst, dv_dst = dests
        transposes(lambda k: latb_[:, k * 128:(k + 1) * 128], 2, 128, rB, latT, latTB)
        pa = nextps(); pb_ = nextps()

        def f(e):
            for hh, pi in ((0, pa), (1, pb_)):
                for k in range(2):
                    ins = e.matmul(ps[pi][:], lhsT=latT[:, k, :], rhs=wukvb[:, k, hh * 512:(hh + 1) * 512], start=(k == 0), stop=(k == 1))
            return ins
        P.op("pe", f, reads=[latTB, wB_], writes=[psB[pa], psB[pb_]])
        for hh, pi in ((0, pa), (1, pb_)):
            v4 = ps[pi][:].rearrange("p (h c) -> p h c", h=4)
            P.op("dve", lambda e, v4=v4, hh=hh: e.tensor_copy(out=kcomb[:, hh * 4:hh * 4 + 4, 0:64], in_=v4[:, :, 0:64]), reads=[psB[pi]], writes=[kcombB])
            P.op("act", lambda e, v4=v4, hh=hh: e.activation(out=v_dst[:, hh * 4:hh * 4 + 4, t4, 1:65] if v_dst is not None else nv[:, hh * 4:hh * 4 + 4, 1:65],
                                                            in_=v4[:, :, 64:128], func=AF.Copy), reads=[psB[pi]], writes=[vstB])
        P.op("pool", lambda e: e.tensor_copy(out=kcomb[:, :, 64:96], in_=kpeb_.unsqueeze(1).broadcast_to([128, 8, 32])), reads=rB + [kcombB], writes=[kcombB])
        transposes(lambda h: kcomb[:, h, :], 8, 96, [kcombB], kT_dst, kTstB, eng="act")
        transposes(lambda h: dkb_[:, h * 128:(h + 1) * 128], 4, 128, rB, dkT_dst, dkTstB)
        if dvb_src is not None:
            P.op("pool", lambda e: e.tensor_copy(out=dv_dst, in_=dvb_src.rearrange("p (h c) -> p h c", h=4)), reads=rB, writes=[dvstB])

    def flush_k(sidx, q4):
        c0 = q4 * 512
        if sidx is None:
            kd, dkd, vd, dvd = KTm, KTd, Vm, Vd
        else:
            kd, dkd, vd, dvd = sKTm[sidx], sKTd[sidx], sVm[sidx], sVd[sidx]
        P.dma("sp", lambda e: e.dma_start(out=kd[:, :, c0:c0 + 512].rearrange("h d t -> d h t"), in_=kTst[0:96, :, :]), reads=[kTstB])
        P.dma("sp", lambda e: e.dma_start(out=dkd[:, :, c0:c0 + 512].rearrange("h d t -> d h t"), in_=dkTst), reads=[dkTstB])
        P.dma("sp", lambda e: e.dma_start(out=vd[:, :, q4 * 4:q4 * 4 + 4, :].rearrange("h p t c -> p h t c"), in_=vst), reads=[vstB])
        P.dma("sp", lambda e: e.dma_start(out=dvd[:, :, q4 * 4:q4 * 4 + 4, :].rearrange("h p t c -> p h t c"), in_=dvst), reads=[dvstB])

    def proj_tile(xsrc, ropesrc, r, own_cols, kv_cols, outs, t4, sample=False):
        P.dma("sp", lambda e: e.dma_start(out=xt[r], in_=xsrc), writes=[xtB[r]])
        P.dma("sp", lambda e: e.dma_start(out=rp[r], in_=ropesrc), writes=[rpB[r]])
        norm_h(xt[r], xtB[r], 0, 1)
        transposes(lambda k: hb[:, k * 128:(k + 1) * 128], 8, 128, [hbB], hT[r], hTB[r], eng="act")

        def inproj(c0, c1):
            pi = nextps()

            def f(e):
                for k in range(8):
                    ins = e.matmul(ps[pi][:, 0:c1 - c0], lhsT=hT[r][:, k, :], rhs=winb[:, k, c0:c1], start=(k == 0), stop=(k == 7))
                return ins
            P.op("pe", f, reads=[hTB[r], wB_], writes=[psB[pi]])
            return pi
        if kv_cols:
            o_lat_, o_kpe_, o_dk_, o_dv_ = outs
            pi = inproj(256, 544)
            rms_ops(ps[pi][:, 0:256], psB[pi], 256, junk[:, 0:256], (ssA[:, 1:2], ssB_[1]), "kv")
            P.op("dve", lambda e: e.scalar_tensor_tensor(out=latf[r], in0=ps[pi][:, 0:256], scalar=ssA[:, 1:2], in1=gains[:, 256:512], op0=ALU.mult, op1=ALU.mult),
                 reads=[psB[pi], ssB_[1], gB], writes=[latfB[r]])
            P.op("pool", lambda e: e.tensor_copy(out=latb, in_=latf[r]), reads=[latfB[r]], writes=[latbB])
            rope("dve", kpef[r].unsqueeze(1), ps[pi][:, 256:288].unsqueeze(1), rp[r], 1, [psB[pi], rpB[r]], [kpefB[r]])
            P.op("pool", lambda e: e.tensor_copy(out=kpeb, in_=kpef[r]), reads=[kpefB[r]], writes=[kpebB])
            P.dma("sp", lambda e: e.dma_start(out=o_lat_, in_=latf[r]), reads=[latfB[r]])
            P.dma("sp", lambda e: e.dma_start(out=o_kpe_, in_=kpef[r]), reads=[kpefB[r]])
            pk = inproj(1056, 1568)
            P.op("act", lambda e: e.activation(out=dkf[r], in_=ps[pk][:], func=AF.Copy), reads=[psB[pk]], writes=[dkfB[r]])
            P.op("dve", lambda e: e.tensor_copy(out=dkb, in_=ps[pk][:]), reads=[psB[pk]], writes=[dkbB])
            P.dma("sp", lambda e: e.dma_start(out=o_dk_, in_=dkf[r]), reads=[dkfB[r]])
            pv = inproj(1568, 2080)
            P.op("act", lambda e: e.activation(out=dvf[r], in_=ps[pv][:], func=AF.Copy), reads=[psB[pv]], writes=[dvfB[r]])
            dvdst = ndv[:] if sample else dvst[:, :, t4, :]
            P.op("dve", lambda e: e.tensor_copy(out=dvdst, in_=ps[pv][:].rearrange("p (h c) -> p h c", h=4)), reads=[psB[pv]], writes=[dvstB])
            P.dma("sp", lambda e: e.dma_start(out=o_dv_, in_=dvf[r]), reads=[dvfB[r]])
            if sample:
                dests = (nkT[0:96, :, :], ndkT[:], None, None)
            else:
                dests = (kTst[0:96, :, t4 * 128:(t4 + 1) * 128], dkTst[:, :, t4 * 128:(t4 + 1) * 128], vst, None)
            kside_derive(t4, dests, latb, kpeb, dkb, None, [latbB, kpebB, dkbB])
        if own_cols:
            pi = inproj(0, 256)
            rms_ops(ps[pi][:, 0:256], psB[pi], 256, junk[:, 0:256], (ssA[:, 2:3], ssB_[2]), "q")
            P.op("dve", lambda e: e.scalar_tensor_tensor(out=qnb, in0=ps[pi][:, 0:256], scalar=ssA[:, 2:3], in1=gains[:, 0:256], op0=ALU.mult, op1=ALU.mult),
                 reads=[psB[pi], ssB_[2], gB], writes=[qnbB])
            pq = inproj(544, 1056)
            P.op("act", lambda e: e.activation(out=dqb, in_=ps[pq][:], func=AF.Copy), reads=[psB[pq]], writes=[dqbB])
            transposes(lambda k: qnb[:, k * 128:(k + 1) * 128], 2, 128, [qnbB], qnT, qnTB)
            pa = nextps(); pb_ = nextps()

            def f(e):
                for hh, pj in ((0, pa), (1, pb_)):
                    for k in range(2):
                        ins = e.matmul(ps[pj][:, 0:384], lhsT=qnT[:, k, :], rhs=wuqb[:, k, hh * 384:(hh + 1) * 384], start=(k == 0), stop=(k == 1))
                return ins
            P.op("pe", f, reads=[qnTB, wB_], writes=[psB[pa], psB[pb_]])
            for hh, pj in ((0, pa), (1, pb_)):
                v4 = ps[pj][:, 0:384].rearrange("p (h c) -> p h c", h=4)
                P.op("act", lambda e, v4=v4, hh=hh: e.activation(out=qc[:, hh * 4:hh * 4 + 4, 0:64], in_=v4[:, :, 0:64], func=AF.Copy), reads=[psB[pj]], writes=[qcB])
                rope("dve", qc[:, hh * 4:hh * 4 + 4, 64:96], v4[:, :, 64:96], rp[r], 4, [psB[pj], rpB[r]], [qcB])
            if sample:
                qd = nqT[0:96, :, :]
                dq0, dq1 = ndqT[0:64, 0, :, :], ndqT[64:128, 1, :, :]
            else:
                qd = qTst[0:96, :, t4 * 128:(t4 + 1) * 128]
                dq0, dq1 = dqTst[0:64, 0, :, t4 * 128:(t4 + 1) * 128], dqTst[64:128, 1, :, t4 * 128:(t4 + 1) * 128]
            transposes(lambda h: qc[:, h, :], 8, 96, [qcB], qd, qTstB, eng="act")
            pq2 = nextps(); pbq = ps[pq2][:].bitcast(BF16)

            def ftq(e):
                for k in range(4):
                    ins = e.transpose(out=pbq[:, k * 128:(k + 1) * 128], in_=dqb[:, k * 128:(k + 1) * 128], identity=identb[:])
                return ins
            P.op("pe", ftq, reads=[dqbB, cB], writes=[psB[pq2]])
            P.op("dve", lambda e: e.tensor_copy(out=dq0, in_=pbq[0:64, 0:512].rearrange("p (a b) -> p a b", a=4)), reads=[psB[pq2]], writes=[dqTstB])
            P.op("dve", lambda e: e.tensor_copy(out=dq1, in_=pbq[64:128, 0:512].rearrange("p (a b) -> p a b", a=4)), reads=[psB[pq2], dqTstB], writes=[dqTstB])

    for t in range(64):
        if KCUT == 2 and t == 4:
            return finish()
        sl = slice(t * 128, (t + 1) * 128)
        proj_tile(x_all[sl, :], rope_all[sl, :], t % NR, False, True, (o_lat[sl, :], o_kpe[sl, :], o_dk[sl, :], o_dv[sl, :]), t % 4)
        if t % 4 == 3:
            flush_k(None, t // 4)
    if KCUT == 3:
        return finish()
    for t in range(32):
        sl = slice(t * 128, (t + 1) * 128)
        proj_tile(x_own[sl, :], rope_own[sl, :], t % NR, True, False, None, t % 4)
        if t % 4 == 3:
            c0 = (t // 4) * 512
            P.dma("sp", lambda e, c0=c0: e.dma_start(out=QTm[:, :, c0:c0 + 512].rearrange("h d t -> d h t"), in_=qTst[0:96, :, :]), reads=[qTstB])
            for m_ in range(2):
                P.dma("sp", lambda e, c0=c0, m_=m_: e.dma_start(out=QTd[:, m_, :, c0:c0 + 512].rearrange("h d t -> d h t"), in_=dqTst[:, m_, :, :]), reads=[dqTstB])
    if KCUT == 4:
        return finish()
    clb = [AR.bf([128, 256]) for _ in range(2)]; ckb = [AR.bf([128, 32]) for _ in range(2)]
    cdkb = [AR.bf([128, 512]) for _ in range(2)]; cdvb = [AR.bf([128, 512]) for _ in range(2)]
    ccB = [Buf(), Buf()]
    for s in range(2):
        for t in range(32):
            r = t % 2; sl = slice(t * 128, (t + 1) * 128)
            P.dma("pool", lambda e, r=r, s=s, sl=sl: e.dma_start(out=clb[r], in_=clat[s, sl, :]), writes=[ccB[r]])
            P.dma("pool", lambda e, r=r, s=s, sl=sl: e.dma_start(out=ckb[r], in_=ckr[s, sl, :]), writes=[ccB[r]])
            P.dma("pool", lambda e, r=r, s=s, sl=sl: e.dma_start(out=cdkb[r], in_=cdk[s, sl, :]), writes=[ccB[r]])
            P.dma("pool", lambda e, r=r, s=s, sl=sl: e.dma_start(out=cdvb[r], in_=cdv[s, sl, :]), writes=[ccB[r]])
            t4 = t % 4
            dests = (kTst[0:96, :, t4 * 128:(t4 + 1) * 128], dkTst[:, :, t4 * 128:(t4 + 1) * 128], vst, dvst[:, :, t4, :])
            kside_derive(t4, dests, clb[r], ckb[r], cdkb[r], cdvb[r], [ccB[r]])
            if t4 == 3:
                flush_k(s, t // 4)
    if KCUT == 5:
        return finish()
    load_mod(True, (0, 1))
    proj_tile(x_s, rope_s, 0, True, True, (o_lats, o_kpes, o_dks, o_dvs), 0, sample=True)
    if STAGE <= 1:
        return finish()
    if KCUT == 6:
        return finish()
    AR.reset()
    ktb = [AR.bf([128, 8192]) for _ in range(2)]; ktB = [Buf(), Buf()]
    vb = [AR.bf([128, 64, 128]) for _ in range(2)]; vB = [Buf(), Buf()]
    qtb = [AR.bf([128, 2, 4096]) for _ in range(2)]; qtB = [Buf(), Buf()]
    NPT = 8
    pt = [AR.bf([128, 512]) for _ in range(NPT)]; ptB = [Buf() for _ in range(NPT)]
    sbs = [AR.f32([128, 128]) for _ in range(2)]; sbsB = [Buf(), Buf()]
    rl = AR.f32([128, 2, 512]); rlB = Buf()
    bcs = AR.f32([128, 512]); bcsB = Buf()
    o1 = AR.f32([128, 512]); o2 = AR.f32([128, 512]); oB = Buf()
    sq = AR.f32([128, 512]); sqB = Buf()
    otile = [AR.bf([128, 512]) for _ in range(2)]; otB = [Buf(), Buf()]
    zt = AR.bf([128, 1024]); ztB = Buf()
    rings = {"s": 0, "p": 0, "o": 0, "b": 0}

    def ring(name, n):
        i = rings[name]; rings[name] = (i + 1) % n
        return i

    P.op("pool", lambda e: e.memset(zt, 0.0), writes=[ztB])

    def zero_fill():
        for XG in XGs:
            for c in range(HALF // 2048):
                P.dma("sp", lambda e, c=c, XG=XG: e.dma_start(out=XG[c * 2048:(c + 1) * 2048, :].rearrange("(p r) c -> p r c", p=128),
                                                             in_=zt.unsqueeze(1).broadcast_to([128, 16, 1024])), reads=[ztB])

    def load_pass(kind, h, slot, s=None):
        if s is None:
            if kind == "m":
                P.dma("sp", lambda e: e.dma_start(out=ktb[slot][0:96, :], in_=KTm[h]), writes=[ktB[slot]])
                P.dma("sp", lambda e: e.dma_start(out=vb[slot][:, :, 0:65], in_=Vm[h]), writes=[vB[slot]])
                P.dma("sp", lambda e: e.dma_start(out=qtb[slot][0:96, 0, :], in_=QTm[h]), writes=[qtB[slot]])
            else:
                P.dma("sp", lambda e: e.dma_start(out=ktb[slot], in_=KTd[h]), writes=[ktB[slot]])
                P.dma("sp", lambda e: e.dma_start(out=vb[slot], in_=Vd[h]), writes=[vB[slot]])
                P.dma("sp", lambda e: e.dma_start(out=qtb[slot], in_=QTd[h].rearrange("m d t -> d m t")), writes=[qtB[slot]])
        else:
            if kind == "m":
                P.dma("sp", lambda e: e.dma_start(out=ktb[slot][0:96, 0:4096], in_=sKTm[s, h]), writes=[ktB[slot]])
                P.dma("sp", lambda e: e.dma_start(out=vb[slot][:, 0:32, 0:65], in_=sVm[s, h]), writes=[vB[slot]])
            else:
                P.dma("sp", lambda e: e.dma_start(out=ktb[slot][:, 0:4096], in_=sKTd[s, h]), writes=[ktB[slot]])
                P.dma("sp", lambda e: e.dma_start(out=vb[slot][:, 0:32, :], in_=sVd[s, h]), writes=[vB[slot]])

    def fin_mla(acc, h, col0, N):
        P.op("dve", lambda e: e.reciprocal(out=rl[0:1, 0, 0:N], in_=ps[acc][0:1, 0:N]), reads=[psB[acc]], writes=[rlB])
        P.op("pe", lambda e: e.matmul(ps[7][0:65, 0:N], lhsT=onesf[0:1, 0:65], rhs=rl[0:1, 0, 0:N], start=True, stop=True), reads=[rlB, cB], writes=[psB[7]])
        P.op("dve", lambda e: e.tensor_copy(out=bcs[0:65, 0:N], in_=ps[7][0:65, 0:N]), reads=[psB[7]], writes=[bcsB])
        k = ring("o", 2); ot = otile[k]
        P.op("dve", lambda e: e.tensor_tensor(out=ot[0:65, 0:N], in0=ps[acc][0:65, 0:N], in1=bcs[0:65, 0:N], op=ALU.mult), reads=[psB[acc], bcsB], writes=[otB[k]])
        P.dma("sp", lambda e: e.dma_start(out=OT[h * 64:(h + 1) * 64, col0:col0 + N], in_=ot[1:65, 0:N]), reads=[otB[k]], waits=(tz if col0 >= 4096 else ()))

    def fin_diff(h, col0, N):
        P.op("dve", lambda e: e.reciprocal(out=rl[:, 0, 0:N], in_=ps[5][:, 0:N]), reads=[psB[5]], writes=[rlB])
        P.op("dve", lambda e: e.reciprocal(out=rl[:, 1, 0:N], in_=ps[6][:, 0:N]), reads=[psB[6], rlB], writes=[rlB])
        P.op("dve", lambda e: e.tensor_tensor(out=o1[:, 0:N], in0=ps[3][:, 0:N], in1=rl[:, 0, 0:N], op=ALU.mult), reads=[psB[3], rlB], writes=[oB])
        P.op("dve", lambda e: e.tensor_tensor(out=o2[:, 0:N], in0=ps[4][:, 0:N], in1=rl[:, 1, 0:N], op=ALU.mult), reads=[psB[4], rlB, oB], writes=[oB])
        P.op("dve", lambda e: e.scalar_tensor_tensor(out=o1[:, 0:N], in0=o2[:, 0:N], scalar=lamt[:, 0:1], in1=o1[:, 0:N], op0=ALU.mult, op1=ALU.add),
             reads=[oB, lamB], writes=[oB])
        P.op("pool", lambda e: e.tensor_tensor(out=sq[:, 0:N], in0=o1[:, 0:N], in1=o1[:, 0:N], op=ALU.mult), reads=[oB], writes=[sqB])
        P.op("pe", lambda e: e.matmul(ps[7][:, 0:N], lhsT=onesf[:], rhs=sq[:, 0:N], start=True, stop=True), reads=[sqB, cB], writes=[psB[7]])
        P.op("dve", lambda e: e.tensor_scalar(out=bcs[:, 0:N], in0=ps[7][:, 0:N], scalar1=128.0 * EPS, scalar2=None, op0=ALU.add), reads=[psB[7]], writes=[bcsB])
        P.op("pool", lambda e: e.tensor_tensor(out=bcs[:, 0:N], in0=bcs[:, 0:N], in1=nhalf[:, 0:1].broadcast_to([128, N]), op=ALU.pow), reads=[bcsB], writes=[bcsB])
        k = ring("o", 2); ot = otile[k]
        P.op("dve", lambda e: e.scalar_tensor_tensor(out=ot[:, 0:N], in0=o1[:, 0:N], scalar=lamt[:, 1:2], in1=bcs[:, 0:N], op0=ALU.mult, op1=ALU.mult),
             reads=[oB, bcsB, lamB], writes=[otB[k]])
        P.dma("sp", lambda e: e.dma_start(out=OT[512 + h * 128:512 + (h + 1) * 128, col0:col0 + N], in_=ot[:, 0:N]), reads=[otB[k]], waits=(tz if col0 >= 4096 else ()))

    def attn_tiles(kind, h, tiles, q_fn, qB_, col0, N, accs):
        pend = []
        n = len(tiles)
        nm = 1 if kind == "m" else 2
        LOOK = 3 if kind == "m" else 2

        def pv(item):
            i, tl, pjs = item
            for m in range(nm):
                pj = pjs[m]; c0 = tl["c0"]; rows = tl["rows"]
                first = (i == 0); last = (i == n - 1)
                parts = [(0, rows, c0)]
                for (r0, r1, cc) in parts:
                    vv = tl["v_ap"]
                    if kind == "m":
                        P.op("pe", lambda e, r0=r0, r1=r1, cc=cc, vv=vv, pj=pj, first=first, last=last: e.matmul(
                            ps[accs[0]][0:65, cc:N], lhsT=vv[r0:r1, 0:65], rhs=pt[pj][r0:r1, cc:N], start=first and r0 == 0, stop=last and r1 == rows),
                            reads=[ptB[pj], tl["vB"]], writes=[psB[accs[0]]])
                    else:
                        def f(e, r0=r0, r1=r1, cc=cc, vv=vv, pj=pj, first=first, last=last, m=m):
                            e.matmul(ps[accs[m]][:, cc:N], lhsT=vv[r0:r1, :], rhs=pt[pj][r0:r1, cc:N], start=first and r0 == 0, stop=last and r1 == rows)
                            return e.matmul(ps[accs[2 + m]][:, cc:N], lhsT=onesb[r0:r1, :], rhs=pt[pj][r0:r1, cc:N], start=first and r0 == 0, stop=last and r1 == rows)
                        P.op("pe", f, reads=[ptB[pj], tl["vB"], cB], writes=[psB[accs[m]], psB[accs[2 + m]]])

        for i, tl in enumerate(tiles):
            c0 = tl["c0"]; rows = tl["rows"]; pjs = []
            for m in range(nm):
                sk = (0, 1, 2, 7)[ring("s", 4)]; pj = ring("p", NPT); pjs.append(pj)
                kap = tl["k_ap"](m); qap = q_fn(m, c0)
                P.op("pe", lambda e, sk=sk, kap=kap, qap=qap, c0=c0, rows=rows: e.matmul(ps[sk][0:rows, c0:N], lhsT=kap, rhs=qap, start=True, stop=True),
                     reads=[tl["kB"], qB_], writes=[psB[sk]])
                if kind == "m":
                    P.op("act", lambda e, sk=sk, pj=pj, c0=c0, rows=rows: e.activation(out=pt[pj][0:rows, c0:N], in_=ps[sk][0:rows, c0:N], func=AF.Exp, scale=MLA_SCALE),
                         reads=[psB[sk]], writes=[ptB[pj]])
                else:
                    hm = h * 2 + m; b15 = Bb[0:rows, hm, 128:129]
                    if tl["bias"] is not None:
                        lo, w = tl["bias"]; w = min(w, N - c0)
                        bi = ring("b", 2)
                        P.op("dve", lambda e, sk=sk, bi=bi, c0=c0, w=w, lo=lo, hm=hm, rows=rows: e.scalar_tensor_tensor(
                            out=sbs[bi][0:rows, 0:w], in0=ps[sk][0:rows, c0:c0 + w], scalar=DIFF_SCALE, in1=Bb[0:rows, hm, lo:lo + w], op0=ALU.mult, op1=ALU.add),
                            reads=[psB[sk], BbB], writes=[sbsB[bi]])
                        P.op("act", lambda e, bi=bi, pj=pj, c0=c0, w=w, rows=rows: e.activation(out=pt[pj][0:rows, c0:c0 + w], in_=sbs[bi][0:rows, 0:w], func=AF.Exp),
                             reads=[sbsB[bi]], writes=[ptB[pj]])
                        if c0 + w < N:
                            P.op("act", lambda e, sk=sk, pj=pj, c0=c0, w=w, b15=b15, rows=rows: e.activation(
                                out=pt[pj][0:rows, c0 + w:N], in_=ps[sk][0:rows, c0 + w:N], func=AF.Exp, scale=DIFF_SCALE, bias=b15),
                                reads=[psB[sk], BbB, ptB[pj]], writes=[ptB[pj]])
                    else:
                        P.op("act", lambda e, sk=sk, pj=pj, c0=c0, b15=b15, rows=rows: e.activation(
                            out=pt[pj][0:rows, c0:N], in_=ps[sk][0:rows, c0:N], func=AF.Exp, scale=DIFF_SCALE, bias=b15),
                            reads=[psB[sk], BbB], writes=[ptB[pj]])
                if tl["diag"]:
                    P.op("pool", lambda e, pj=pj, c0=c0: e.tensor_tensor(out=pt[pj][64:128, c0:c0 + 64], in0=pt[pj][64:128, c0:c0 + 64], in1=mpib[64:128, :], op=ALU.mult),
                         reads=[ptB[pj], cB], writes=[ptB[pj]])
                if tl["pmask"] is not None:
                    pm = tl["pmask"]
                    P.op("pool", lambda e, pj=pj, pm=pm, rows=rows: e.tensor_tensor(out=pt[pj][0:rows, 0:N], in0=pt[pj][0:rows, 0:N], in1=pm.broadcast_to([rows, N]), op=ALU.mult),
                         reads=[ptB[pj], cB], writes=[ptB[pj]])
            pend.append((i, tl, pjs))
            if len(pend) > LOOK:
                pv(pend.pop(0))
        while pend:
            pv(pend.pop(0))

    def prompt_pass(kind, h, slot):
        kt_, v_, q_ = ktb[slot], vb[slot], qtb[slot]
        for G in range(8):
            tiles = []
            for kt in range(8 * G):
                bias = (64, 64) if (kt == 8 * G - 1) else None
                tiles.append(dict(kt=kt, c0=0, rows=128, diag=False, bias=bias, pmask=None))
            for k in range(8):
                tiles.append(dict(kt=8 * G + k, c0=64 * k, rows=128, diag=True, bias=(0, 128), pmask=None))
            for tl in tiles:
                kt = tl["kt"]
                if kind == "m":
                    tl["k_ap"] = (lambda m, kt=kt: kt_[0:96, kt * 128:(kt + 1) * 128])
                    tl["v_ap"] = v_[:, kt, :]
                else:
                    tl["k_ap"] = (lambda m, kt=kt: kt_[:, kt * 128:(kt + 1) * 128])
                    tl["v_ap"] = v_[:, kt, :]
                tl["kB"] = ktB[slot]; tl["vB"] = vB[slot]
            if kind == "m":
                acc = 3 + (G % 2)
                attn_tiles("m", h, tiles, lambda m, c0, G=G: q_[0:96, 0, G * 512 + c0:(G + 1) * 512], qtB[slot], G * 512, 512, [acc])
                fin_mla(acc, h, G * 512, 512)
            else:
                attn_tiles("d", h, tiles, lambda m, c0, G=G: q_[:, m, G * 512 + c0:(G + 1) * 512], qtB[slot], G * 512, 512, [3, 4, 5, 6])
                fin_diff(h, G * 512, 512)

    def sample_pass(kind, h, slot, s):
        kt_, v_ = ktb[slot], vb[slot]
        tiles = []
        for kt in range(32):
            tl = dict(c0=0, rows=128, diag=False, bias=((192, 16) if kt == 31 else None), pmask=None, kB=ktB[slot], vB=vB[slot], v_ap=v_[:, kt, :])
            if kind == "m":
                tl["k_ap"] = (lambda m, kt=kt: kt_[0:96, kt * 128:(kt + 1) * 128])
            else:
                tl["k_ap"] = (lambda m, kt=kt: kt_[:, kt * 128:(kt + 1) * 128])
            tiles.append(tl)
        tl = dict(c0=0, rows=48, diag=False, bias=((256 + 64 * s, 16)), pmask=C2[0:48, 512 + s:513 + s], kB=nB, vB=nB)
        if kind == "m":
            tl["k_ap"] = (lambda m: nkT[0:96, h, 0:48]); tl["v_ap"] = nv[:, h, :]
            qf = lambda m, c0: nqT[0:96, h, s * 32:s * 32 + 16]
        else:
            tl["k_ap"] = (lambda m: ndkT[:, h, 0:48]); tl["v_ap"] = ndv[:, h, :]
            qf = lambda m, c0: ndqT[:, m, h, s * 32:s * 32 + 16]
        tiles.append(tl)
        col0 = 4096 + s * 32
        if kind == "m":
            acc = 3 + (ring("o2", 2) if False else 0)
            attn_tiles("m", h, tiles, qf, nB, col0, 16, [3])
            fin_mla(3, h, col0, 16)
        else:
            attn_tiles("d", h, tiles, qf, nB, col0, 16, [3, 4, 5, 6])
            fin_diff(h, col0, 16)

    passes = [("m", h) for h in range(8)] + [("d", h) for h in range(4)]
    load_pass(passes[0][0], passes[0][1], 0)
    tz = [P.dma("sp", lambda e: e.dma_start(out=OT[:, 4096:4224].rearrange("(k p) t -> p k t", p=128), in_=zt[:, 0:1024].rearrange("p (k t) -> p k t", k=8)), reads=[ztB])]
    zero_fill()
    for i, (kind, h) in enumerate(passes):
        if i + 1 < len(passes):
            load_pass(passes[i + 1][0], passes[i + 1][1], (i + 1) % 2)
        prompt_pass(kind, h, i % 2)
    sp_list = [(kind, h, s) for s in range(2) for (kind, h) in passes]
    load_pass(sp_list[0][0], sp_list[0][1], 0, s=sp_list[0][2])
    for i, (kind, h, s) in enumerate(sp_list):
        if i + 1 < len(sp_list):
            load_pass(sp_list[i + 1][0], sp_list[i + 1][1], (i + 1) % 2, s=sp_list[i + 1][2])
        sample_pass(kind, h, i % 2, s)
    if KCUT == 7:
        return finish()

    AR.reset()
    woutb = AR.bf([128, 8, 1024]); wrb = AR.bf([128, 8, 256]); wsgb = AR.bf([128, 8, 512]); wsdb = AR.bf([128, 2, 1024])
    wDB = [Buf() for _ in range(4)]
    P.dma("pool", lambda e: e.dma_start(out=woutb, in_=w_out.rearrange("(k p) n -> p k n", p=128)), writes=[wDB[0]])
    P.dma("pool", lambda e: e.dma_start(out=wrb, in_=w_router.rearrange("(k p) n -> p k n", p=128)), writes=[wDB[1]])
    P.dma("pool", lambda e: e.dma_start(out=wsgb, in_=w_sgu.rearrange("(k p) n -> p k n", p=128)), writes=[wDB[2]])
    P.dma("pool", lambda e: e.dma_start(out=wsdb, in_=w_sdn.rearrange("(k p) n -> p k n", p=128)), writes=[wDB[3]])
    wrf = AR.f32([128, 8, 256]); wrfB = Buf()
    P.dma("sp", lambda e: e.dma_start(out=wrf, in_=w_router.rearrange("(k p) n -> p k n", p=128)), writes=[wrfB])
    h2ff = AR.f32([128, 1024]); h2ffB = Buf(); h2Tf = AR.f32([128, 8, 128]); h2TfB = Buf()
    rbb = AR.f32([128, 256]); rbbB = Buf()
    P.dma("sp", lambda e: e.dma_start(out=rbb, in_=rbias.broadcast_to([128, 256])), writes=[rbbB])
    oTs = [AR.bf([128, 8, 512]) for _ in range(2)]; oTsB = [Buf(), Buf()]
    xd = [AR.f32([128, 1024]) for _ in range(2)]; xdB = [Buf(), Buf()]
    x1 = AR.f32([128, 1024]); x1B = Buf()
    tmpd = AR.f32([128, 1024]); tmpdB = Buf()
    h2f = AR.f32([128, 1024]); h2fB = Buf()
    h2b = [AR.bf([128, 1024]) for _ in range(2)]; h2bB = [Buf(), Buf()]
    h2T = AR.bf([128, 8, 128]); h2TB = Buf()
    junkd = AR.bf([128, 1024])
    ssD = AR.f32([128, 8]); ssDB = Buf()
    sc = AR.f32([128, 256]); scB = Buf()
    sgd = AR.f32([128, 256]); sgdB = Buf()
    abd = AR.bf([128, 256]); abdB = Buf(); aTd = AR.bf([128, 2, 128]); aTdB = Buf()
    biased = AR.f32([128, 256]); m8 = AR.f32([128, 8, 8]); gs = AR.f32([128, 8]); t8 = AR.f32([128, 8]); gm = AR.f32([128, 8])
    masked = AR.f32([128, 256]); v8 = AR.f32([128, 8]); sel = AR.f32([128, 256]); gsel = AR.f32([128, 256]); den = AR.f32([128, 8])
    Gt = AR.f32([128, 256]); selb = AR.bf([128, 256]); posf = AR.f32([128, 256]); key = AR.f32([128, 256]); d8 = AR.f32([128, 8]); neg = AR.f32([128, 8])
    neg2 = AR.f32([128, 8]); dA = AR.f32([128, 8]); dB = AR.f32([128, 8])
    key2 = AR.f32([128, 256]); key3 = AR.f32([128, 256]); v8b = AR.f32([128, 8]); v8c = AR.f32([128, 8])
    rtB = Buf()
    selbB = Buf()

    def rt(fn, extra_r=(), extra_w=()):
        P.op("dve", fn, reads=[rtB] + list(extra_r), writes=[rtB] + list(extra_w))

    def phaseD_tile(tile):
        sample = (tile == 32)
        r = tile % 2
        if sample:
            P.dma("sp", lambda e: e.dma_start(out=oTs[0][:, :, 0:128], in_=OT[:, 4096:4224].rearrange("(k p) t -> p k t", p=128)), writes=[oTsB[0]])
            oT = oTs[0]; oTB = oTsB[0]; tc0 = 0
            xsrc = x_s
        else:
            G = tile // 4
            if tile % 4 == 0:
                P.dma("sp", lambda e: e.dma_start(out=oTs[G % 2], in_=OT[:, G * 512:(G + 1) * 512].rearrange("(k p) t -> p k t", p=128)), writes=[oTsB[G % 2]])
            oT = oTs[G % 2]; oTB = oTsB[G % 2]; tc0 = (tile % 4) * 128
            xsrc = x_own[tile * 128:(tile + 1) * 128, :]
        P.dma("sp", lambda e: e.dma_start(out=xd[r], in_=xsrc), writes=[xdB[r]])
        pa = nextps(); pb_ = nextps()

        def f(e):
            for nh, pj in ((0, pa), (1, pb_)):
                for k in range(8):
                    ins = e.matmul(ps[pj][:], lhsT=oT[:, k, tc0:tc0 + 128], rhs=woutb[:, k, nh * 512:(nh + 1) * 512], start=(k == 0), stop=(k == 7))
            return ins
        P.op("pe", f, reads=[oTB, wDB[0]], writes=[psB[pa], psB[pb_]])
        for nh, pj in ((0, pa), (1, pb_)):
            P.op("dve", lambda e, nh=nh, pj=pj: e.tensor_tensor(out=tmpd[:, nh * 512:(nh + 1) * 512], in0=ps[pj][:], in1=M[2][:, nh * 512:(nh + 1) * 512], op=ALU.mult),
                 reads=[psB[pj], MB[2]], writes=[tmpdB])
        P.op("pool", lambda e: e.tensor_tensor(out=x1, in0=tmpd, in1=xd[r], op=ALU.add), reads=[tmpdB, xdB[r]], writes=[x1B])
        P.op("pool", lambda e: e.memset(ssD[:, 0:1], 0.0), writes=[ssDB])
        P.op("act", lambda e: e.activation(out=junkd, in_=x1, func=AF.Square, accum_out=ssD[:, 0:1]), reads=[x1B], writes=[ssDB])
        P.op("dve", lambda e: e.tensor_scalar(out=ssD[:, 0:1], in0=ssD[:, 0:1], scalar1=1.0 / 1024, scalar2=EPS, op0=ALU.mult, op1=ALU.add), reads=[ssDB], writes=[ssDB])
        P.op("pool", lambda e: e.tensor_tensor(out=ssD[:, 0:1], in0=ssD[:, 0:1], in1=nhalf[:, 0:1], op=ALU.pow), reads=[ssDB], writes=[ssDB])
        P.op("dve", lambda e: e.scalar_tensor_tensor(out=h2f, in0=x1, scalar=ssD[:, 0:1], in1=M[3][:], op0=ALU.mult, op1=ALU.mult), reads=[x1B, ssDB, MB[3]], writes=[h2fB])
        P.op("pool", lambda e: e.tensor_tensor(out=h2ff, in0=h2f, in1=M[4][:], op=ALU.add), reads=[h2fB, MB[4]], writes=[h2ffB])
        P.op("pool", lambda e: e.tensor_copy(out=h2b[r], in_=h2ff), reads=[h2ffB], writes=[h2bB[r]])
        transposes(lambda k: h2b[r][:, k * 128:(k + 1) * 128], 8, 128, [h2bB[r]], h2T, h2TB, eng="act")
        for hh in range(2):
            pt_ = nextps()

            def ft(e, pt_=pt_, hh=hh):
                for k in range(4):
                    ins = e.transpose(out=ps[pt_][:, k * 128:(k + 1) * 128], in_=h2ff[:, (hh * 4 + k) * 128:(hh * 4 + k + 1) * 128], identity=identf)
                return ins
            P.op("pe", ft, reads=[h2ffB, CFb], writes=[psB[pt_]])
            P.op("dve", lambda e, pt_=pt_, hh=hh: e.tensor_copy(out=h2Tf[:, hh * 4:hh * 4 + 4, :], in_=ps[pt_][:].rearrange("p (a b) -> p a b", a=4)),
                 reads=[psB[pt_]], writes=[h2TfB])
        pr = nextps(); pg = nextps()

        def f2(e):
            for k in range(8):
                e.matmul(ps[pr][:, 0:256], lhsT=h2Tf[:, k, :], rhs=wrf[:, k, :], start=(k == 0), stop=(k == 7))
            for k in range(8):
                ins = e.matmul(ps[pg][:], lhsT=h2T[:, k, :], rhs=wsgb[:, k, :], start=(k == 0), stop=(k == 7))
            return ins
        P.op("pe", f2, reads=[h2TB, h2TfB, wrfB, wDB[2]], writes=[psB[pr], psB[pg]])
        P.op("act", lambda e: e.activation(out=sc, in_=ps[pr][:, 0:256], func=AF.Sigmoid), reads=[psB[pr]], writes=[scB])
        P.op("act", lambda e: e.activation(out=sgd, in_=ps[pg][:, 0:256], func=AF.Silu), reads=[psB[pg]], writes=[sgdB])
        P.op("dve", lambda e: e.tensor_tensor(out=abd, in0=ps[pg][:, 256:512], in1=sgd, op=ALU.mult), reads=[psB[pg], sgdB], writes=[abdB])
        transposes(lambda k: abd[:, k * 128:(k + 1) * 128], 2, 128, [abdB], aTd, aTdB)
        pa2 = nextps(); pb2 = nextps()

        def f3(e):
            for nh, pj in ((0, pa2), (1, pb2)):
                for k in range(2):
                    ins = e.matmul(ps[pj][:], lhsT=aTd[:, k, :], rhs=wsdb[:, k, nh * 512:(nh + 1) * 512], start=(k == 0), stop=(k == 1))
            return ins
        P.op("pe", f3, reads=[aTdB, wDB[3]], writes=[psB[pa2], psB[pb2]])
        for nh, pj in ((0, pa2), (1, pb2)):
            P.op("dve", lambda e, nh=nh, pj=pj: e.tensor_tensor(out=tmpd[:, nh * 512:(nh + 1) * 512], in0=ps[pj][:], in1=M[5][:, nh * 512:(nh + 1) * 512], op=ALU.mult),
                 reads=[psB[pj], MB[5]], writes=[tmpdB])
        P.op("pool", lambda e: e.tensor_tensor(out=x1, in0=tmpd, in1=x1, op=ALU.add), reads=[tmpdB, x1B], writes=[x1B])
        P.dma("sp", lambda e: e.dma_start(out=X1[tile * 128:(tile + 1) * 128, :], in_=x1), reads=[x1B])
        rt(lambda e: e.tensor_tensor(out=biased, in0=sc, in1=rbb, op=ALU.add), extra_r=[scB, rbbB])
        for g in range(8):
            rt(lambda e, g=g: e.max(out=m8[:, g, :], in_=biased[:, g * 32:(g + 1) * 32]))
        rt(lambda e: e.tensor_tensor(out=gs, in0=m8[:, :, 0], in1=m8[:, :, 1], op=ALU.add))
        rt(lambda e: e.max(out=t8, in_=gs))
        rt(lambda e: e.tensor_single_scalar(out=gm, in_=gs, scalar=t8[:, 3:4], op=ALU.is_ge))
        rt(lambda e: e.tensor_scalar(out=gm, in0=gm, scalar1=-1.0, scalar2=1e9, op0=ALU.add, op1=ALU.mult))
        rt(lambda e: e.tensor_tensor(out=masked.rearrange("p (g c) -> p g c", g=8), in0=biased.rearrange("p (g c) -> p g c", g=8),
                                     in1=gm.unsqueeze(2).broadcast_to([128, 8, 32]), op=ALU.add))
        rt(lambda e: e.max(out=v8, in_=masked))
        rt(lambda e: e.tensor_single_scalar(out=sel, in_=masked, scalar=v8[:, 7:8], op=ALU.is_ge))
        vcol = C2[:, 514:515] if sample else C2[:, 515:516]
        rt(lambda e: e.tensor_scalar(out=sel, in0=sel, scalar1=vcol, scalar2=None, op0=ALU.mult), extra_r=[cB])
        rt(lambda e: e.tensor_tensor(out=gsel, in0=sel, in1=sc, op=ALU.mult))
        rt(lambda e: e.tensor_reduce(out=den[:, 0:1], in_=gsel, axis=AX.X, op=ALU.add))
        rt(lambda e: e.tensor_scalar(out=den[:, 0:1], in0=den[:, 0:1], scalar1=1e-20, scalar2=None, op0=ALU.add))
        rt(lambda e: e.reciprocal(out=den[:, 1:2], in_=den[:, 0:1]))
        rt(lambda e: e.tensor_scalar(out=Gt, in0=gsel, scalar1=den[:, 1:2], scalar2=2.5, op0=ALU.mult, op1=ALU.mult))
        P.op("pool", lambda e: e.tensor_copy(out=selb, in_=sel), reads=[rtB], writes=[selbB])
        pp = nextps(); pc = nextps()

        def f4(e):
            e.matmul(ps[pp][:, 0:256], lhsT=Ub[:], rhs=selb, start=True, stop=True)
            return e.matmul(ps[pc][:, 0:256], lhsT=onesb[:], rhs=selb, start=True, stop=True)
        P.op("pe", f4, reads=[selbB, cB], writes=[psB[pp], psB[pc]])
        rt(lambda e: e.tensor_tensor(out=posf, in0=ps[pp][:, 0:256], in1=cntbc[:], op=ALU.add), extra_r=[psB[pp], cntB])
        rt(lambda e: e.tensor_tensor(out=cntbc[:], in0=ps[pc][:, 0:256], in1=cntbc[:], op=ALU.add), extra_r=[psB[pc]], extra_w=[cntB])
        rt(lambda e: e.tensor_single_scalar(out=key, in_=posf, scalar=float(CAP), op=ALU.is_lt))
        rt(lambda e: e.tensor_tensor(out=sel, in0=sel, in1=key, op=ALU.mult))
        rt(lambda e: e.tensor_tensor(out=posf, in0=posf, in1=C2[:, 0:256], op=ALU.add))
        rt(lambda e: e.tensor_tensor(out=key, in0=posf, in1=sel, op=ALU.mult))
        rt(lambda e: e.max(out=d8, in_=key))
        rt(lambda e: e.tensor_scalar(out=d8, in0=d8, scalar1=-1.0, scalar2=None, op0=ALU.add))
        rt(lambda e: e.tensor_single_scalar(out=neg, in_=d8, scalar=0.0, op=ALU.is_lt))
        rt(lambda e: e.tensor_single_scalar(out=neg2, in_=d8, scalar=float(HALF), op=ALU.is_ge))
        rt(lambda e: e.tensor_tensor(out=neg, in0=neg, in1=neg2, op=ALU.add))
        rt(lambda e: e.scalar_tensor_tensor(out=dA, in0=neg, scalar=BIG, in1=d8, op0=ALU.mult, op1=ALU.add))
        rt(lambda e: e.tensor_copy(out=didx[:, tile, 0:8], in_=dA))
        rt(lambda e: e.tensor_scalar(out=dB, in0=neg2, scalar1=-1.0, scalar2=-BIG, op0=ALU.add, op1=ALU.mult))
        rt(lambda e: e.scalar_tensor_tensor(out=dB, in0=d8, scalar=-float(HALF), in1=dB, op0=ALU.add, op1=ALU.add))
        rt(lambda e: e.tensor_copy(out=didx[:, tile, 8:16], in_=dB))
        rt(lambda e: e.tensor_scalar(out=neg, in0=neg, scalar1=-1.0, scalar2=-1.0, op0=ALU.add, op1=ALU.mult))
        rt(lambda e: e.tensor_tensor(out=dA, in0=d8, in1=neg, op=ALU.mult))
        rt(lambda e: e.tensor_copy(out=didx[:, tile, 16:24], in_=dA))
        rt(lambda e: e.scalar_tensor_tensor(out=dB, in0=d8, scalar=-float(HALF), in1=neg2, op0=ALU.add, op1=ALU.mult))
        rt(lambda e: e.tensor_copy(out=didx[:, tile, 24:32], in_=dB), extra_w=[dgB[tile]])
        rt(lambda e: e.tensor_tensor(out=key3, in0=C2[:, 256:512], in1=sel, op=ALU.mult))
        rt(lambda e: e.tensor_tensor(out=key2, in0=Gt, in1=sel, op=ALU.mult))
        rt(lambda e: e.tensor_tensor(out=key2, in0=key2, in1=key3, op=ALU.add))
        rt(lambda e: e.max(out=v8b, in_=key2))
        rt(lambda e: e.max(out=v8c, in_=key3))
        rt(lambda e: e.tensor_tensor(out=v8b, in0=v8b, in1=v8c, op=ALU.subtract))
        rt(lambda e: e.tensor_tensor(out=gate8[:, tile, 0:8], in0=v8b, in1=neg, op=ALU.mult))
        rt(lambda e: e.tensor_tensor(out=gate8[:, tile, 8:16], in0=v8b, in1=neg2, op=ALU.mult), extra_w=[dgB[tile]])
        for j in range(16):
            P.dma("pool", lambda e, j=j: e.indirect_dma_start(out=XGs[j // 8][:, :], out_offset=bass.IndirectOffsetOnAxis(ap=didx[:, tile, j:j + 1], axis=0),
                                                            in_=h2b[r][:, :], in_offset=None, bounds_check=getbc(e), oob_is_err=False),
                  reads=[h2bB[r], dgB[tile]])

    load_mod(False, (0, 1))
    for tile in range(32):
        phaseD_tile(tile)
    load_mod(True, (2, 3, 4, 5))
    phaseD_tile(32)
    if KCUT == 8:
        return finish()

    AR.reset()
    wg = [AR.bf([128, 8, 512]) for _ in range(2)]; wgB = [Buf(), Buf()]
    wd = [AR.bf([128, 2, 1024]) for _ in range(2)]; wdB = [Buf(), Buf()]
    xg = [AR.bf([128, NST, 1024]) for _ in range(2)]; xgB = [Buf(), Buf()]
    xgT = [AR.bf([128, 8, CAP]) for _ in range(2)]; xgTB = [Buf(), Buf()]
    sge = [AR.f32([128, 2, 320]) for _ in range(2)]; sgeB = [Buf(), Buf()]
    aTe = [AR.bf([128, 2, CAP]) for _ in range(2)]; aTeB = [Buf(), Buf()]
    yb = [AR.bf([128, NST, 1024]) for _ in range(2)]; ybB = [Buf(), Buf()]
    HN = CAP // 2

    def e_s1(ex):
        r = ex % 2
        XG = XGs[ex // 128]; row0 = (ex % 128) * CAP
        P.dma("pool", lambda e: e.dma_start(out=wg[r], in_=w_gu[ex].rearrange("(k p) n -> p k n", p=128)), writes=[wgB[r]])
        P.dma("pool", lambda e: e.dma_start(out=wd[r], in_=w_dn[ex].rearrange("(k p) n -> p k n", p=128)), writes=[wdB[r]])
        P.dma("sp", lambda e: e.dma_start(out=xg[r], in_=XG[row0:row0 + CAP, :].rearrange("(s p) c -> p s c", p=128)), writes=[xgB[r]])
        for s_ in range(NST):
            transposes(lambda k, s_=s_: xg[r][:, s_, k * 128:(k + 1) * 128], 8, 128, [xgB[r]], xgT[r][:, :, s_ * 128:(s_ + 1) * 128], xgTB[r],
                       eng=("act" if s_ % 2 == 0 else "dve"))

    def e_s2(ex):
        r = ex % 2
        for nh in range(2):
            pbs = [nextps() for _ in range(4)]

            def f(e, pbs=pbs, nh=nh):
                for c in range(4):
                    for k in range(8):
                        ins = e.matmul(ps[pbs[c]][:, 0:HN], lhsT=wg[r][:, k, c * 128:(c + 1) * 128], rhs=xgT[r][:, k, nh * HN:(nh + 1) * HN], start=(k == 0), stop=(k == 7))
                return ins
            P.op("pe", f, reads=[wgB[r], xgTB[r]], writes=[psB[p_] for p_ in pbs])
            for c in range(2):
                P.op("act", lambda e, c=c, pbs=pbs, nh=nh: e.activation(out=sge[nh][:, c, 0:HN], in_=ps[pbs[c]][:, 0:HN], func=AF.Silu), reads=[psB[pbs[c]]], writes=[sgeB[nh]])
                P.op("dve", lambda e, c=c, pbs=pbs, nh=nh: e.tensor_tensor(out=aTe[r][:, c, nh * HN:(nh + 1) * HN], in0=ps[pbs[2 + c]][:, 0:HN], in1=sge[nh][:, c, 0:HN], op=ALU.mult),
                     reads=[psB[pbs[2 + c]], sgeB[nh]], writes=[aTeB[r]])

    def e_s3(ex):
        r = ex % 2
        YG = YGs[ex // 128]; row0 = (ex % 128) * CAP
        for s_ in range(NST):
            for nh in range(2):
                pj = nextps()

                def f2(e, pj=pj, s_=s_, nh=nh):
                    for k in range(2):
                        ins = e.matmul(ps[pj][:], lhsT=aTe[r][:, k, s_ * 128:(s_ + 1) * 128], rhs=wd[r][:, k, nh * 512:(nh + 1) * 512], start=(k == 0), stop=(k == 1))
                    return ins
                P.op("pe", f2, reads=[aTeB[r], wdB[r]], writes=[psB[pj]])
                if (s_ + nh) % 2 == 0:
                    P.op("act", lambda e, pj=pj, s_=s_, nh=nh: e.activation(out=yb[r][:, s_, nh * 512:(nh + 1) * 512], in_=ps[pj][:], func=AF.Copy), reads=[psB[pj]], writes=[ybB[r]])
                else:
                    P.op("dve", lambda e, pj=pj, s_=s_, nh=nh: e.tensor_copy(out=yb[r][:, s_, nh * 512:(nh + 1) * 512], in_=ps[pj][:]), reads=[psB[pj]], writes=[ybB[r]])
        P.dma("sp", lambda e: e.dma_start(out=YG[row0:row0 + CAP, :].rearrange("(s p) c -> p s c", p=128), in_=yb[r]), reads=[ybB[r]])

    e_s1(0)
    for ex in range(256):
        if ex + 1 < 256:
            e_s1(ex + 1)
        e_s2(ex)
        e_s3(ex)
    if KCUT == 9:
        return finish()

    AR.reset()
    yj = [AR.bf([128, 1024]) for _ in range(16)]; yjB = [Buf() for _ in range(16)]
    accF = AR.f32([128, 1024]); accB = Buf()
    x1f = AR.f32([128, 1024]); x1fB = Buf()
    x2 = AR.f32([128, 1024]); x2B = Buf()
    yo = AR.f32([128, 1024]); yoB = Buf()
    fnb = AR.f32([128, 1024]); fnbB = Buf()
    junkf = AR.bf([128, 1024]); ssF = AR.f32([128, 8]); ssFB = Buf()
    P.dma("sp", lambda e: e.dma_start(out=fnb, in_=fnorm.broadcast_to([128, 1024])), writes=[fnbB])
    for j in range(16):
        P.op("pool", lambda e, j=j: e.memset(yj[j], 0.0), writes=[yjB[j]])

    def phaseF_tile(tile):
        for j in range(16):
            P.dma("pool", lambda e, j=j: e.indirect_dma_start(out=yj[j][:, :], out_offset=None, in_=YGs[j // 8][:, :],
                                                            in_offset=bass.IndirectOffsetOnAxis(ap=didx[:, tile, 16 + j:17 + j], axis=0),
                                                            bounds_check=getbc(e), oob_is_err=False), reads=[dgB[tile]], writes=[yjB[j]])
        P.dma("sp", lambda e: e.dma_start(out=x1f, in_=X1[tile * 128:(tile + 1) * 128, :]), writes=[x1fB])
        P.op("dve", lambda e: e.tensor_scalar(out=accF, in0=yj[0], scalar1=gate8[:, tile, 0:1], scalar2=None, op0=ALU.mult), reads=[yjB[0], dgB[tile]], writes=[accB])
        for j in range(1, 16):
            P.op("dve", lambda e, j=j: e.scalar_tensor_tensor(out=accF, in0=yj[j], scalar=gate8[:, tile, j:j + 1], in1=accF, op0=ALU.mult, op1=ALU.add),
                 reads=[yjB[j], dgB[tile], accB], writes=[accB])
        P.op("dve", lambda e: e.tensor_tensor(out=accF, in0=accF, in1=M[5][:], op=ALU.mult), reads=[accB, MB[5]], writes=[accB])
        P.op("pool", lambda e: e.tensor_tensor(out=x2, in0=accF, in1=x1f, op=ALU.add), reads=[accB, x1fB], writes=[x2B])
        P.op("pool", lambda e: e.memset(ssF[:, 0:1], 0.0), writes=[ssFB])
        P.op("act", lambda e: e.activation(out=junkf, in_=x2, func=AF.Square, accum_out=ssF[:, 0:1]), reads=[x2B], writes=[ssFB])
        P.op("dve", lambda e: e.tensor_scalar(out=ssF[:, 0:1], in0=ssF[:, 0:1], scalar1=1.0 / 1024, scalar2=EPS, op0=ALU.mult, op1=ALU.add), reads=[ssFB], writes=[ssFB])
        P.op("pool", lambda e: e.tensor_tensor(out=ssF[:, 0:1], in0=ssF[:, 0:1], in1=nhalf[:, 0:1], op=ALU.pow), reads=[ssFB], writes=[ssFB])
        P.op("dve", lambda e: e.scalar_tensor_tensor(out=yo, in0=x2, scalar=ssF[:, 0:1], in1=fnb, op0=ALU.mult, op1=ALU.mult), reads=[x2B, ssFB, fnbB], writes=[yoB])
        dst = o_ys if tile == 32 else o_y[tile * 128:(tile + 1) * 128, :]
        P.dma("sp", lambda e: e.dma_start(out=dst, in_=yo), reads=[yoB])

    phaseF_tile(32)
    load_mod(False, (5,))
    for tile in range(32):
        phaseF_tile(tile)
    return finish()


_CACHE = {}


def _bucket(rel):
    rel = np.asarray(rel, np.int64)
    n = np.abs(rel)
    nf = np.maximum(n, 1).astype(np.float32)
    large = 8 + (np.log(nf / np.float32(8)) / np.float32(math.log(128 / 8)) * np.float32(8)).astype(np.int32)
    large = np.minimum(large, 15)
    return np.where(rel > 0, 16, 0) + np.where(n < 8, n, large)


def _consts(pi):
    cst = np.zeros((128, 1600), np.float32)
    cst[:, 0:128] = np.eye(128, dtype=np.float32)
    cst[:, 128:256] = np.eye(128, dtype=np.float32)[::-1]
    offs = [64 * pi, 64 * pi + 128, 64 * pi + 256, 128, 0, 32]
    oh = np.zeros((32, 6, 192), np.float32)
    for b, off in enumerate(offs):
        j = np.arange(192)
        bk = _bucket(127 - j - off)
        oh[bk, b, j] = 1.0
    cst[0:32, 256:1408] = oh.reshape(32, 1152)
    cst[0:64, 1408:1472] = 1.0
    cst[64:128, 1408:1472] = float(pi)
    cst[:, 1472:1600] = np.triu(np.ones((128, 128), np.float32), 1)
    return cst


def _consts2():
    c = np.zeros((128, 520), np.float32)
    e = np.arange(256, dtype=np.float32)
    c[:, 0:256] = e * CAP + 1.0
    c[:, 256:512] = 4.0 * e + 1.0
    c[0:16, 512] = 1.0; c[32:48, 513] = 1.0
    c[0:16, 514] = 1.0; c[32:48, 514] = 1.0
    c[:, 515] = 1.0
    return c


def _rope_tab(pos):
    half = 16
    inv = (10000.0 ** (-np.arange(half, dtype=np.float32) / half)).astype(np.float32)
    ang = pos.astype(np.float32)[:, None] * inv
    c = np.cos(ang).astype(np.float32); s = np.sin(ang).astype(np.float32)
    return np.concatenate([c, c, -s, s], axis=1).astype(np.float32)


def _in_maps(inp, cores=range(8)):
    f = lambda a: np.ascontiguousarray(np.asarray(a, dtype=np.float32))
    xp = f(inp["x_prompt"]); xs = f(inp["x_sample"])
    in_maps = []
    for c in cores:
        b, pi = c // 2, c % 2
        xo = xp[b].reshape(64, 2, 64, 1024)[:, pi].reshape(4096, 1024)
        pos_own = (np.arange(64)[:, None] * 128 + pi * 64 + np.arange(64)[None, :]).reshape(-1)
        x_s = np.zeros((128, 1024), np.float32); x_s[0:16] = xs[2 * c]; x_s[32:48] = xs[2 * c + 1]
        rs = np.zeros((128, 64), np.float32); rs[:, 0:32] = 1.0
        rs[0:16] = _rope_tab(4096 + np.arange(16)); rs[32:48] = rs[0:16]
        m = {
            "x_all": xp[b], "x_own": f(xo), "x_s": x_s,
            "clat": f(inp["cache_mla_latent"][0, 2 * c:2 * c + 2]), "ckr": f(inp["cache_mla_krope"][0, 2 * c:2 * c + 2]),
            "cdk": f(inp["cache_diff_k"][0, 2 * c:2 * c + 2]).reshape(2, 4096, 512),
            "cdv": f(inp["cache_diff_v"][0, 2 * c:2 * c + 2]).reshape(2, 4096, 512),
            "c3": f(np.stack([inp["c_prompt"][b], inp["c_sample"][2 * c], inp["c_sample"][2 * c + 1]])),
            "w_ada": f(inp["w_ada"][0]), "b_ada": f(inp["b_ada"]), "norm_attn": f(inp["norm_attn"]), "w_in": f(inp["w_in"][0]),
            "qg": f(inp["mla_q_norm"]), "kvg": f(inp["mla_kv_norm"]), "w_uq": f(inp["w_uq"][0]), "w_ukv": f(inp["w_ukv"][0]),
            "lam4": f(np.concatenate([inp["lambda_q1"], inp["lambda_k1"], inp["lambda_q2"], inp["lambda_k2"]], 0)),
            "subln": f(inp["diff_subln"]).reshape(128, 1), "relb": f(inp["rel_bias"]).reshape(32, 8),
            "w_out": f(inp["w_out"][0]), "norm_ffn": f(inp["norm_ffn"]), "w_router": f(inp["w_router"][0]), "rbias": f(inp["router_bias"]),
            "w_sgu": f(inp["w_shared_gu"][0]), "w_sdn": f(inp["w_shared_down"][0]),
            "fnorm": f(inp["final_norm"]).reshape(1, 1024),
            "cst": _consts(pi), "cst2": _consts2(), "rope_all": _rope_tab(np.arange(8192)), "rope_own": _rope_tab(pos_own), "rope_s": rs,
        }
        if STAGE > 1:
            m["w_gu"] = f(inp["w_exp_gu"][0]); m["w_dn"] = f(inp["w_exp_down"][0])
        in_maps.append(m)
    return in_maps


def kernel(**inp):
    if "prog" not in _CACHE:
        _CACHE["prog"] = build_program()
    nc, _es = _CACHE["prog"]
    in_maps = _in_maps(inp)
    res = run_bass_kernel_spmd(nc, in_maps, core_ids=list(range(8))).results
    y_p = np.zeros((4, 8192, 1024), np.float32); y_s = np.zeros((16, 16, 1024), np.float32)
    lat_p = np.zeros((1, 4, 8192, 256), np.float32); kpe_p = np.zeros((1, 4, 8192, 32), np.float32)
    dk_p = np.zeros((1, 4, 8192, 4, 2, 64), np.float32); dv_p = np.zeros((1, 4, 8192, 4, 128), np.float32)
    lat_s = np.zeros((1, 16, 16, 256), np.float32); kpe_s = np.zeros((1, 16, 16, 32), np.float32)
    dk_s = np.zeros((1, 16, 16, 4, 2, 64), np.float32); dv_s = np.zeros((1, 16, 16, 4, 128), np.float32)
    for c in range(8):
        b, pi = c // 2, c % 2
        r = res[c]
        y_p[b].reshape(64, 2, 64, 1024)[:, pi] = r["o_y"].reshape(64, 64, 1024)
        if pi == 0:
            lat_p[0, b] = r["o_lat"]; kpe_p[0, b] = r["o_kpe"]
            dk_p[0, b] = r["o_dk"].reshape(8192, 4, 2, 64); dv_p[0, b] = r["o_dv"].reshape(8192, 4, 128)
        for s in range(2):
            sl = slice(32 * s, 32 * s + 16)
            y_s[2 * c + s] = r["o_ys"][sl]
            lat_s[0, 2 * c + s] = r["o_lats"][sl]; kpe_s[0, 2 * c + s] = r["o_kpes"][sl]
            dk_s[0, 2 * c + s] = r["o_dks"][sl].reshape(16, 4, 2, 64); dv_s[0, 2 * c + s] = r["o_dvs"][sl].reshape(16, 4, 128)
    return (y_p, y_s, lat_p, kpe_p, dk_p, dv_p, lat_s, kpe_s, dk_s, dv_s)
```

```python
import math
import os
from contextlib import ExitStack
import numpy as np
import concourse.bass as bass
import concourse.mybir as mybir
from concourse.bass_utils import run_bass_kernel_spmd

F32 = mybir.dt.float32; BF16 = mybir.dt.bfloat16; I32 = mybir.dt.int32
ALU = mybir.AluOpType; AF = mybir.ActivationFunctionType; AX = mybir.AxisListType
ENG = ("pe", "act", "dve", "pool", "sp")
EPS = 1e-6
MLA_SCALE = 96 ** -0.5
DIFF_SCALE = 64 ** -0.5
LAM_INIT = 0.8 - 0.6 * math.exp(0.0)
CAP = 640
NST = CAP // 128
HALF = 128 * CAP
BIG = 1.0e6
STAGE = 9
KCUT = int(os.environ.get('KCUT', '99'))


class Buf:
    __slots__ = ("w", "r", "excl")

    def __init__(self, excl=False):
        self.w = []; self.r = []; self.excl = excl


def _prune(toks):
    best = {}
    for t in toks:
        k = (t[0], t[1])
        if k not in best or best[k][2] < t[2]:
            best[k] = t
    return list(best.values())


class Prog:
    def __init__(self, nc, es, nds=48):
        self.nc = nc
        self.ops = {e: [] for e in ENG}
        self.need = {e: set() for e in ENG}
        self.psem = {e: es.enter_context(nc.semaphore("pg_" + e)) for e in ENG}
        self.dsem = [es.enter_context(nc.semaphore("dq%d" % i)) for i in range(nds)]
        self.dcnt = [0] * nds
        self.dn = 0; self.dns = 0; self.NHW = 32
        self.nops = 0; self.limit = int(os.environ.get("KOPS", "100000000"))
        self.pend = {e: [] for e in ENG}

    def _deps(self, eng, reads, writes, waits):
        toks = list(waits) + self.pend[eng]
        self.pend[eng] = []
        for b in reads:
            toks += b.w
            if b.excl:
                toks += [t for t in b.r if not (t[0] == "c" and t[1] == eng)]
        for b in writes:
            toks += b.w; toks += b.r
        res = []
        for t in _prune(toks):
            if t[0] == "c":
                if t[1] == eng and eng == "pe":
                    continue
                self.need[t[1]].add(t[2])
            res.append(t)
        return res

    def _upd(self, tok, reads, writes):
        for b in reads:
            b.r = _prune(b.r + [tok])
        for b in writes:
            b.w = [tok]; b.r = []

    def op(self, eng, fn, reads=(), writes=(), waits=()):
        self.nops += 1
        if self.nops > self.limit:
            return ("d", 0, 0)
        if os.environ.get("KDBG"):
            import inspect
            fr = inspect.stack()[1]; fr2 = inspect.stack()[2]
            print("OP", self.nops, eng, fr.lineno, fr2.lineno)
        deps = self._deps(eng, reads, writes, waits)
        tok = ("c", eng, len(self.ops[eng]))
        self.ops[eng].append((fn, deps, None))
        self._upd(tok, reads, writes)
        return tok

    def dma(self, eng, fn, reads=(), writes=(), waits=()):
        self.nops += 1
        if self.nops > self.limit:
            return ("d", 0, 0)
        if os.environ.get("KDBG"):
            import inspect
            fr = inspect.stack()[1]; fr2 = inspect.stack()[2]
            print("DMA", self.nops, eng, fr.lineno, fr2.lineno)
        if eng == "pool":
            i = self.NHW + self.dns; self.dns = (self.dns + 1) % (len(self.dsem) - self.NHW)
        else:
            i = self.dn; self.dn = (self.dn + 1) % self.NHW
        w = list(waits)
        if self.dcnt[i] > 0:
            w.append(("d", i, self.dcnt[i]))
        deps = self._deps(eng, reads, writes, w)
        self.dcnt[i] += 16
        tok = ("d", i, self.dcnt[i])
        self.ops[eng].append((fn, deps, i))
        self._upd(tok, reads, writes)
        return tok

    def barrier(self):
        toks = []
        for e in ENG:
            for idx in range(len(self.ops[e]) - 1, -1, -1):
                if self.ops[e][idx][2] is None and self.ops[e][idx][0] is not None:
                    toks.append(("c", e, idx))
                    break
        for i, c in enumerate(self.dcnt):
            if c > 0:
                toks.append(("d", i, c))
        for e in ENG:
            self.pend[e] = self.pend[e] + toks

    def check(self):
        rank = {e: {idx: i + 1 for i, idx in enumerate(sorted(self.need[e]))} for e in ENG}
        pc = {e: 0 for e in ENG}; cs = {e: 0 for e in ENG}; ds = [0] * len(self.dsem)
        while True:
            prog = False
            for e in ENG:
                while pc[e] < len(self.ops[e]):
                    fn, deps, di = self.ops[e][pc[e]]
                    ok = True
                    for t in deps:
                        if t[0] == "c":
                            if t[2] not in rank[t[1]] or cs[t[1]] < rank[t[1]][t[2]]:
                                ok = False; break
                        elif ds[t[1]] < t[2]:
                            ok = False; break
                    if not ok:
                        break
                    if di is not None:
                        ds[di] += 16
                    elif pc[e] in rank[e]:
                        cs[e] += 1
                    pc[e] += 1; prog = True
            if not prog:
                break
        stuck = {e: (pc[e], len(self.ops[e]), self.ops[e][pc[e]][1]) for e in ENG if pc[e] < len(self.ops[e])}
        assert not stuck, "DEADLOCK %r" % (stuck,)
        print("sync check ok:", {e: len(self.ops[e]) for e in ENG}, {e: len(self.need[e]) for e in ENG})

    def emit(self, block):
        for e in ENG:
            pass
        self.limit = 10 ** 9
        self.barrier()
        self.op("sp", None)
        print("total ops", self.nops)
        self.check()
        rank = {e: {idx: i + 1 for i, idx in enumerate(sorted(self.need[e]))} for e in ENG}

        def run(e, h):
            waited = {}
            for idx, (fn, deps, di) in enumerate(self.ops[e]):
                for t in deps:
                    if t[0] == "c":
                        sem = self.psem[t[1]]; val = rank[t[1]][t[2]]
                    else:
                        sem = self.dsem[t[1]]; val = t[2]
                    key = (t[0], t[1])
                    if waited.get(key, 0) >= val:
                        continue
                    waited[key] = val
                    h.wait_ge(sem, val)
                if fn is None:
                    continue
                ins = fn(h)
                if di is not None:
                    ins.then_inc(self.dsem[di], 16)
                elif idx in rank[e]:
                    ins.then_inc(self.psem[e], 1)

        @block.tensor
        def _(h): run("pe", h)

        @block.scalar
        def _(h): run("act", h)

        @block.vector
        def _(h): run("dve", h)

        @block.gpsimd
        def _(h): run("pool", h)

        @block.sync
        def _(h): run("sp", h)


def build_program():
    nc = bass.Bass("TRN2", target_bir_lowering=False)
    es = ExitStack()
    P = Prog(nc, es)

    def din(name, shape, dt=F32):
        return nc.dram_tensor(name, list(shape), dt, kind="ExternalInput").ap()

    def dout(name, shape, dt=F32):
        return nc.dram_tensor(name, list(shape), dt, kind="ExternalOutput").ap()

    def dscr(name, shape, dt):
        return nc.dram_tensor(name, list(shape), dt).ap()

    x_all = din("x_all", [8192, 1024]); x_own = din("x_own", [4096, 1024]); x_s = din("x_s", [128, 1024])
    clat = din("clat", [2, 4096, 256]); ckr = din("ckr", [2, 4096, 32])
    cdk = din("cdk", [2, 4096, 512]); cdv = din("cdv", [2, 4096, 512])
    c3 = din("c3", [3, 1024])
    w_ada = din("w_ada", [1024, 6144]); b_ada = din("b_ada", [1, 6144])
    norm_attn = din("norm_attn", [1, 1024]); w_in = din("w_in", [1024, 2080])
    qg = din("qg", [1, 256]); kvg = din("kvg", [1, 256])
    w_uq = din("w_uq", [256, 768]); w_ukv = din("w_ukv", [256, 1024])
    lam4 = din("lam4", [4, 64]); subln = din("subln", [128, 1]); relb = din("relb", [32, 8])
    w_out = din("w_out", [1024, 1024]); norm_ffn = din("norm_ffn", [1, 1024])
    w_router = din("w_router", [1024, 256]); rbias = din("rbias", [1, 256])
    if STAGE > 1:
        w_gu = din("w_gu", [256, 1024, 512]); w_dn = din("w_dn", [256, 256, 1024])
    w_sgu = din("w_sgu", [1024, 512]); w_sdn = din("w_sdn", [256, 1024]); fnorm = din("fnorm", [1, 1024])
    cst = din("cst", [128, 1600]); cst2 = din("cst2", [128, 520]); rope_all = din("rope_all", [8192, 64]); rope_own = din("rope_own", [4096, 64])
    rope_s = din("rope_s", [128, 64])

    o_y = dout("o_y", [4096, 1024]); o_ys = dout("o_ys", [128, 1024])
    o_lat = dout("o_lat", [8192, 256]); o_kpe = dout("o_kpe", [8192, 32])
    o_dk = dout("o_dk", [8192, 512]); o_dv = dout("o_dv", [8192, 512])
    o_lats = dout("o_lats", [128, 256]); o_kpes = dout("o_kpes", [128, 32])
    o_dks = dout("o_dks", [128, 512]); o_dvs = dout("o_dvs", [128, 512])

    modD = dscr("modD", [3, 6144], F32); gD = dscr("gD", [8, 6, 192], F32)
    KTm = dscr("KTm", [8, 96, 8192], BF16); Vm = dscr("Vm", [8, 128, 64, 65], BF16)
    KTd = dscr("KTd", [4, 128, 8192], BF16); Vd = dscr("Vd", [4, 128, 64, 128], BF16)
    QTm = dscr("QTm", [8, 96, 4096], BF16); QTd = dscr("QTd", [4, 2, 128, 4096], BF16)
    sKTm = dscr("sKTm", [2, 8, 96, 4096], BF16); sVm = dscr("sVm", [2, 8, 128, 32, 65], BF16)
    sKTd = dscr("sKTd", [2, 4, 128, 4096], BF16); sVd = dscr("sVd", [2, 4, 128, 32, 128], BF16)
    OT = dscr("OT", [1024, 4224], BF16)
    X1 = dscr("X1", [4224, 1024], F32)
    if STAGE > 1:
        XGs = [dscr("XGa", [HALF, 1024], BF16), dscr("XGb", [HALF, 1024], BF16)]
        YGs = [dscr("YGa", [HALF, 1024], BF16), dscr("YGb", [HALF, 1024], BF16)]

    def sb(name, shape, dt):
        return es.enter_context(nc.sbuf_tensor(name, list(shape), dt))

    regs = {}

    def getbc(e):
        if "bc" not in regs:
            regs["bc"] = e.to_reg(HALF - 1)
        return regs["bc"]

    def finish():
        with nc.Block() as block:
            P.emit(block)
        return nc, es

    CF = sb("CF", [128, 1600], F32); CFb = Buf()
    C2 = sb("C2", [128, 520], F32)
    identb = sb("identb", [128, 128], BF16); Ub = sb("Ub", [128, 128], BF16)
    onesb = sb("onesb", [128, 128], BF16); onesf = sb("onesf", [128, 128], F32)
    mpib = sb("mpib", [128, 64], BF16); cB = Buf()
    identf = CF[:, 0:128]; Jf = CF[:, 128:256]
    M = [sb("M%d" % i, [128, 1024], F32) for i in range(6)]; MB = [Buf() for _ in range(6)]
    gains = sb("gains", [128, 512], F32); gB = Buf()
    Bb = sb("Bb", [128, 8, 384], F32); BbB = Buf()
    lamt = sb("lamt", [128, 8], F32); lamB = Buf()
    nkT = sb("nkT", [128, 8, 128], BF16); ndkT = sb("ndkT", [128, 4, 128], BF16)
    nv = sb("nv", [128, 8, 65], BF16); ndv = sb("ndv", [128, 4, 128], BF16)
    nqT = sb("nqT", [128, 8, 128], BF16); ndqT = sb("ndqT", [128, 2, 4, 128], BF16); nB = Buf()
    didx = sb("didx", [128, 33, 32], I32); gate8 = sb("gate8", [128, 33, 16], F32); dgB = [Buf() for _ in range(33)]
    cntbc = sb("cntbc", [128, 256], F32); cntB = Buf()
    nhalf = sb("nhalf", [128, 8], F32)
    ARN = 35584
    arena = sb("arena", [128, ARN], F32)
    ps = [es.enter_context(nc.psum_tensor("psb%d" % i, [128, 512], F32)) for i in range(8)]
    psB = [Buf(excl=True) for _ in range(8)]
    psn = [0]

    def nextps():
        i = psn[0]; psn[0] = (i + 1) % 8
        return i

    class Arena:
        def __init__(self): self.off = 0

        def reset(self):
            self.off = 0; P.barrier()

        def f32(self, shape):
            n = int(np.prod(shape[1:])); a = arena[:, self.off:self.off + n]; self.off += n
            assert self.off <= ARN, self.off
            if len(shape) == 3:
                a = a.rearrange("p (a b) -> p a b", a=shape[1])
            return a

        def bf(self, shape):
            n = int(np.prod(shape[1:])); nf = (n + 1) // 2
            a = arena[:, self.off:self.off + nf].bitcast(BF16)[:, 0:n]; self.off += nf
            assert self.off <= ARN, self.off
            if len(shape) == 3:
                a = a.rearrange("p (a b) -> p a b", a=shape[1])
            elif len(shape) == 4:
                a = a.rearrange("p (a b c) -> p a b c", a=shape[1], b=shape[2])
            return a

        def i32(self, shape):
            n = int(np.prod(shape[1:])); a = arena[:, self.off:self.off + n].bitcast(I32); self.off += n
            return a

    AR = Arena()

    def rms_ops(src_ap, srcB, D, junk, ssb, tag):
        ss, ssB = ssb
        P.op("pool", lambda e: e.memset(ss, 0.0), writes=[ssB])
        P.op("act", lambda e: e.activation(out=junk, in_=src_ap, func=AF.Square, accum_out=ss), reads=[srcB], writes=[ssB])
        P.op("dve", lambda e: e.tensor_scalar(out=ss, in0=ss, scalar1=1.0 / D, scalar2=EPS, op0=ALU.mult, op1=ALU.add), reads=[ssB], writes=[ssB])
        P.op("pool", lambda e: e.tensor_tensor(out=ss, in0=ss, in1=nhalf[:, 0:1], op=ALU.pow), reads=[ssB], writes=[ssB])

    P.dma("sp", lambda e: e.dma_start(out=CF[:], in_=cst), writes=[CFb])
    P.dma("sp", lambda e: e.dma_start(out=C2[:], in_=cst2), writes=[cB])
    P.op("dve", lambda e: e.tensor_copy(out=identb[:], in_=CF[:, 0:128]), reads=[CFb], writes=[cB])
    P.op("dve", lambda e: e.tensor_copy(out=Ub[:], in_=CF[:, 1472:1600]), reads=[CFb], writes=[cB])
    P.op("dve", lambda e: e.tensor_copy(out=mpib[:], in_=CF[:, 1408:1472]), reads=[CFb], writes=[cB])
    P.op("pool", lambda e: e.memset(onesb[:], 1.0), writes=[cB])
    P.op("pool", lambda e: e.memset(onesf[:], 1.0), writes=[cB])
    P.op("pool", lambda e: e.memset(nv[:], 1.0), writes=[nB])
    P.op("pool", lambda e: e.memset(ndqT[:], 0.0), writes=[nB])
    P.op("pool", lambda e: e.memset(cntbc[:], 0.0), writes=[cntB])
    P.op("pool", lambda e: e.memset(nhalf[:], -0.5), writes=[cB])
    P.dma("sp", lambda e: e.dma_start(out=gains[:, 0:256], in_=qg.broadcast_to([128, 256])), writes=[gB])
    P.dma("sp", lambda e: e.dma_start(out=gains[:, 256:512], in_=kvg.broadcast_to([128, 256])), writes=[gB])
    cT = AR.f32([128, 8, 3]); cTB = Buf()
    modS = AR.f32([128, 6144]); modB = Buf()
    badd = AR.f32([128, 6144]); baB = Buf()
    wad = [AR.f32([128, 8, 512]) for _ in range(2)]; wadB = [Buf(), Buf()]
    for k in range(8):
        P.dma("sp", lambda e, k=k: e.dma_start(out=cT[:, k, :], in_=c3[:, k * 128:(k + 1) * 128].rearrange("c p -> p c"),
                                             allow_slow_non_contiguous=True), writes=[cTB])
    P.op("act", lambda e: e.activation(out=cT, in_=cT, func=AF.Silu), reads=[cTB], writes=[cTB])
    P.dma("sp", lambda e: e.dma_start(out=badd[0:3, :], in_=b_ada.broadcast_to([3, 6144])), writes=[baB])
    for n in range(12):
        wb = wad[n % 2]; wB = wadB[n % 2]
        P.dma("sp", lambda e, wb=wb, n=n: e.dma_start(out=wb, in_=w_ada[:, n * 512:(n + 1) * 512].rearrange("(k p) n -> p k n", p=128)), writes=[wB])
        pi = nextps()

        def f(e, wb=wb, pi=pi):
            for k in range(8):
                ins = e.matmul(ps[pi][0:3, :], lhsT=cT[:, k, :], rhs=wb[:, k, :], start=(k == 0), stop=(k == 7))
            return ins
        P.op("pe", f, reads=[cTB, wB], writes=[psB[pi]])
        P.op("dve", lambda e, pi=pi, n=n: e.tensor_tensor(out=modS[0:3, n * 512:(n + 1) * 512], in0=ps[pi][0:3, :],
                                                         in1=badd[0:3, n * 512:(n + 1) * 512], op=ALU.add),
             reads=[psB[pi], baB], writes=[modB])
    P.dma("sp", lambda e: e.dma_start(out=modD, in_=modS[0:3, :]), reads=[modB])
    P.barrier()
    if KCUT == 0:
        return finish()

    def load_mod(sample, which):
        src = {0: 1, 1: 0, 2: 2, 3: 4, 4: 3, 5: 5}
        for j in which:
            c0 = src[j] * 1024
            if not sample:
                P.dma("sp", lambda e, j=j, c0=c0: e.dma_start(out=M[j][:], in_=modD[0:1, c0:c0 + 1024].broadcast_to([128, 1024])), writes=[MB[j]])
            else:
                P.dma("sp", lambda e, j=j, c0=c0: e.dma_start(out=M[j][0:32, :], in_=modD[1:2, c0:c0 + 1024].broadcast_to([32, 1024])), writes=[MB[j]])
                P.dma("sp", lambda e, j=j, c0=c0: e.dma_start(out=M[j][32:128, :], in_=modD[2:3, c0:c0 + 1024].broadcast_to([96, 1024])), writes=[MB[j]])
            if j in (0, 3):
                gsrc = norm_attn if j == 0 else norm_ffn
                tmp = AR_tmp[:]
                P.dma("sp", lambda e, gsrc=gsrc: e.dma_start(out=tmp, in_=gsrc.broadcast_to([128, 1024])), writes=[tmpB])
                P.op("dve", lambda e, j=j: e.scalar_tensor_tensor(out=M[j][:], in0=M[j][:], scalar=1.0, in1=tmp, op0=ALU.add, op1=ALU.mult),
                     reads=[tmpB, MB[j]], writes=[MB[j]])

    AR_tmp = sb("modtmp", [128, 1024], F32); tmpB = Buf()
    load_mod(False, range(6))

    lv = AR.f32([128, 4, 64]); lvB = Buf()
    P.dma("sp", lambda e: e.dma_start(out=lv, in_=bass.AP(lam4.tensor, 0, [[0, 128], [64, 4], [1, 64]])), writes=[lvB])
    lp = AR.f32([128, 2, 64]); l2 = AR.f32([128, 4]); l2B = Buf()
    P.op("dve", lambda e: e.tensor_tensor(out=lp[:, 0, :], in0=lv[:, 0, :], in1=lv[:, 1, :], op=ALU.mult), reads=[lvB], writes=[l2B])
    P.op("dve", lambda e: e.tensor_tensor(out=lp[:, 1, :], in0=lv[:, 2, :], in1=lv[:, 3, :], op=ALU.mult), reads=[lvB, l2B], writes=[l2B])
    P.op("dve", lambda e: e.tensor_reduce(out=l2[:, 0:2], in_=lp, axis=AX.X, op=ALU.add), reads=[l2B], writes=[l2B])
    P.op("act", lambda e: e.activation(out=l2[:, 2:4], in_=l2[:, 0:2], func=AF.Exp), reads=[l2B], writes=[l2B])
    P.op("dve", lambda e: e.tensor_tensor(out=lamt[:, 0:1], in0=l2[:, 3:4], in1=l2[:, 2:3], op=ALU.subtract), reads=[l2B], writes=[lamB])
    P.op("dve", lambda e: e.tensor_scalar(out=lamt[:, 0:1], in0=lamt[:, 0:1], scalar1=-LAM_INIT, scalar2=None, op0=ALU.add), reads=[lamB], writes=[lamB])
    P.dma("sp", lambda e: e.dma_start(out=lamt[:, 1:2], in_=subln), writes=[lamB])
    P.op("dve", lambda e: e.tensor_scalar(out=lamt[:, 1:2], in0=lamt[:, 1:2], scalar1=(1.0 - LAM_INIT) * math.sqrt(128.0), scalar2=None, op0=ALU.mult),
         reads=[lamB], writes=[lamB])
    rb = AR.f32([128, 8]); rbB = Buf()
    P.dma("sp", lambda e: e.dma_start(out=rb[0:32, :], in_=relb), writes=[rbB])
    gS = AR.f32([128, 1152]); gSB = Buf()
    for half in range(3):
        pi = nextps()
        P.op("pe", lambda e, pi=pi, half=half: e.matmul(ps[pi][0:8, 0:384], lhsT=rb[0:32, :], rhs=CF[0:32, 256 + half * 384:256 + (half + 1) * 384], start=True, stop=True),
             reads=[rbB, CFb], writes=[psB[pi]])
        P.op("dve", lambda e, pi=pi, half=half: e.tensor_copy(out=gS[0:8, half * 384:(half + 1) * 384], in_=ps[pi][0:8, 0:384]), reads=[psB[pi]], writes=[gSB])
    tgd = P.dma("sp", lambda e: e.dma_start(out=gD.rearrange("a b c -> a (b c)"), in_=gS[0:8, :]), reads=[gSB])
    Tp = AR.f32([128, 8, 384]); TpB = Buf()
    for blk in range(6):
        P.dma("sp", lambda e, blk=blk: e.dma_start(out=Tp[:, :, blk * 64:(blk + 1) * 64],
                                                  in_=bass.AP(gD.tensor, blk * 192, [[1, 128], [1152, 8], [1, 64]])), writes=[TpB], waits=[tgd])
    for hm in range(8):
        pi = nextps()
        P.op("pe", lambda e, pi=pi, hm=hm: e.matmul(ps[pi][:, 0:384], lhsT=Jf, rhs=Tp[:, hm, :], start=True, stop=True), reads=[TpB, CFb], writes=[psB[pi]])
        P.op("dve", lambda e, pi=pi, hm=hm: e.tensor_copy(out=Bb[:, hm, :], in_=ps[pi][:, 0:384]), reads=[psB[pi]], writes=[BbB])

    if KCUT == 1:
        return finish()
    AR.reset()
    winb = AR.bf([128, 8, 2080]); wuqb = AR.bf([128, 2, 768]); wukvb = AR.bf([128, 2, 1024]); wB_ = Buf()
    for k in range(8):
        P.dma("pool", lambda e, k=k: e.dma_start(out=winb[:, k, :], in_=w_in[k * 128:(k + 1) * 128, :]), writes=[wB_])
    P.dma("pool", lambda e: e.dma_start(out=wuqb, in_=w_uq.rearrange("(k p) n -> p k n", p=128)), writes=[wB_])
    P.dma("pool", lambda e: e.dma_start(out=wukvb, in_=w_ukv.rearrange("(k p) n -> p k n", p=128)), writes=[wB_])
    NR = 2
    xt = [AR.f32([128, 1024]) for _ in range(NR)]; xtB = [Buf() for _ in range(NR)]
    rp = [AR.f32([128, 64]) for _ in range(NR)]; rpB = [Buf() for _ in range(NR)]
    hf = AR.f32([128, 1024]); hfB = Buf()
    hb = AR.bf([128, 1024]); hbB = Buf()
    junk = AR.bf([128, 1024]); junkB = Buf()
    hT = [AR.bf([128, 8, 128]) for _ in range(NR)]; hTB = [Buf() for _ in range(NR)]
    ssA = AR.f32([128, 8]); ssB_ = [Buf() for _ in range(8)]
    latf = [AR.f32([128, 256]) for _ in range(NR)]; latfB = [Buf() for _ in range(NR)]
    kpef = [AR.f32([128, 32]) for _ in range(NR)]; kpefB = [Buf() for _ in range(NR)]
    rtmp = AR.f32([128, 8, 32]); rtmpB = Buf(); rtmp2 = AR.f32([128, 8, 32])
    dkf = [AR.f32([128, 512]) for _ in range(NR)]; dkfB = [Buf() for _ in range(NR)]
    dvf = [AR.f32([128, 512]) for _ in range(NR)]; dvfB = [Buf() for _ in range(NR)]
    latb = AR.bf([128, 256]); latbB = Buf(); kpeb = AR.bf([128, 32]); kpebB = Buf()
    dkb = AR.bf([128, 512]); dkbB = Buf()
    latT = AR.bf([128, 2, 128]); latTB = Buf()
    kcomb = AR.bf([128, 8, 96]); kcombB = Buf()
    kTst = AR.bf([128, 8, 512]); kTstB = Buf(); dkTst = AR.bf([128, 4, 512]); dkTstB = Buf()
    vst = AR.bf([128, 8, 4, 65]); vstB = Buf(); dvst = AR.bf([128, 4, 4, 128]); dvstB = Buf()
    qnb = AR.bf([128, 256]); qnbB = Buf(); qnT = AR.bf([128, 2, 128]); qnTB = Buf()
    qc = AR.bf([128, 8, 96]); qcB = Buf(); dqb = AR.bf([128, 512]); dqbB = Buf()
    qTst = AR.bf([128, 8, 512]); qTstB = Buf(); dqTst = AR.bf([128, 2, 4, 512]); dqTstB = Buf()
    P.op("pool", lambda e: e.memset(vst, 1.0), writes=[vstB])
    P.op("pool", lambda e: e.memset(dqTst, 0.0), writes=[dqTstB])

    def transposes(src_fn, n, rows, dstB_reads, dst_ap, dstB, eng="dve"):
        pi = nextps(); pb = ps[pi][:].bitcast(BF16)

        def f(e):
            for k in range(n):
                ins = e.transpose(out=pb[0:rows, k * 128:(k + 1) * 128], in_=src_fn(k), identity=identb[:])
            return ins
        P.op("pe", f, reads=dstB_reads + [cB], writes=[psB[pi]])
        src = pb[0:rows, 0:n * 128].rearrange("p (a b) -> p a b", a=n)
        if eng == "act":
            P.op("act", lambda e: e.activation(out=dst_ap, in_=src, func=AF.Copy), reads=[psB[pi]], writes=[dstB])
        else:
            P.op(eng, lambda e: e.tensor_copy(out=dst_ap, in_=src), reads=[psB[pi]], writes=[dstB])

    def norm_h(xa, xB, Aj, Bj):
        rms_ops(xa, xB, 1024, junk, (ssA[:, 0:1], ssB_[0]), "x")
        P.op("dve", lambda e: e.scalar_tensor_tensor(out=hf, in0=xa, scalar=ssA[:, 0:1], in1=M[Aj][:], op0=ALU.mult, op1=ALU.mult),
             reads=[xB, ssB_[0], MB[Aj]], writes=[hfB])
        P.op("pool", lambda e: e.tensor_tensor(out=hb, in0=hf, in1=M[Bj][:], op=ALU.add), reads=[hfB, MB[Bj]], writes=[hbB])

    def rope(eng, dst, srcv, tab, nh, reads, writes):
        cs = tab[:, 0:32].unsqueeze(1).broadcast_to([128, nh, 32])
        s1 = tab[:, 32:48].unsqueeze(1).broadcast_to([128, nh, 16])
        s2 = tab[:, 48:64].unsqueeze(1).broadcast_to([128, nh, 16])
        t1 = rtmp[:, 0:nh, :]; t2 = rtmp2[:, 0:nh, :]
        P.op(eng, lambda e: e.tensor_tensor(out=t1, in0=srcv, in1=cs, op=ALU.mult), reads=reads, writes=[rtmpB])
        P.op(eng, lambda e: e.tensor_tensor(out=t2[:, :, 0:16], in0=srcv[:, :, 16:32], in1=s1, op=ALU.mult), reads=reads + [rtmpB], writes=[rtmpB])
        P.op(eng, lambda e: e.tensor_tensor(out=t2[:, :, 16:32], in0=srcv[:, :, 0:16], in1=s2, op=ALU.mult), reads=reads + [rtmpB], writes=[rtmpB])
        P.op(eng, lambda e: e.tensor_tensor(out=dst, in0=t1, in1=t2, op=ALU.add), reads=[rtmpB], writes=writes)

    def kside_derive(t4, dests, latb_, kpeb_, dkb_, dvb_src, rB):
        kT_dst, dkT_dst, v_dst, dv_dst = dests
        transposes(lambda k: latb_[:, k * 128:(k + 1) * 128], 2, 128, rB, latT, latTB)
        pa = nextps(); pb_ = nextps()

        def f(e):
            for hh, pi in ((0, pa), (1, pb_)):
                for k in range(2):
                    ins = e.matmul(ps[pi][:], lhsT=latT[:, k, :], rhs=wukvb[:, k, hh * 512:(hh + 1) * 512], start=(k == 0), stop=(k == 1))
            return ins
        P.op("pe", f, reads=[latTB, wB_], writes=[psB[pa], psB[pb_]])
        for hh, pi in ((0, pa), (1, pb_)):
            v4 = ps[pi][:].rearrange("p (h c) -> p h c", h=4)
            P.op("dve", lambda e, v4=v4, hh=hh: e.tensor_copy(out=kcomb[:, hh * 4:hh * 4 + 4, 0:64], in_=v4[:, :, 0:64]), reads=[psB[pi]], writes=[kcombB])
            P.op("act", lambda e, v4=v4, hh=hh: e.activation(out=v_dst[:, hh * 4:hh * 4 + 4, t4, 1:65] if v_dst is not None else nv[:, hh * 4:hh * 4 + 4, 1:65],
                                                            in_=v4[:, :, 64:128], func=AF.Copy), reads=[psB[pi]], writes=[vstB])
        P.op("pool", lambda e: e.tensor_copy(out=kcomb[:, :, 64:96], in_=kpeb_.unsqueeze(1).broadcast_to([128, 8, 32])), reads=rB + [kcombB], writes=[kcombB])
        transposes(lambda h: kcomb[:, h, :], 8, 96, [kcombB], kT_dst, kTstB, eng="act")
        transposes(lambda h: dkb_[:, h * 128:(h + 1) * 128], 4, 128, rB, dkT_dst, dkTstB)
        if dvb_src is not None:
            P.op("pool", lambda e: e.tensor_copy(out=dv_dst, in_=dvb_src.rearrange("p (h c) -> p h c", h=4)), reads=rB, writes=[dvstB])

    def flush_k(sidx, q4):
        c0 = q4 * 512
        if sidx is None:
            kd, dkd, vd, dvd = KTm, KTd, Vm, Vd
        else:
            kd, dkd, vd, dvd = sKTm[sidx], sKTd[sidx], sVm[sidx], sVd[sidx]
        P.dma("sp", lambda e: e.dma_start(out=kd[:, :, c0:c0 + 512].rearrange("h d t -> d h t"), in_=kTst[0:96, :, :]), reads=[kTstB])
        P.dma("sp", lambda e: e.dma_start(out=dkd[:, :, c0:c0 + 512].rearrange("h d t -> d h t"), in_=dkTst), reads=[dkTstB])
        P.dma("sp", lambda e: e.dma_start(out=vd[:, :, q4 * 4:q4 * 4 + 4, :].rearrange("h p t c -> p h t c"), in_=vst), reads=[vstB])
        P.dma("sp", lambda e: e.dma_start(out=dvd[:, :, q4 * 4:q4 * 4 + 4, :].rearrange("h p t c -> p h t c"), in_=dvst), reads=[dvstB])

    def proj_tile(xsrc, ropesrc, r, own_cols, kv_cols, outs, t4, sample=False):
        P.dma("sp", lambda e: e.dma_start(out=xt[r], in_=xsrc), writes=[xtB[r]])
        P.dma("sp", lambda e: e.dma_start(out=rp[r], in_=ropesrc), writes=[rpB[r]])
        norm_h(xt[r], xtB[r], 0, 1)
        transposes(lambda k: hb[:, k * 128:(k + 1) * 128], 8, 128, [hbB], hT[r], hTB[r], eng="act")

        def inproj(c0, c1):
            pi = nextps()

            def f(e):
                for k in range(8):
                    ins = e.matmul(ps[pi][:, 0:c1 - c0], lhsT=hT[r][:, k, :], rhs=winb[:, k, c0:c1], start=(k == 0), stop=(k == 7))
                return ins
            P.op("pe", f, reads=[hTB[r], wB_], writes=[psB[pi]])
            return pi
        if kv_cols:
            o_lat_, o_kpe_, o_dk_, o_dv_ = outs
            pi = inproj(256, 544)
            rms_ops(ps[pi][:, 0:256], psB[pi], 256, junk[:, 0:256], (ssA[:, 1:2], ssB_[1]), "kv")
            P.op("dve", lambda e: e.scalar_tensor_tensor(out=latf[r], in0=ps[pi][:, 0:256], scalar=ssA[:, 1:2], in1=gains[:, 256:512], op0=ALU.mult, op1=ALU.mult),
                 reads=[psB[pi], ssB_[1], gB], writes=[latfB[r]])
            P.op("pool", lambda e: e.tensor_copy(out=latb, in_=latf[r]), reads=[latfB[r]], writes=[latbB])
            rope("dve", kpef[r].unsqueeze(1), ps[pi][:, 256:288].unsqueeze(1), rp[r], 1, [psB[pi], rpB[r]], [kpefB[r]])
            P.op("pool", lambda e: e.tensor_copy(out=kpeb, in_=kpef[r]), reads=[kpefB[r]], writes=[kpebB])
            P.dma("sp", lambda e: e.dma_start(out=o_lat_, in_=latf[r]), reads=[latfB[r]])
            P.dma("sp", lambda e: e.dma_start(out=o_kpe_, in_=kpef[r]), reads=[kpefB[r]])
            pk = inproj(1056, 1568)
            P.op("act", lambda e: e.activation(out=dkf[r], in_=ps[pk][:], func=AF.Copy), reads=[psB[pk]], writes=[dkfB[r]])
            P.op("dve", lambda e: e.tensor_copy(out=dkb, in_=ps[pk][:]), reads=[psB[pk]], writes=[dkbB])
            P.dma("sp", lambda e: e.dma_start(out=o_dk_, in_=dkf[r]), reads=[dkfB[r]])
            pv = inproj(1568, 2080)
            P.op("act", lambda e: e.activation(out=dvf[r], in_=ps[pv][:], func=AF.Copy), reads=[psB[pv]], writes=[dvfB[r]])
            dvdst = ndv[:] if sample else dvst[:, :, t4, :]
            P.op("dve", lambda e: e.tensor_copy(out=dvdst, in_=ps[pv][:].rearrange("p (h c) -> p h c", h=4)), reads=[psB[pv]], writes=[dvstB])
            P.dma("sp", lambda e: e.dma_start(out=o_dv_, in_=dvf[r]), reads=[dvfB[r]])
            if sample:
                dests = (nkT[0:96, :, :], ndkT[:], None, None)
            else:
                dests = (kTst[0:96, :, t4 * 128:(t4 + 1) * 128], dkTst[:, :, t4 * 128:(t4 + 1) * 128], vst, None)
            kside_derive(t4, dests, latb, kpeb, dkb, None, [latbB, kpebB, dkbB])
        if own_cols:
            pi = inproj(0, 256)
            rms_ops(ps[pi][:, 0:256], psB[pi], 256, junk[:, 0:256], (ssA[:, 2:3], ssB_[2]), "q")
            P.op("dve", lambda e: e.scalar_tensor_tensor(out=qnb, in0=ps[pi][:, 0:256], scalar=ssA[:, 2:3], in1=gains[:, 0:256], op0=ALU.mult, op1=ALU.mult),
                 reads=[psB[pi], ssB_[2], gB], writes=[qnbB])
            pq = inproj(544, 1056)
            P.op("act", lambda e: e.activation(out=dqb, in_=ps[pq][:], func=AF.Copy), reads=[psB[pq]], writes=[dqbB])
            transposes(lambda k: qnb[:, k * 128:(k + 1) * 128], 2, 128, [qnbB], qnT, qnTB)
            pa = nextps(); pb_ = nextps()

            def f(e):
                for hh, pj in ((0, pa), (1, pb_)):
                    for k in range(2):
                        ins = e.matmul(ps[pj][:, 0:384], lhsT=qnT[:, k, :], rhs=wuqb[:, k, hh * 384:(hh + 1) * 384], start=(k == 0), stop=(k == 1))
                return ins
            P.op("pe", f, reads=[qnTB, wB_], writes=[psB[pa], psB[pb_]])
            for hh, pj in ((0, pa), (1, pb_)):
                v4 = ps[pj][:, 0:384].rearrange("p (h c) -> p h c", h=4)
                P.op("act", lambda e, v4=v4, hh=hh: e.activation(out=qc[:, hh * 4:hh * 4 + 4, 0:64], in_=v4[:, :, 0:64], func=AF.Copy), reads=[psB[pj]], writes=[qcB])
                rope("dve", qc[:, hh * 4:hh * 4 + 4, 64:96], v4[:, :, 64:96], rp[r], 4, [psB[pj], rpB[r]], [qcB])
            if sample:
                qd = nqT[0:96, :, :]
                dq0, dq1 = ndqT[0:64, 0, :, :], ndqT[64:128, 1, :, :]
            else:
                qd = qTst[0:96, :, t4 * 128:(t4 + 1) * 128]
                dq0, dq1 = dqTst[0:64, 0, :, t4 * 128:(t4 + 1) * 128], dqTst[64:128, 1, :, t4 * 128:(t4 + 1) * 128]
            transposes(lambda h: qc[:, h, :], 8, 96, [qcB], qd, qTstB, eng="act")
            pq2 = nextps(); pbq = ps[pq2][:].bitcast(BF16)

            def ftq(e):
                for k in range(4):
                    ins = e.transpose(out=pbq[:, k * 128:(k + 1) * 128], in_=dqb[:, k * 128:(k + 1) * 128], identity=identb[:])
                return ins
            P.op("pe", ftq, reads=[dqbB, cB], writes=[psB[pq2]])
            P.op("dve", lambda e: e.tensor_copy(out=dq0, in_=pbq[0:64, 0:512].rearrange("p (a b) -> p a b", a=4)), reads=[psB[pq2]], writes=[dqTstB])
            P.op("dve", lambda e: e.tensor_copy(out=dq1, in_=pbq[64:128, 0:512].rearrange("p (a b) -> p a b", a=4)), reads=[psB[pq2], dqTstB], writes=[dqTstB])

    for t in range(64):
        if KCUT == 2 and t == 4:
            return finish()
        sl = slice(t * 128, (t + 1) * 128)
        proj_tile(x_all[sl, :], rope_all[sl, :], t % NR, False, True, (o_lat[sl, :], o_kpe[sl, :], o_dk[sl, :], o_dv[sl, :]), t % 4)
        if t % 4 == 3:
            flush_k(None, t // 4)
    if KCUT == 3:
        return finish()
    for t in range(32):
        sl = slice(t * 128, (t + 1) * 128)
        proj_tile(x_own[sl, :], rope_own[sl, :], t % NR, True, False, None, t % 4)
        if t % 4 == 3:
            c0 = (t // 4) * 512
            P.dma("sp", lambda e, c0=c0: e.dma_start(out=QTm[:, :, c0:c0 + 512].rearrange("h d t -> d h t"), in_=qTst[0:96, :, :]), reads=[qTstB])
            for m_ in range(2):
                P.dma("sp", lambda e, c0=c0, m_=m_: e.dma_start(out=QTd[:, m_, :, c0:c0 + 512].rearrange("h d t -> d h t"), in_=dqTst[:, m_, :, :]), reads=[dqTstB])
    if KCUT == 4:
        return finish()
    clb = [AR.bf([128, 256]) for _ in range(2)]; ckb = [AR.bf([128, 32]) for _ in range(2)]
    cdkb = [AR.bf([128, 512]) for _ in range(2)]; cdvb = [AR.bf([128, 512]) for _ in range(2)]
    ccB = [Buf(), Buf()]
    for s in range(2):
        for t in range(32):
            r = t % 2; sl = slice(t * 128, (t + 1) * 128)
            P.dma("pool", lambda e, r=r, s=s, sl=sl: e.dma_start(out=clb[r], in_=clat[s, sl, :]), writes=[ccB[r]])
            P.dma("pool", lambda e, r=r, s=s, sl=sl: e.dma_start(out=ckb[r], in_=ckr[s, sl, :]), writes=[ccB[r]])
            P.dma("pool", lambda e, r=r, s=s, sl=sl: e.dma_start(out=cdkb[r], in_=cdk[s, sl, :]), writes=[ccB[r]])
            P.dma("pool", lambda e, r=r, s=s, sl=sl: e.dma_start(out=cdvb[r], in_=cdv[s, sl, :]), writes=[ccB[r]])
            t4 = t % 4
            dests = (kTst[0:96, :, t4 * 128:(t4 + 1) * 128], dkTst[:, :, t4 * 128:(t4 + 1) * 128], vst, dvst[:, :, t4, :])
            kside_derive(t4, dests, clb[r], ckb[r], cdkb[r], cdvb[r], [ccB[r]])
            if t4 == 3:
                flush_k(s, t // 4)
    if KCUT == 5:
        return finish()
    load_mod(True, (0, 1))
    proj_tile(x_s, rope_s, 0, True, True, (o_lats, o_kpes, o_dks, o_dvs), 0, sample=True)
    if STAGE <= 1:
        return finish()
    if KCUT == 6:
        return finish()
    AR.reset()
    ktb = [AR.bf([128, 8192]) for _ in range(2)]; ktB = [Buf(), Buf()]
    vb = [AR.bf([128, 64, 128]) for _ in range(2)]; vB = [Buf(), Buf()]
    qtb = [AR.bf([128, 2, 4096]) for _ in range(2)]; qtB = [Buf(), Buf()]
    NPT = 8
    pt = [AR.bf([128, 512]) for _ in range(NPT)]; ptB = [Buf() for _ in range(NPT)]
    sbs = [AR.f32([128, 128]) for _ in range(2)]; sbsB = [Buf(), Buf()]
    rl = AR.f32([128, 2, 512]); rlB = Buf()
    bcs = AR.f32([128, 512]); bcsB = Buf()
    o1 = AR.f32([128, 512]); o2 = AR.f32([128, 512]); oB = Buf()
    sq = AR.f32([128, 512]); sqB = Buf()
    otile = [AR.bf([128, 512]) for _ in range(2)]; otB = [Buf(), Buf()]
    zt = AR.bf([128, 1024]); ztB = Buf()
    rings = {"s": 0, "p": 0, "o": 0, "b": 0}

    def ring(name, n):
        i = rings[name]; rings[name] = (i + 1) % n
        return i

    P.op("pool", lambda e: e.memset(zt, 0.0), writes=[ztB])

    def zero_fill():
        for XG in XGs:
            for c in range(HALF // 2048):
                P.dma("sp", lambda e, c=c, XG=XG: e.dma_start(out=XG[c * 2048:(c + 1) * 2048, :].rearrange("(p r) c -> p r c", p=128),
                                                             in_=zt.unsqueeze(1).broadcast_to([128, 16, 1024])), reads=[ztB])

    def load_pass(kind, h, slot, s=None):
        if s is None:
            if kind == "m":
                P.dma("sp", lambda e: e.dma_start(out=ktb[slot][0:96, :], in_=KTm[h]), writes=[ktB[slot]])
                P.dma("sp", lambda e: e.dma_start(out=vb[slot][:, :, 0:65], in_=Vm[h]), writes=[vB[slot]])
                P.dma("sp", lambda e: e.dma_start(out=qtb[slot][0:96, 0, :], in_=QTm[h]), writes=[qtB[slot]])
            else:
                P.dma("sp", lambda e: e.dma_start(out=ktb[slot], in_=KTd[h]), writes=[ktB[slot]])
                P.dma("sp", lambda e: e.dma_start(out=vb[slot], in_=Vd[h]), writes=[vB[slot]])
                P.dma("sp", lambda e: e.dma_start(out=qtb[slot], in_=QTd[h].rearrange("m d t -> d m t")), writes=[qtB[slot]])
        else:
            if kind == "m":
                P.dma("sp", lambda e: e.dma_start(out=ktb[slot][0:96, 0:4096], in_=sKTm[s, h]), writes=[ktB[slot]])
                P.dma("sp", lambda e: e.dma_start(out=vb[slot][:, 0:32, 0:65], in_=sVm[s, h]), writes=[vB[slot]])
            else:
                P.dma("sp", lambda e: e.dma_start(out=ktb[slot][:, 0:4096], in_=sKTd[s, h]), writes=[ktB[slot]])
                P.dma("sp", lambda e: e.dma_start(out=vb[slot][:, 0:32, :], in_=sVd[s, h]), writes=[vB[slot]])

    def fin_mla(acc, h, col0, N):
        P.op("dve", lambda e: e.reciprocal(out=rl[0:1, 0, 0:N], in_=ps[acc][0:1, 0:N]), reads=[psB[acc]], writes=[rlB])
        P.op("pe", lambda e: e.matmul(ps[7][0:65, 0:N], lhsT=onesf[0:1, 0:65], rhs=rl[0:1, 0, 0:N], start=True, stop=True), reads=[rlB, cB], writes=[psB[7]])
        P.op("dve", lambda e: e.tensor_copy(out=bcs[0:65, 0:N], in_=ps[7][0:65, 0:N]), reads=[psB[7]], writes=[bcsB])
        k = ring("o", 2); ot = otile[k]
        P.op("dve", lambda e: e.tensor_tensor(out=ot[0:65, 0:N], in0=ps[acc][0:65, 0:N], in1=bcs[0:65, 0:N], op=ALU.mult), reads=[psB[acc], bcsB], writes=[otB[k]])
        P.dma("sp", lambda e: e.dma_start(out=OT[h * 64:(h + 1) * 64, col0:col0 + N], in_=ot[1:65, 0:N]), reads=[otB[k]], waits=(tz if col0 >= 4096 else ()))

    def fin_diff(h, col0, N):
        P.op("dve", lambda e: e.reciprocal(out=rl[:, 0, 0:N], in_=ps[5][:, 0:N]), reads=[psB[5]], writes=[rlB])
        P.op("dve", lambda e: e.reciprocal(out=rl[:, 1, 0:N], in_=ps[6][:, 0:N]), reads=[psB[6], rlB], writes=[rlB])
        P.op("dve", lambda e: e.tensor_tensor(out=o1[:, 0:N], in0=ps[3][:, 0:N], in1=rl[:, 0, 0:N], op=ALU.mult), reads=[psB[3], rlB], writes=[oB])
        P.op("dve", lambda e: e.tensor_tensor(out=o2[:, 0:N], in0=ps[4][:, 0:N], in1=rl[:, 1, 0:N], op=ALU.mult), reads=[psB[4], rlB, oB], writes=[oB])
        P.op("dve", lambda e: e.scalar_tensor_tensor(out=o1[:, 0:N], in0=o2[:, 0:N], scalar=lamt[:, 0:1], in1=o1[:, 0:N], op0=ALU.mult, op1=ALU.add),
             reads=[oB, lamB], writes=[oB])
        P.op("pool", lambda e: e.tensor_tensor(out=sq[:, 0:N], in0=o1[:, 0:N], in1=o1[:, 0:N], op=ALU.mult), reads=[oB], writes=[sqB])
        P.op("pe", lambda e: e.matmul(ps[7][:, 0:N], lhsT=onesf[:], rhs=sq[:, 0:N], start=True, stop=True), reads=[sqB, cB], writes=[psB[7]])
        P.op("dve", lambda e: e.tensor_scalar(out=bcs[:, 0:N], in0=ps[7][:, 0:N], scalar1=128.0 * EPS, scalar2=None, op0=ALU.add), reads=[psB[7]], writes=[bcsB])
        P.op("pool", lambda e: e.tensor_tensor(out=bcs[:, 0:N], in0=bcs[:, 0:N], in1=nhalf[:, 0:1].broadcast_to([128, N]), op=ALU.pow), reads=[bcsB], writes=[bcsB])
        k = ring("o", 2); ot = otile[k]
        P.op("dve", lambda e: e.scalar_tensor_tensor(out=ot[:, 0:N], in0=o1[:, 0:N], scalar=lamt[:, 1:2], in1=bcs[:, 0:N], op0=ALU.mult, op1=ALU.mult),
             reads=[oB, bcsB, lamB], writes=[otB[k]])
        P.dma("sp", lambda e: e.dma_start(out=OT[512 + h * 128:512 + (h + 1) * 128, col0:col0 + N], in_=ot[:, 0:N]), reads=[otB[k]], waits=(tz if col0 >= 4096 else ()))

    def attn_tiles(kind, h, tiles, q_fn, qB_, col0, N, accs):
        pend = []
        n = len(tiles)
        nm = 1 if kind == "m" else 2
        LOOK = 3 if kind == "m" else 2

        def pv(item):
            i, tl, pjs = item
            for m in range(nm):
                pj = pjs[m]; c0 = tl["c0"]; rows = tl["rows"]
                first = (i == 0); last = (i == n - 1)
                parts = [(0, rows, c0)]
                for (r0, r1, cc) in parts:
                    vv = tl["v_ap"]
                    if kind == "m":
                        P.op("pe", lambda e, r0=r0, r1=r1, cc=cc, vv=vv, pj=pj, first=first, last=last: e.matmul(
                            ps[accs[0]][0:65, cc:N], lhsT=vv[r0:r1, 0:65], rhs=pt[pj][r0:r1, cc:N], start=first and r0 == 0, stop=last and r1 == rows),
                            reads=[ptB[pj], tl["vB"]], writes=[psB[accs[0]]])
                    else:
                        def f(e, r0=r0, r1=r1, cc=cc, vv=vv, pj=pj, first=first, last=last, m=m):
                            e.matmul(ps[accs[m]][:, cc:N], lhsT=vv[r0:r1, :], rhs=pt[pj][r0:r1, cc:N], start=first and r0 == 0, stop=last and r1 == rows)
                            return e.matmul(ps[accs[2 + m]][:, cc:N], lhsT=onesb[r0:r1, :], rhs=pt[pj][r0:r1, cc:N], start=first and r0 == 0, stop=last and r1 == rows)
                        P.op("pe", f, reads=[ptB[pj], tl["vB"], cB], writes=[psB[accs[m]], psB[accs[2 + m]]])

        for i, tl in enumerate(tiles):
            c0 = tl["c0"]; rows = tl["rows"]; pjs = []
            for m in range(nm):
                sk = (0, 1, 2, 7)[ring("s", 4)]; pj = ring("p", NPT); pjs.append(pj)
                kap = tl["k_ap"](m); qap = q_fn(m, c0)
                P.op("pe", lambda e, sk=sk, kap=kap, qap=qap, c0=c0, rows=rows: e.matmul(ps[sk][0:rows, c0:N], lhsT=kap, rhs=qap, start=True, stop=True),
                     reads=[tl["kB"], qB_], writes=[psB[sk]])
                if kind == "m":
                    P.op("act", lambda e, sk=sk, pj=pj, c0=c0, rows=rows: e.activation(out=pt[pj][0:rows, c0:N], in_=ps[sk][0:rows, c0:N], func=AF.Exp, scale=MLA_SCALE),
                         reads=[psB[sk]], writes=[ptB[pj]])
                else:
                    hm = h * 2 + m; b15 = Bb[0:rows, hm, 128:129]
                    if tl["bias"] is not None:
                        lo, w = tl["bias"]; w = min(w, N - c0)
                        bi = ring("b", 2)
                        P.op("dve", lambda e, sk=sk, bi=bi, c0=c0, w=w, lo=lo, hm=hm, rows=rows: e.scalar_tensor_tensor(
                            out=sbs[bi][0:rows, 0:w], in0=ps[sk][0:rows, c0:c0 + w], scalar=DIFF_SCALE, in1=Bb[0:rows, hm, lo:lo + w], op0=ALU.mult, op1=ALU.add),
                            reads=[psB[sk], BbB], writes=[sbsB[bi]])
                        P.op("act", lambda e, bi=bi, pj=pj, c0=c0, w=w, rows=rows: e.activation(out=pt[pj][0:rows, c0:c0 + w], in_=sbs[bi][0:rows, 0:w], func=AF.Exp),
                             reads=[sbsB[bi]], writes=[ptB[pj]])
                        if c0 + w < N:
                            P.op("act", lambda e, sk=sk, pj=pj, c0=c0, w=w, b15=b15, rows=rows: e.activation(
                                out=pt[pj][0:rows, c0 + w:N], in_=ps[sk][0:rows, c0 + w:N], func=AF.Exp, scale=DIFF_SCALE, bias=b15),
                                reads=[psB[sk], BbB, ptB[pj]], writes=[ptB[pj]])
                    else:
                        P.op("act", lambda e, sk=sk, pj=pj, c0=c0, b15=b15, rows=rows: e.activation(
                            out=pt[pj][0:rows, c0:N], in_=ps[sk][0:rows, c0:N], func=AF.Exp, scale=DIFF_SCALE, bias=b15),
                            reads=[psB[sk], BbB], writes=[ptB[pj]])
                if tl["diag"]:
                    P.op("pool", lambda e, pj=pj, c0=c0: e.tensor_tensor(out=pt[pj][64:128, c0:c0 + 64], in0=pt[pj][64:128, c0:c0 + 64], in1=mpib[64:128, :], op=ALU.mult),
                         reads=[ptB[pj], cB], writes=[ptB[pj]])
                if tl["pmask"] is not None:
                    pm = tl["pmask"]
                    P.op("pool", lambda e, pj=pj, pm=pm, rows=rows: e.tensor_tensor(out=pt[pj][0:rows, 0:N], in0=pt[pj][0:rows, 0:N], in1=pm.broadcast_to([rows, N]), op=ALU.mult),
                         reads=[ptB[pj], cB], writes=[ptB[pj]])
            pend.append((i, tl, pjs))
            if len(pend) > LOOK:
                pv(pend.pop(0))
        while pend:
            pv(pend.pop(0))

    def prompt_pass(kind, h, slot):
        kt_, v_, q_ = ktb[slot], vb[slot], qtb[slot]
        for G in range(8):
            tiles = []
            for kt in range(8 * G):
                bias = (64, 64) if (kt == 8 * G - 1) else None
                tiles.append(dict(kt=kt, c0=0, rows=128, diag=False, bias=bias, pmask=None))
            for k in range(8):
                tiles.append(dict(kt=8 * G + k, c0=64 * k, rows=128, diag=True, bias=(0, 128), pmask=None))
            for tl in tiles:
                kt = tl["kt"]
                if kind == "m":
                    tl["k_ap"] = (lambda m, kt=kt: kt_[0:96, kt * 128:(kt + 1) * 128])
                    tl["v_ap"] = v_[:, kt, :]
                else:
                    tl["k_ap"] = (lambda m, kt=kt: kt_[:, kt * 128:(kt + 1) * 128])
                    tl["v_ap"] = v_[:, kt, :]
                tl["kB"] = ktB[slot]; tl["vB"] = vB[slot]
            if kind == "m":
                acc = 3 + (G % 2)
                attn_tiles("m", h, tiles, lambda m, c0, G=G: q_[0:96, 0, G * 512 + c0:(G + 1) * 512], qtB[slot], G * 512, 512, [acc])
                fin_mla(acc, h, G * 512, 512)
            else:
                attn_tiles("d", h, tiles, lambda m, c0, G=G: q_[:, m, G * 512 + c0:(G + 1) * 512], qtB[slot], G * 512, 512, [3, 4, 5, 6])
                fin_diff(h, G * 512, 512)

    def sample_pass(kind, h, slot, s):
        kt_, v_ = ktb[slot], vb[slot]
        tiles = []
        for kt in range(32):
            tl = dict(c0=0, rows=128, diag=False, bias=((192, 16) if kt == 31 else None), pmask=None, kB=ktB[slot], vB=vB[slot], v_ap=v_[:, kt, :])
            if kind == "m":
                tl["k_ap"] = (lambda m, kt=kt: kt_[0:96, kt * 128:(kt + 1) * 128])
            else:
                tl["k_ap"] = (lambda m, kt=kt: kt_[:, kt * 128:(kt + 1) * 128])
            tiles.append(tl)
        tl = dict(c0=0, rows=48, diag=False, bias=((256 + 64 * s, 16)), pmask=C2[0:48, 512 + s:513 + s], kB=nB, vB=nB)
        if kind == "m":
            tl["k_ap"] = (lambda m: nkT[0:96, h, 0:48]); tl["v_ap"] = nv[:, h, :]
            qf = lambda m, c0: nqT[0:96, h, s * 32:s * 32 + 16]
        else:
            tl["k_ap"] = (lambda m: ndkT[:, h, 0:48]); tl["v_ap"] = ndv[:, h, :]
            qf = lambda m, c0: ndqT[:, m, h, s * 32:s * 32 + 16]
        tiles.append(tl)
        col0 = 4096 + s * 32
        if kind == "m":
            acc = 3 + (ring("o2", 2) if False else 0)
            attn_tiles("m", h, tiles, qf, nB, col0, 16, [3])
            fin_mla(3, h, col0, 16)
        else:
            attn_tiles("d", h, tiles, qf, nB, col0, 16, [3, 4, 5, 6])
            fin_diff(h, col0, 16)

    passes = [("m", h) for h in range(8)] + [("d", h) for h in range(4)]
    load_pass(passes[0][0], passes[0][1], 0)
    tz = [P.dma("sp", lambda e: e.dma_start(out=OT[:, 4096:4224].rearrange("(k p) t -> p k t", p=128), in_=zt[:, 0:1024].rearrange("p (k t) -> p k t", k=8)), reads=[ztB])]
    zero_fill()
    for i, (kind, h) in enumerate(passes):
        if i + 1 < len(passes):
            load_pass(passes[i + 1][0], passes[i + 1][1], (i + 1) % 2)
        prompt_pass(kind, h, i % 2)
    sp_list = [(kind, h, s) for s in range(2) for (kind, h) in passes]
    load_pass(sp_list[0][0], sp_list[0][1], 0, s=sp_list[0][2])
    for i, (kind, h, s) in enumerate(sp_list):
        if i + 1 < len(sp_list):
            load_pass(sp_list[i + 1][0], sp_list[i + 1][1], (i + 1) % 2, s=sp_list[i + 1][2])
        sample_pass(kind, h, i % 2, s)
    if KCUT == 7:
        return finish()

    AR.reset()
    woutb = AR.bf([128, 8, 1024]); wrb = AR.bf([128, 8, 256]); wsgb = AR.bf([128, 8, 512]); wsdb = AR.bf([128, 2, 1024])
    wDB = [Buf() for _ in range(4)]
    P.dma("pool", lambda e: e.dma_start(out=woutb, in_=w_out.rearrange("(k p) n -> p k n", p=128)), writes=[wDB[0]])
    P.dma("pool", lambda e: e.dma_start(out=wrb, in_=w_router.rearrange("(k p) n -> p k n", p=128)), writes=[wDB[1]])
    P.dma("pool", lambda e: e.dma_start(out=wsgb, in_=w_sgu.rearrange("(k p) n -> p k n", p=128)), writes=[wDB[2]])
    P.dma("pool", lambda e: e.dma_start(out=wsdb, in_=w_sdn.rearrange("(k p) n -> p k n", p=128)), writes=[wDB[3]])
    wrf = AR.f32([128, 8, 256]); wrfB = Buf()
    P.dma("sp", lambda e: e.dma_start(out=wrf, in_=w_router.rearrange("(k p) n -> p k n", p=128)), writes=[wrfB])
    h2ff = AR.f32([128, 1024]); h2ffB = Buf(); h2Tf = AR.f32([128, 8, 128]); h2TfB = Buf()
    rbb = AR.f32([128, 256]); rbbB = Buf()
    P.dma("sp", lambda e: e.dma_start(out=rbb, in_=rbias.broadcast_to([128, 256])), writes=[rbbB])
    oTs = [AR.bf([128, 8, 512]) for _ in range(2)]; oTsB = [Buf(), Buf()]
    xd = [AR.f32([128, 1024]) for _ in range(2)]; xdB = [Buf(), Buf()]
    x1 = AR.f32([128, 1024]); x1B = Buf()
    tmpd = AR.f32([128, 1024]); tmpdB = Buf()
    h2f = AR.f32([128, 1024]); h2fB = Buf()
    h2b = [AR.bf([128, 1024]) for _ in range(2)]; h2bB = [Buf(), Buf()]
    h2T = AR.bf([128, 8, 128]); h2TB = Buf()
    junkd = AR.bf([128, 1024])
    ssD = AR.f32([128, 8]); ssDB = Buf()
    sc = AR.f32([128, 256]); scB = Buf()
    sgd = AR.f32([128, 256]); sgdB = Buf()
    abd = AR.bf([128, 256]); abdB = Buf(); aTd = AR.bf([128, 2, 128]); aTdB = Buf()
    biased = AR.f32([128, 256]); m8 = AR.f32([128, 8, 8]); gs = AR.f32([128, 8]); t8 = AR.f32([128, 8]); gm = AR.f32([128, 8])
    masked = AR.f32([128, 256]); v8 = AR.f32([128, 8]); sel = AR.f32([128, 256]); gsel = AR.f32([128, 256]); den = AR.f32([128, 8])
    Gt = AR.f32([128, 256]); selb = AR.bf([128, 256]); posf = AR.f32([128, 256]); key = AR.f32([128, 256]); d8 = AR.f32([128, 8]); neg = AR.f32([128, 8])
    neg2 = AR.f32([128, 8]); dA = AR.f32([128, 8]); dB = AR.f32([128, 8])
    key2 = AR.f32([128, 256]); key3 = AR.f32([128, 256]); v8b = AR.f32([128, 8]); v8c = AR.f32([128, 8])
    rtB = Buf()
    selbB = Buf()

    def rt(fn, extra_r=(), extra_w=()):
        P.op("dve", fn, reads=[rtB] + list(extra_r), writes=[rtB] + list(extra_w))

    def phaseD_tile(tile):
        sample = (tile == 32)
        r = tile % 2
        if sample:
            P.dma("sp", lambda e: e.dma_start(out=oTs[0][:, :, 0:128], in_=OT[:, 4096:4224].rearrange("(k p) t -> p k t", p=128)), writes=[oTsB[0]])
            oT = oTs[0]; oTB = oTsB[0]; tc0 = 0
            xsrc = x_s
        else:
            G = tile // 4
            if tile % 4 == 0:
                P.dma("sp", lambda e: e.dma_start(out=oTs[G % 2], in_=OT[:, G * 512:(G + 1) * 512].rearrange("(k p) t -> p k t", p=128)), writes=[oTsB[G % 2]])
            oT = oTs[G % 2]; oTB = oTsB[G % 2]; tc0 = (tile % 4) * 128
            xsrc = x_own[tile * 128:(tile + 1) * 128, :]
        P.dma("sp", lambda e: e.dma_start(out=xd[r], in_=xsrc), writes=[xdB[r]])
        pa = nextps(); pb_ = nextps()

        def f(e):
            for nh, pj in ((0, pa), (1, pb_)):
                for k in range(8):
                    ins = e.matmul(ps[pj][:], lhsT=oT[:, k, tc0:tc0 + 128], rhs=woutb[:, k, nh * 512:(nh + 1) * 512], start=(k == 0), stop=(k == 7))
            return ins
        P.op("pe", f, reads=[oTB, wDB[0]], writes=[psB[pa], psB[pb_]])
        for nh, pj in ((0, pa), (1, pb_)):
            P.op("dve", lambda e, nh=nh, pj=pj: e.tensor_tensor(out=tmpd[:, nh * 512:(nh + 1) * 512], in0=ps[pj][:], in1=M[2][:, nh * 512:(nh + 1) * 512], op=ALU.mult),
                 reads=[psB[pj], MB[2]], writes=[tmpdB])
        P.op("pool", lambda e: e.tensor_tensor(out=x1, in0=tmpd, in1=xd[r], op=ALU.add), reads=[tmpdB, xdB[r]], writes=[x1B])
        P.op("pool", lambda e: e.memset(ssD[:, 0:1], 0.0), writes=[ssDB])
        P.op("act", lambda e: e.activation(out=junkd, in_=x1, func=AF.Square, accum_out=ssD[:, 0:1]), reads=[x1B], writes=[ssDB])
        P.op("dve", lambda e: e.tensor_scalar(out=ssD[:, 0:1], in0=ssD[:, 0:1], scalar1=1.0 / 1024, scalar2=EPS, op0=ALU.mult, op1=ALU.add), reads=[ssDB], writes=[ssDB])
        P.op("pool", lambda e: e.tensor_tensor(out=ssD[:, 0:1], in0=ssD[:, 0:1], in1=nhalf[:, 0:1], op=ALU.pow), reads=[ssDB], writes=[ssDB])
        P.op("dve", lambda e: e.scalar_tensor_tensor(out=h2f, in0=x1, scalar=ssD[:, 0:1], in1=M[3][:], op0=ALU.mult, op1=ALU.mult), reads=[x1B, ssDB, MB[3]], writes=[h2fB])
        P.op("pool", lambda e: e.tensor_tensor(out=h2ff, in0=h2f, in1=M[4][:], op=ALU.add), reads=[h2fB, MB[4]], writes=[h2ffB])
        P.op("pool", lambda e: e.tensor_copy(out=h2b[r], in_=h2ff), reads=[h2ffB], writes=[h2bB[r]])
        transposes(lambda k: h2b[r][:, k * 128:(k + 1) * 128], 8, 128, [h2bB[r]], h2T, h2TB, eng="act")
        for hh in range(2):
            pt_ = nextps()

            def ft(e, pt_=pt_, hh=hh):
                for k in range(4):
                    ins = e.transpose(out=ps[pt_][:, k * 128:(k + 1) * 128], in_=h2ff[:, (hh * 4 + k) * 128:(hh * 4 + k + 1) * 128], identity=identf)
                return ins
            P.op("pe", ft, reads=[h2ffB, CFb], writes=[psB[pt_]])
            P.op("dve", lambda e, pt_=pt_, hh=hh: e.tensor_copy(out=h2Tf[:, hh * 4:hh * 4 + 4, :], in_=ps[pt_][:].rearrange("p (a b) -> p a b", a=4)),
                 reads=[psB[pt_]], writes=[h2TfB])
        pr = nextps(); pg = nextps()

        def f2(e):
            for k in range(8):
                e.matmul(ps[pr][:, 0:256], lhsT=h2Tf[:, k, :], rhs=wrf[:, k, :], start=(k == 0), stop=(k == 7))
            for k in range(8):
                ins = e.matmul(ps[pg][:], lhsT=h2T[:, k, :], rhs=wsgb[:, k, :], start=(k == 0), stop=(k == 7))
            return ins
        P.op("pe", f2, reads=[h2TB, h2TfB, wrfB, wDB[2]], writes=[psB[pr], psB[pg]])
        P.op("act", lambda e: e.activation(out=sc, in_=ps[pr][:, 0:256], func=AF.Sigmoid), reads=[psB[pr]], writes=[scB])
        P.op("act", lambda e: e.activation(out=sgd, in_=ps[pg][:, 0:256], func=AF.Silu), reads=[psB[pg]], writes=[sgdB])
        P.op("dve", lambda e: e.tensor_tensor(out=abd, in0=ps[pg][:, 256:512], in1=sgd, op=ALU.mult), reads=[psB[pg], sgdB], writes=[abdB])
        transposes(lambda k: abd[:, k * 128:(k + 1) * 128], 2, 128, [abdB], aTd, aTdB)
        pa2 = nextps(); pb2 = nextps()

        def f3(e):
            for nh, pj in ((0, pa2), (1, pb2)):
                for k in range(2):
                    ins = e.matmul(ps[pj][:], lhsT=aTd[:, k, :], rhs=wsdb[:, k, nh * 512:(nh + 1) * 512], start=(k == 0), stop=(k == 1))
            return ins
        P.op("pe", f3, reads=[aTdB, wDB[3]], writes=[psB[pa2], psB[pb2]])
        for nh, pj in ((0, pa2), (1, pb2)):
            P.op("dve", lambda e, nh=nh, pj=pj: e.tensor_tensor(out=tmpd[:, nh * 512:(nh + 1) * 512], in0=ps[pj][:], in1=M[5][:, nh * 512:(nh + 1) * 512], op=ALU.mult),
                 reads=[psB[pj], MB[5]], writes=[tmpdB])
        P.op("pool", lambda e: e.tensor_tensor(out=x1, in0=tmpd, in1=x1, op=ALU.add), reads=[tmpdB, x1B], writes=[x1B])
        P.dma("sp", lambda e: e.dma_start(out=X1[tile * 128:(tile + 1) * 128, :], in_=x1), reads=[x1B])
        rt(lambda e: e.tensor_tensor(out=biased, in0=sc, in1=rbb, op=ALU.add), extra_r=[scB, rbbB])
        for g in range(8):
            rt(lambda e, g=g: e.max(out=m8[:, g, :], in_=biased[:, g * 32:(g + 1) * 32]))
        rt(lambda e: e.tensor_tensor(out=gs, in0=m8[:, :, 0], in1=m8[:, :, 1], op=ALU.add))
        rt(lambda e: e.max(out=t8, in_=gs))
        rt(lambda e: e.tensor_single_scalar(out=gm, in_=gs, scalar=t8[:, 3:4], op=ALU.is_ge))
        rt(lambda e: e.tensor_scalar(out=gm, in0=gm, scalar1=-1.0, scalar2=1e9, op0=ALU.add, op1=ALU.mult))
        rt(lambda e: e.tensor_tensor(out=masked.rearrange("p (g c) -> p g c", g=8), in0=biased.rearrange("p (g c) -> p g c", g=8),
                                     in1=gm.unsqueeze(2).broadcast_to([128, 8, 32]), op=ALU.add))
        rt(lambda e: e.max(out=v8, in_=masked))
        rt(lambda e: e.tensor_single_scalar(out=sel, in_=masked, scalar=v8[:, 7:8], op=ALU.is_ge))
        vcol = C2[:, 514:515] if sample else C2[:, 515:516]
        rt(lambda e: e.tensor_scalar(out=sel, in0=sel, scalar1=vcol, scalar2=None, op0=ALU.mult), extra_r=[cB])
        rt(lambda e: e.tensor_tensor(out=gsel, in0=sel, in1=sc, op=ALU.mult))
        rt(lambda e: e.tensor_reduce(out=den[:, 0:1], in_=gsel, axis=AX.X, op=ALU.add))
        rt(lambda e: e.tensor_scalar(out=den[:, 0:1], in0=den[:, 0:1], scalar1=1e-20, scalar2=None, op0=ALU.add))
        rt(lambda e: e.reciprocal(out=den[:, 1:2], in_=den[:, 0:1]))
        rt(lambda e: e.tensor_scalar(out=Gt, in0=gsel, scalar1=den[:, 1:2], scalar2=2.5, op0=ALU.mult, op1=ALU.mult))
        P.op("pool", lambda e: e.tensor_copy(out=selb, in_=sel), reads=[rtB], writes=[selbB])
        pp = nextps(); pc = nextps()

        def f4(e):
            e.matmul(ps[pp][:, 0:256], lhsT=Ub[:], rhs=selb, start=True, stop=True)
            return e.matmul(ps[pc][:, 0:256], lhsT=onesb[:], rhs=selb, start=True, stop=True)
        P.op("pe", f4, reads=[selbB, cB], writes=[psB[pp], psB[pc]])
        rt(lambda e: e.tensor_tensor(out=posf, in0=ps[pp][:, 0:256], in1=cntbc[:], op=ALU.add), extra_r=[psB[pp], cntB])
        rt(lambda e: e.tensor_tensor(out=cntbc[:], in0=ps[pc][:, 0:256], in1=cntbc[:], op=ALU.add), extra_r=[psB[pc]], extra_w=[cntB])
        rt(lambda e: e.tensor_single_scalar(out=key, in_=posf, scalar=float(CAP), op=ALU.is_lt))
        rt(lambda e: e.tensor_tensor(out=sel, in0=sel, in1=key, op=ALU.mult))
        rt(lambda e: e.tensor_tensor(out=posf, in0=posf, in1=C2[:, 0:256], op=ALU.add))
        rt(lambda e: e.tensor_tensor(out=key, in0=posf, in1=sel, op=ALU.mult))
        rt(lambda e: e.max(out=d8, in_=key))
        rt(lambda e: e.tensor_scalar(out=d8, in0=d8, scalar1=-1.0, scalar2=None, op0=ALU.add))
        rt(lambda e: e.tensor_single_scalar(out=neg, in_=d8, scalar=0.0, op=ALU.is_lt))
        rt(lambda e: e.tensor_single_scalar(out=neg2, in_=d8, scalar=float(HALF), op=ALU.is_ge))
        rt(lambda e: e.tensor_tensor(out=neg, in0=neg, in1=neg2, op=ALU.add))
        rt(lambda e: e.scalar_tensor_tensor(out=dA, in0=neg, scalar=BIG, in1=d8, op0=ALU.mult, op1=ALU.add))
        rt(lambda e: e.tensor_copy(out=didx[:, tile, 0:8], in_=dA))
        rt(lambda e: e.tensor_scalar(out=dB, in0=neg2, scalar1=-1.0, scalar2=-BIG, op0=ALU.add, op1=ALU.mult))
        rt(lambda e: e.scalar_tensor_tensor(out=dB, in0=d8, scalar=-float(HALF), in1=dB, op0=ALU.add, op1=ALU.add))
        rt(lambda e: e.tensor_copy(out=didx[:, tile, 8:16], in_=dB))
        rt(lambda e: e.tensor_scalar(out=neg, in0=neg, scalar1=-1.0, scalar2=-1.0, op0=ALU.add, op1=ALU.mult))
        rt(lambda e: e.tensor_tensor(out=dA, in0=d8, in1=neg, op=ALU.mult))
        rt(lambda e: e.tensor_copy(out=didx[:, tile, 16:24], in_=dA))
        rt(lambda e: e.scalar_tensor_tensor(out=dB, in0=d8, scalar=-float(HALF), in1=neg2, op0=ALU.add, op1=ALU.mult))
        rt(lambda e: e.tensor_copy(out=didx[:, tile, 24:32], in_=dB), extra_w=[dgB[tile]])
        rt(lambda e: e.tensor_tensor(out=key3, in0=C2[:, 256:512], in1=sel, op=ALU.mult))
        rt(lambda e: e.tensor_tensor(out=key2, in0=Gt, in1=sel, op=ALU.mult))
        rt(lambda e: e.tensor_tensor(out=key2, in0=key2, in1=key3, op=ALU.add))
        rt(lambda e: e.max(out=v8b, in_=key2))
        rt(lambda e: e.max(out=v8c, in_=key3))
        rt(lambda e: e.tensor_tensor(out=v8b, in0=v8b, in1=v8c, op=ALU.subtract))
        rt(lambda e: e.tensor_tensor(out=gate8[:, tile, 0:8], in0=v8b, in1=neg, op=ALU.mult))
        rt(lambda e: e.tensor_tensor(out=gate8[:, tile, 8:16], in0=v8b, in1=neg2, op=ALU.mult), extra_w=[dgB[tile]])
        for j in range(16):
            P.dma("pool", lambda e, j=j: e.indirect_dma_start(out=XGs[j // 8][:, :], out_offset=bass.IndirectOffsetOnAxis(ap=didx[:, tile, j:j + 1], axis=0),
                                                            in_=h2b[r][:, :], in_offset=None, bounds_check=getbc(e), oob_is_err=False),
                  reads=[h2bB[r], dgB[tile]])

    load_mod(False, (0, 1))
    for tile in range(32):
        phaseD_tile(tile)
    load_mod(True, (2, 3, 4, 5))
    phaseD_tile(32)
    if KCUT == 8:
        return finish()

    AR.reset()
    wg = [AR.bf([128, 8, 512]) for _ in range(2)]; wgB = [Buf(), Buf()]
    wd = [AR.bf([128, 2, 1024]) for _ in range(2)]; wdB = [Buf(), Buf()]
    xg = [AR.bf([128, NST, 1024]) for _ in range(2)]; xgB = [Buf(), Buf()]
    xgT = [AR.bf([128, 8, CAP]) for _ in range(2)]; xgTB = [Buf(), Buf()]
    sge = [AR.f32([128, 2, 320]) for _ in range(2)]; sgeB = [Buf(), Buf()]
    aTe = [AR.bf([128, 2, CAP]) for _ in range(2)]; aTeB = [Buf(), Buf()]
    yb = [AR.bf([128, NST, 1024]) for _ in range(2)]; ybB = [Buf(), Buf()]
    HN = CAP // 2

    def e_s1(ex):
        r = ex % 2
        XG = XGs[ex // 128]; row0 = (ex % 128) * CAP
        P.dma("pool", lambda e: e.dma_start(out=wg[r], in_=w_gu[ex].rearrange("(k p) n -> p k n", p=128)), writes=[wgB[r]])
        P.dma("pool", lambda e: e.dma_start(out=wd[r], in_=w_dn[ex].rearrange("(k p) n -> p k n", p=128)), writes=[wdB[r]])
        P.dma("sp", lambda e: e.dma_start(out=xg[r], in_=XG[row0:row0 + CAP, :].rearrange("(s p) c -> p s c", p=128)), writes=[xgB[r]])
        for s_ in range(NST):
            transposes(lambda k, s_=s_: xg[r][:, s_, k * 128:(k + 1) * 128], 8, 128, [xgB[r]], xgT[r][:, :, s_ * 128:(s_ + 1) * 128], xgTB[r],
                       eng=("act" if s_ % 2 == 0 else "dve"))

    def e_s2(ex):
        r = ex % 2
        for nh in range(2):
            pbs = [nextps() for _ in range(4)]

            def f(e, pbs=pbs, nh=nh):
                for c in range(4):
                    for k in range(8):
                        ins = e.matmul(ps[pbs[c]][:, 0:HN], lhsT=wg[r][:, k, c * 128:(c + 1) * 128], rhs=xgT[r][:, k, nh * HN:(nh + 1) * HN], start=(k == 0), stop=(k == 7))
                return ins
            P.op("pe", f, reads=[wgB[r], xgTB[r]], writes=[psB[p_] for p_ in pbs])
            for c in range(2):
                P.op("act", lambda e, c=c, pbs=pbs, nh=nh: e.activation(out=sge[nh][:, c, 0:HN], in_=ps[pbs[c]][:, 0:HN], func=AF.Silu), reads=[psB[pbs[c]]], writes=[sgeB[nh]])
                P.op("dve", lambda e, c=c, pbs=pbs, nh=nh: e.tensor_tensor(out=aTe[r][:, c, nh * HN:(nh + 1) * HN], in0=ps[pbs[2 + c]][:, 0:HN], in1=sge[nh][:, c, 0:HN], op=ALU.mult),
                     reads=[psB[pbs[2 + c]], sgeB[nh]], writes=[aTeB[r]])

    def e_s3(ex):
        r = ex % 2
        YG = YGs[ex // 128]; row0 = (ex % 128) * CAP
        for s_ in range(NST):
            for nh in range(2):
                pj = nextps()

                def f2(e, pj=pj, s_=s_, nh=nh):
                    for k in range(2):
                        ins = e.matmul(ps[pj][:], lhsT=aTe[r][:, k, s_ * 128:(s_ + 1) * 128], rhs=wd[r][:, k, nh * 512:(nh + 1) * 512], start=(k == 0), stop=(k == 1))
                    return ins
                P.op("pe", f2, reads=[aTeB[r], wdB[r]], writes=[psB[pj]])
                if (s_ + nh) % 2 == 0:
                    P.op("act", lambda e, pj=pj, s_=s_, nh=nh: e.activation(out=yb[r][:, s_, nh * 512:(nh + 1) * 512], in_=ps[pj][:], func=AF.Copy), reads=[psB[pj]], writes=[ybB[r]])
                else:
                    P.op("dve", lambda e, pj=pj, s_=s_, nh=nh: e.tensor_copy(out=yb[r][:, s_, nh * 512:(nh + 1) * 512], in_=ps[pj][:]), reads=[psB[pj]], writes=[ybB[r]])
        P.dma("sp", lambda e: e.dma_start(out=YG[row0:row0 + CAP, :].rearrange("(s p) c -> p s c", p=128), in_=yb[r]), reads=[ybB[r]])

    e_s1(0)
    for ex in range(256):
        if ex + 1 < 256:
            e_s1(ex + 1)
        e_s2(ex)
        e_s3(ex)
    if KCUT == 9:
        return finish()

    AR.reset()
    yjS = [[AR.bf([128, 1024]) for _ in range(16)] for _ in range(2)]; yjBS = [[Buf() for _ in range(16)] for _ in range(2)]
    fcount = [0]
    accF = AR.f32([128, 1024]); accB = Buf()
    x1f = AR.f32([128, 1024]); x1fB = Buf()
    x2 = AR.f32([128, 1024]); x2B = Buf()
    yo = AR.f32([128, 1024]); yoB = Buf()
    fnb = AR.f32([128, 1024]); fnbB = Buf()
    junkf = AR.bf([128, 1024]); ssF = AR.f32([128, 8]); ssFB = Buf()
    P.dma("sp", lambda e: e.dma_start(out=fnb, in_=fnorm.broadcast_to([128, 1024])), writes=[fnbB])
    for q_ in range(2):
        for j in range(16):
            P.op("pool", lambda e, j=j, q_=q_: e.memset(yjS[q_][j], 0.0), writes=[yjBS[q_][j]])

    def phaseF_tile(tile):
        yj = yjS[fcount[0] % 2]; yjB = yjBS[fcount[0] % 2]; fcount[0] += 1
        for j in range(16):
            P.dma("pool", lambda e, j=j: e.indirect_dma_start(out=yj[j][:, :], out_offset=None, in_=YGs[j // 8][:, :],
                                                            in_offset=bass.IndirectOffsetOnAxis(ap=didx[:, tile, 16 + j:17 + j], axis=0),
                                                            bounds_check=getbc(e), oob_is_err=False), reads=[dgB[tile]], writes=[yjB[j]])
        P.dma("sp", lambda e: e.dma_start(out=x1f, in_=X1[tile * 128:(tile + 1) * 128, :]), writes=[x1fB])
        P.op("dve", lambda e: e.tensor_scalar(out=accF, in0=yj[0], scalar1=gate8[:, tile, 0:1], scalar2=None, op0=ALU.mult), reads=[yjB[0], dgB[tile]], writes=[accB])
        for j in range(1, 16):
            P.op("dve", lambda e, j=j: e.scalar_tensor_tensor(out=accF, in0=yj[j], scalar=gate8[:, tile, j:j + 1], in1=accF, op0=ALU.mult, op1=ALU.add),
                 reads=[yjB[j], dgB[tile], accB], writes=[accB])
        P.op("dve", lambda e: e.tensor_tensor(out=accF, in0=accF, in1=M[5][:], op=ALU.mult), reads=[accB, MB[5]], writes=[accB])
        P.op("pool", lambda e: e.tensor_tensor(out=x2, in0=accF, in1=x1f, op=ALU.add), reads=[accB, x1fB], writes=[x2B])
        P.op("pool", lambda e: e.memset(ssF[:, 0:1], 0.0), writes=[ssFB])
        P.op("act", lambda e: e.activation(out=junkf, in_=x2, func=AF.Square, accum_out=ssF[:, 0:1]), reads=[x2B], writes=[ssFB])
        P.op("dve", lambda e: e.tensor_scalar(out=ssF[:, 0:1], in0=ssF[:, 0:1], scalar1=1.0 / 1024, scalar2=EPS, op0=ALU.mult, op1=ALU.add), reads=[ssFB], writes=[ssFB])
        P.op("pool", lambda e: e.tensor_tensor(out=ssF[:, 0:1], in0=ssF[:, 0:1], in1=nhalf[:, 0:1], op=ALU.pow), reads=[ssFB], writes=[ssFB])
        P.op("dve", lambda e: e.scalar_tensor_tensor(out=yo, in0=x2, scalar=ssF[:, 0:1], in1=fnb, op0=ALU.mult, op1=ALU.mult), reads=[x2B, ssFB, fnbB], writes=[yoB])
        dst = o_ys if tile == 32 else o_y[tile * 128:(tile + 1) * 128, :]
        P.dma("sp", lambda e: e.dma_start(out=dst, in_=yo), reads=[yoB])

    phaseF_tile(32)
    load_mod(False, (5,))
    for tile in range(32):
        phaseF_tile(tile)
    return finish()


_CACHE = {}


def _bucket(rel):
    rel = np.asarray(rel, np.int64)
    n = np.abs(rel)
    nf = np.maximum(n, 1).astype(np.float32)
    large = 8 + (np.log(nf / np.float32(8)) / np.float32(math.log(128 / 8)) * np.float32(8)).astype(np.int32)
    large = np.minimum(large, 15)
    return np.where(rel > 0, 16, 0) + np.where(n < 8, n, large)


def _consts(pi):
    cst = np.zeros((128, 1600), np.float32)
    cst[:, 0:128] = np.eye(128, dtype=np.float32)
    cst[:, 128:256] = np.eye(128, dtype=np.float32)[::-1]
    offs = [64 * pi, 64 * pi + 128, 64 * pi + 256, 128, 0, 32]
    oh = np.zeros((32, 6, 192), np.float32)
    for b, off in enumerate(offs):
        j = np.arange(192)
        bk = _bucket(127 - j - off)
        oh[bk, b, j] = 1.0
    cst[0:32, 256:1408] = oh.reshape(32, 1152)
    cst[0:64, 1408:1472] = 1.0
    cst[64:128, 1408:1472] = float(pi)
    cst[:, 1472:1600] = np.triu(np.ones((128, 128), np.float32), 1)
    return cst


def _consts2():
    c = np.zeros((128, 520), np.float32)
    e = np.arange(256, dtype=np.float32)
    c[:, 0:256] = e * CAP + 1.0
    c[:, 256:512] = 4.0 * e + 1.0
    c[0:16, 512] = 1.0; c[32:48, 513] = 1.0
    c[0:16, 514] = 1.0; c[32:48, 514] = 1.0
    c[:, 515] = 1.0
    return c


def _rope_tab(pos):
    half = 16
    inv = (10000.0 ** (-np.arange(half, dtype=np.float32) / half)).astype(np.float32)
    ang = pos.astype(np.float32)[:, None] * inv
    c = np.cos(ang).astype(np.float32); s = np.sin(ang).astype(np.float32)
    return np.concatenate([c, c, -s, s], axis=1).astype(np.float32)


def _in_maps(inp, cores=range(8)):
    f = lambda a: np.ascontiguousarray(np.asarray(a, dtype=np.float32))
    xp = f(inp["x_prompt"]); xs = f(inp["x_sample"])
    in_maps = []
    for c in cores:
        b, pi = c // 2, c % 2
        xo = xp[b].reshape(64, 2, 64, 1024)[:, pi].reshape(4096, 1024)
        pos_own = (np.arange(64)[:, None] * 128 + pi * 64 + np.arange(64)[None, :]).reshape(-1)
        x_s = np.zeros((128, 1024), np.float32); x_s[0:16] = xs[2 * c]; x_s[32:48] = xs[2 * c + 1]
        rs = np.zeros((128, 64), np.float32); rs[:, 0:32] = 1.0
        rs[0:16] = _rope_tab(4096 + np.arange(16)); rs[32:48] = rs[0:16]
        m = {
            "x_all": xp[b], "x_own": f(xo), "x_s": x_s,
            "clat": f(inp["cache_mla_latent"][0, 2 * c:2 * c + 2]), "ckr": f(inp["cache_mla_krope"][0, 2 * c:2 * c + 2]),
            "cdk": f(inp["cache_diff_k"][0, 2 * c:2 * c + 2]).reshape(2, 4096, 512),
            "cdv": f(inp["cache_diff_v"][0, 2 * c:2 * c + 2]).reshape(2, 4096, 512),
            "c3": f(np.stack([inp["c_prompt"][b], inp["c_sample"][2 * c], inp["c_sample"][2 * c + 1]])),
            "w_ada": f(inp["w_ada"][0]), "b_ada": f(inp["b_ada"]), "norm_attn": f(inp["norm_attn"]), "w_in": f(inp["w_in"][0]),
            "qg": f(inp["mla_q_norm"]), "kvg": f(inp["mla_kv_norm"]), "w_uq": f(inp["w_uq"][0]), "w_ukv": f(inp["w_ukv"][0]),
            "lam4": f(np.concatenate([inp["lambda_q1"], inp["lambda_k1"], inp["lambda_q2"], inp["lambda_k2"]], 0)),
            "subln": f(inp["diff_subln"]).reshape(128, 1), "relb": f(inp["rel_bias"]).reshape(32, 8),
            "w_out": f(inp["w_out"][0]), "norm_ffn": f(inp["norm_ffn"]), "w_router": f(inp["w_router"][0]), "rbias": f(inp["router_bias"]),
            "w_sgu": f(inp["w_shared_gu"][0]), "w_sdn": f(inp["w_shared_down"][0]),
            "fnorm": f(inp["final_norm"]).reshape(1, 1024),
            "cst": _consts(pi), "cst2": _consts2(), "rope_all": _rope_tab(np.arange(8192)), "rope_own": _rope_tab(pos_own), "rope_s": rs,
        }
        if STAGE > 1:
            m["w_gu"] = f(inp["w_exp_gu"][0]); m["w_dn"] = f(inp["w_exp_down"][0])
        in_maps.append(m)
    return in_maps


def kernel(**inp):
    if "prog" not in _CACHE:
        _CACHE["prog"] = build_program()
    nc, _es = _CACHE["prog"]
    in_maps = _in_maps(inp)
    res = run_bass_kernel_spmd(nc, in_maps, core_ids=list(range(8))).results
    y_p = np.zeros((4, 8192, 1024), np.float32); y_s = np.zeros((16, 16, 1024), np.float32)
    lat_p = np.zeros((1, 4, 8192, 256), np.float32); kpe_p = np.zeros((1, 4, 8192, 32), np.float32)
    dk_p = np.zeros((1, 4, 8192, 4, 2, 64), np.float32); dv_p = np.zeros((1, 4, 8192, 4, 128), np.float32)
    lat_s = np.zeros((1, 16, 16, 256), np.float32); kpe_s = np.zeros((1, 16, 16, 32), np.float32)
    dk_s = np.zeros((1, 16, 16, 4, 2, 64), np.float32); dv_s = np.zeros((1, 16, 16, 4, 128), np.float32)
    for c in range(8):
        b, pi = c // 2, c % 2
        r = res[c]
        y_p[b].reshape(64, 2, 64, 1024)[:, pi] = r["o_y"].reshape(64, 64, 1024)
        if pi == 0:
            lat_p[0, b] = r["o_lat"]; kpe_p[0, b] = r["o_kpe"]
            dk_p[0, b] = r["o_dk"].reshape(8192, 4, 2, 64); dv_p[0, b] = r["o_dv"].reshape(8192, 4, 128)
        for s in range(2):
            sl = slice(32 * s, 32 * s + 16)
            y_s[2 * c + s] = r["o_ys"][sl]
            lat_s[0, 2 * c + s] = r["o_lats"][sl]; kpe_s[0, 2 * c + s] = r["o_kpes"][sl]
            dk_s[0, 2 * c + s] = r["o_dks"][sl].reshape(16, 4, 2, 64); dv_s[0, 2 * c + s] = r["o_dvs"][sl].reshape(16, 4, 128)
    return (y_p, y_s, lat_p, kpe_p, dk_p, dv_p, lat_s, kpe_s, dk_s, dv_s)
```
